# Optimizing a Trainium2 kernel written in Bass

```python
import math
import jax
import jax.numpy as jnp
from jax import lax
import numpy as np

D_MODEL = 1024
BATCH = 32
SEQ = 2048
DEPTH = 4

GRID_W = 64
CTX_LEN = 256
N_MOD = 6
EPS = 1e-6
W_A = 256
W_B = 256
W_C = 256
W_D = 256
D_MIX = W_A + W_B + W_C + W_D
LRU_HEADS = 4
LRU_HEAD_DIM = W_A // LRU_HEADS
LRU_CONV = 4
RG_C = 8.0
DIFF_HEADS = 4
DIFF_DV = W_B // DIFF_HEADS
DIFF_DK = DIFF_DV // 2
ROPE_BASE = 10000.0
ROPE_FREQS = DIFF_DK // 4
Q_BLOCK = 128
CONF_K = 31
HYENA_ORDER = 2
HYENA_SHORT = 3
HYENA_EMB = 33
HYENA_BANDS = (HYENA_EMB - 1) // 2
HYENA_FFN = 64
HYENA_MIN_DECAY = -math.log(1e-2) / 1.5
HYENA_MAX_DECAY = -math.log(1e-2) / 0.3
N_GROUPS = 4
EXPERTS_PER_GROUP = 8
N_EXPERTS = N_GROUPS * EXPERTS_PER_GROUP
TOP_K = 2
D_EXPERT = 512
MOE_BLOCK = 128
IN_A = 2 * W_A
IN_B = 3 * W_B
IN_C = 2 * W_C
IN_D = (HYENA_ORDER + 1) * W_D
OFF_B = IN_A
OFF_C = OFF_B + IN_B
OFF_D = OFF_C + IN_C
D_IN = OFF_D + IN_D

kernel_name = 'hybrid_parallel_groups_dit_block'


def rms_norm(x, g, eps=EPS):
    xf = x.astype(jnp.float32)
    y = xf * lax.rsqrt(jnp.mean(xf * xf, -1, keepdims=True) + eps)
    return (y * g).astype(x.dtype)


def layer_norm(x, g, b, eps=1e-5):
    xf = x.astype(jnp.float32)
    mu = jnp.mean(xf, -1, keepdims=True)
    var = jnp.mean(jnp.square(xf - mu), -1, keepdims=True)
    return ((xf - mu) * lax.rsqrt(var + eps) * g + b).astype(x.dtype)


def modulate(x, shift, scale):
    return x * (1.0 + scale) + shift


def dwconv(x, w, b, pad):
    y = lax.conv_general_dilated(
        x, w.astype(x.dtype)[:, None, :], window_strides=(1,), padding=[pad],
        dimension_numbers=('NWC', 'WIO', 'NWC'), feature_group_count=x.shape[-1])
    return y + b


def linear_scan(a, b, h0, reverse):
    edge = -1 if reverse else 0
    b = b.at[:, edge].add(a[:, edge] * h0)

    def combine(left, right):
        a1, b1 = left
        a2, b2 = right
        return a1 * a2, a2 * b1 + b2

    _, h = lax.associative_scan(combine, (a, b), reverse=reverse, axis=1)
    return h


def rglru_direction(x, conv_w, conv_b, w_r, b_r, w_i, b_i, lam, pad, reverse, h0):
    u = dwconv(x, conv_w, conv_b, pad)
    B_, L, _ = u.shape
    uh = u.reshape(B_, L, LRU_HEADS, LRU_HEAD_DIM)
    r = jax.nn.sigmoid((jnp.einsum('blhi,hij->blhj', uh, w_r).reshape(B_, L, W_A) + b_r).astype(jnp.float32))
    i = jax.nn.sigmoid((jnp.einsum('blhi,hij->blhj', uh, w_i).reshape(B_, L, W_A) + b_i).astype(jnp.float32))
    log_a = -RG_C * r * jax.nn.softplus(-lam.astype(jnp.float32))
    a = jnp.exp(log_a)
    b = jnp.sqrt(-jnp.expm1(2.0 * log_a)) * (i * u.astype(jnp.float32))
    return linear_scan(a, b, h0, reverse)


def rglru_mixer(pa, pa_c, conv_w, conv_b, w_r, b_r, w_i, b_i, lam, ctx_out):
    xr, xg = pa[..., :W_A], pa[..., W_A:]
    xr_c, xg_c = pa_c[..., :W_A], pa_c[..., W_A:]
    h_lat, h_ctx = [], []
    for d, reverse in enumerate((False, True)):
        pad = (0, LRU_CONV - 1) if reverse else (LRU_CONV - 1, 0)
        prm = (conv_w[d], conv_b[d], w_r[d], b_r[d], w_i[d], b_i[d], lam[d], pad, reverse)
        hc = rglru_direction(xr_c, *prm, jnp.zeros((xr_c.shape[0], W_A), jnp.float32))
        h_final = hc[:, 0] if reverse else hc[:, -1]
        h_lat.append(rglru_direction(xr, *prm, h_final))
        h_ctx.append(hc)
    y = (h_lat[0] + h_lat[1]) * jax.nn.gelu(xg)
    y_c = (h_ctx[0] + h_ctx[1]) * jax.nn.gelu(xg_c) if ctx_out else None
    return y, y_c


def axial_rope_tables(L):
    rows = L // GRID_W
    row = jnp.repeat(jnp.arange(rows, dtype=jnp.float32), GRID_W)
    col = jnp.tile(jnp.arange(GRID_W, dtype=jnp.float32), rows)
    inv = ROPE_BASE ** (-jnp.arange(ROPE_FREQS, dtype=jnp.float32) / ROPE_FREQS)
    ang = jnp.concatenate([row[:, None] * inv, col[:, None] * inv], -1)
    return jnp.cos(ang), jnp.sin(ang)


def apply_rope(x, cos, sin):
    half = x.shape[-1] // 2
    cs = cos[None, :, None, None, :]
    sn = sin[None, :, None, None, :]
    x1, x2 = x[..., :half], x[..., half:]
    return jnp.concatenate([x1 * cs - x2 * sn, x1 * sn + x2 * cs], -1).astype(x.dtype)


def diff_softmax(q, k, v, lam):
    s = jnp.einsum('bqhcd,bkhcd->bhcqk', q, k).astype(jnp.float32) * (DIFF_DK ** -0.5)
    p = jax.nn.softmax(s, -1)
    w = p[:, :, 0] - lam * p[:, :, 1]
    return jnp.einsum('bhqk,bkhd->bqhd', w, v)


def diff_attn_mixer(pb, pb_c, rope_cos, rope_sin, lq1, lk1, lq2, lk2, sub_g, lam_init, ctx_out):
    B_, L, _ = pb.shape
    Lc = pb_c.shape[1]
    lam = (jnp.exp(jnp.sum(lq1.astype(jnp.float32) * lk1)) -
           jnp.exp(jnp.sum(lq2.astype(jnp.float32) * lk2)) + lam_init)

    def split_qkv(pp, n):
        q, k, v = jnp.split(pp, 3, -1)
        return (q.reshape(B_, n, DIFF_HEADS, 2, DIFF_DK), k.reshape(B_, n, DIFF_HEADS, 2, DIFF_DK),
                v.reshape(B_, n, DIFF_HEADS, DIFF_DV))

    q, k, v = split_qkv(pb, L)
    q = apply_rope(q, rope_cos, rope_sin)
    k = apply_rope(k, rope_cos, rope_sin)
    q_c, k_c, v_c = split_qkv(pb_c, Lc)
    keys = jnp.concatenate([k, k_c], 1)
    vals = jnp.concatenate([v, v_c], 1)
    n_blk = L // Q_BLOCK
    qb = jnp.moveaxis(q.reshape(B_, n_blk, Q_BLOCK, DIFF_HEADS, 2, DIFF_DK), 1, 0)
    o = lax.map(lambda qq: diff_softmax(qq, keys, vals, lam), qb)
    o = jnp.moveaxis(o, 0, 1).reshape(B_, L, DIFF_HEADS, DIFF_DV)

    def head_out(oo):
        return (rms_norm(oo, sub_g) * (1.0 - lam_init)).reshape(oo.shape[0], oo.shape[1], W_B)

    y = head_out(o)
    y_c = head_out(diff_softmax(q_c, k_c, v_c, lam)) if ctx_out else None
    return y, y_c


def conformer_conv(pc, conv_w, conv_b, ln_g, ln_b, w_pw, b_pw):
    u = pc[..., :W_C] * jax.nn.sigmoid(pc[..., W_C:])
    u = dwconv(u, conv_w, conv_b, (CONF_K // 2, CONF_K // 2))
    u = layer_norm(u, ln_g, ln_b)
    return jax.nn.silu(u) @ w_pw + b_pw


def hyena_filters(L, w_f1, b_f1, freq, w_f2, b_f2, w_f3, decay):
    t = jnp.arange(L, dtype=jnp.float32)
    t_unit = t / max(L - 1, 1)
    bands = jnp.linspace(1e-4, HYENA_BANDS - 1, HYENA_BANDS, dtype=jnp.float32)
    ang = (2.0 * math.pi / L) * t[:, None] * bands[None, :]
    feats = jnp.concatenate([t_unit[:, None], jnp.cos(ang), -jnp.sin(ang)], -1)
    f = jnp.sin(freq * (feats @ w_f1 + b_f1))
    f = jnp.sin(freq * (f @ w_f2 + b_f2))
    h = (f @ w_f3) * jnp.exp(-t_unit[:, None] * jnp.abs(decay))
    h = h.astype(jnp.float32).reshape(L, HYENA_ORDER, 2, W_D)
    energy = jnp.sum(jnp.square(h[:, :, 0]), 0) + jnp.sum(jnp.square(h[1:, :, 1]), 0)
    return h * lax.rsqrt(energy + EPS)[None, :, None, :]


def long_conv_bidir(z, h_fwd, h_bwd, bias):
    B_, L, C = z.shape
    n = 2 * L
    taps = jnp.concatenate([h_fwd, jnp.zeros((1, C), jnp.float32), h_bwd[:0:-1]], 0)
    zf = z.astype(jnp.float32)
    y = jnp.fft.irfft(jnp.fft.rfft(zf, n=n, axis=1) * jnp.fft.rfft(taps, n=n, axis=0)[None], n=n, axis=1)
    return y[:, :L] + zf * bias


def hyena_mixer(pd, filt, conv_w, conv_b, h_bias):
    u = dwconv(pd, conv_w, conv_b, (HYENA_SHORT // 2, HYENA_SHORT // 2))
    v, x1, x2 = jnp.split(u, HYENA_ORDER + 1, -1)
    z = v
    for o, gate in enumerate((x1, x2)):
        z = gate * long_conv_bidir(z, filt[:, o, 0], filt[:, o, 1], h_bias[o])
    return z


def hier_moe(h, w_rg, b_rg, w_re, b_re, w_gate, w_up, w_down):
    T, D = h.shape
    hf = h.astype(jnp.float32)
    p_grp, g_idx = lax.top_k(jax.nn.softmax(hf @ w_rg.astype(jnp.float32) + b_rg, -1), 1)
    le = (hf @ w_re.astype(jnp.float32) + b_re).reshape(T, N_GROUPS, EXPERTS_PER_GROUP)
    le = jnp.take_along_axis(le, g_idx[:, :, None], axis=1)[:, 0]
    p_in, e_in = lax.top_k(jax.nn.softmax(le, -1), TOP_K)
    p_in = p_in / jnp.sum(p_in, -1, keepdims=True)
    gate = (p_grp * p_in).reshape(-1)
    expert = (g_idx * EXPERTS_PER_GROUP + e_in).reshape(-1)
    token = jnp.repeat(jnp.arange(T, dtype=jnp.int32), TOP_K)
    n_assign = T * TOP_K
    order = jnp.argsort(expert)
    e_sorted = expert[order]
    counts = jnp.bincount(expert, length=N_EXPERTS)
    padded = (counts + MOE_BLOCK - 1) // MOE_BLOCK * MOE_BLOCK
    start = jnp.cumsum(counts) - counts
    p_end = jnp.cumsum(padded)
    p_start = p_end - padded
    dest = p_start[e_sorted] + jnp.arange(n_assign, dtype=jnp.int32) - start[e_sorted]
    n_slots = -(-n_assign // MOE_BLOCK) * MOE_BLOCK + N_EXPERTS * MOE_BLOCK
    n_blk = n_slots // MOE_BLOCK
    slot_tok = jnp.full((n_slots,), T, jnp.int32).at[dest].set(token[order])
    slot_gate = jnp.zeros((n_slots,), jnp.float32).at[dest].set(gate[order])
    blk_expert = jnp.minimum(
        jnp.searchsorted(p_end, jnp.arange(n_blk, dtype=jnp.int32) * MOE_BLOCK, side='right'), N_EXPERTS - 1)
    h_pad = jnp.concatenate([h, jnp.zeros((1, D), h.dtype)], 0)

    def expert_block(args):
        toks, gw, e = args
        xb = h_pad[toks]
        hid = jax.nn.silu(xb @ w_gate[e]) * (xb @ w_up[e])
        return (hid @ w_down[e]) * gw[:, None]

    y = lax.map(expert_block, (slot_tok.reshape(n_blk, MOE_BLOCK),
                               slot_gate.reshape(n_blk, MOE_BLOCK), blk_expert))
    y = jax.ops.segment_sum(y.reshape(n_slots, D), slot_tok, num_segments=T + 1)
    return y[:T]


def setup_inputs(seed: int = 0) -> dict:
    key = jax.random.key(seed)
    ks = iter(jax.random.split(key, 64))

    def nrm(shape, scale):
        return scale * jax.random.normal(next(ks), shape, jnp.float32)

    def gain(shape):
        return 1.0 + nrm(shape, 0.05)

    x = nrm((BATCH, SEQ, D_MODEL), 1.0)
    c = nrm((BATCH, D_MODEL), 1.0)
    ctx = nrm((BATCH, CTX_LEN, D_MODEL), 1.0)
    c_ctx = nrm((D_MODEL,), 1.0)
    w_ada = nrm((DEPTH, D_MODEL, N_MOD * D_MODEL), 0.5 * D_MODEL ** -0.5)
    b_ada = nrm((DEPTH, N_MOD * D_MODEL), 0.02)
    g_mix = gain((DEPTH, D_MODEL))
    g_ffn = gain((DEPTH, D_MODEL))
    w_in = nrm((DEPTH, D_MODEL, D_IN), D_MODEL ** -0.5)
    w_out = nrm((DEPTH, D_MIX, D_MODEL), D_MIX ** -0.5)
    a_conv_w = nrm((DEPTH, 2, LRU_CONV, W_A), LRU_CONV ** -0.5)
    a_conv_b = nrm((DEPTH, 2, W_A), 0.02)
    a_w_r = nrm((DEPTH, 2, LRU_HEADS, LRU_HEAD_DIM, LRU_HEAD_DIM), LRU_HEAD_DIM ** -0.5)
    a_b_r = nrm((DEPTH, 2, W_A), 0.02)
    a_w_i = nrm((DEPTH, 2, LRU_HEADS, LRU_HEAD_DIM, LRU_HEAD_DIM), LRU_HEAD_DIM ** -0.5)
    a_b_i = nrm((DEPTH, 2, W_A), 0.02)
    a_pow = jax.random.uniform(next(ks), (DEPTH, 2, W_A), jnp.float32, 0.9, 0.999)
    a_base = a_pow ** (1.0 / RG_C)
    a_lam = jnp.log(a_base) - jnp.log1p(-a_base)
    b_lq1 = nrm((DEPTH, DIFF_DK), 0.1)
    b_lk1 = nrm((DEPTH, DIFF_DK), 0.1)
    b_lq2 = nrm((DEPTH, DIFF_DK), 0.1)
    b_lk2 = nrm((DEPTH, DIFF_DK), 0.1)
    b_sub_g = gain((DEPTH, DIFF_DV))
    c_conv_w = nrm((DEPTH, CONF_K, W_C), CONF_K ** -0.5)
    c_conv_b = nrm((DEPTH, W_C), 0.02)
    c_ln_g = gain((DEPTH, W_C))
    c_ln_b = nrm((DEPTH, W_C), 0.02)
    c_w_pw = nrm((DEPTH, W_C, W_C), W_C ** -0.5)
    c_b_pw = nrm((DEPTH, W_C), 0.02)
    d_conv_w = nrm((DEPTH, HYENA_SHORT, IN_D), HYENA_SHORT ** -0.5)
    d_conv_b = nrm((DEPTH, IN_D), 0.02)
    d_w_f1 = nrm((DEPTH, HYENA_EMB, HYENA_FFN), HYENA_EMB ** -0.5)
    d_b_f1 = nrm((DEPTH, HYENA_FFN), 0.1)
    d_freq = gain((DEPTH, HYENA_FFN))
    d_w_f2 = nrm((DEPTH, HYENA_FFN, HYENA_FFN), HYENA_FFN ** -0.5)
    d_b_f2 = nrm((DEPTH, HYENA_FFN), 0.1)
    d_w_f3 = nrm((DEPTH, HYENA_FFN, 2 * HYENA_ORDER * W_D), HYENA_FFN ** -0.5)
    decay0 = jnp.tile(jnp.linspace(HYENA_MIN_DECAY, HYENA_MAX_DECAY, W_D, dtype=jnp.float32), 2 * HYENA_ORDER)
    d_decay = decay0[None, :] + nrm((DEPTH, 2 * HYENA_ORDER * W_D), 0.1)
    d_bias = nrm((DEPTH, HYENA_ORDER, W_D), 1.0)
    moe_w_rg = nrm((DEPTH, D_MODEL, N_GROUPS), D_MODEL ** -0.5)
    moe_b_rg = nrm((DEPTH, N_GROUPS), 0.01)
    moe_w_re = nrm((DEPTH, D_MODEL, N_EXPERTS), D_MODEL ** -0.5)
    moe_b_re = nrm((DEPTH, N_EXPERTS), 0.01)
    moe_w_gate = nrm((DEPTH, N_EXPERTS, D_MODEL, D_EXPERT), D_MODEL ** -0.5)
    moe_w_up = nrm((DEPTH, N_EXPERTS, D_MODEL, D_EXPERT), D_MODEL ** -0.5)
    moe_w_down = nrm((DEPTH, N_EXPERTS, D_EXPERT, D_MODEL), D_EXPERT ** -0.5)
    g_final = gain((D_MODEL,))
    return {
        'x': x, 'c': c, 'ctx': ctx, 'c_ctx': c_ctx,
        'w_ada': w_ada, 'b_ada': b_ada, 'g_mix': g_mix, 'g_ffn': g_ffn, 'w_in': w_in, 'w_out': w_out,
        'a_conv_w': a_conv_w, 'a_conv_b': a_conv_b, 'a_w_r': a_w_r, 'a_b_r': a_b_r,
        'a_w_i': a_w_i, 'a_b_i': a_b_i, 'a_lam': a_lam,
        'b_lq1': b_lq1, 'b_lk1': b_lk1, 'b_lq2': b_lq2, 'b_lk2': b_lk2, 'b_sub_g': b_sub_g,
        'c_conv_w': c_conv_w, 'c_conv_b': c_conv_b, 'c_ln_g': c_ln_g, 'c_ln_b': c_ln_b,
        'c_w_pw': c_w_pw, 'c_b_pw': c_b_pw,
        'd_conv_w': d_conv_w, 'd_conv_b': d_conv_b, 'd_w_f1': d_w_f1, 'd_b_f1': d_b_f1, 'd_freq': d_freq,
        'd_w_f2': d_w_f2, 'd_b_f2': d_b_f2, 'd_w_f3': d_w_f3, 'd_decay': d_decay, 'd_bias': d_bias,
        'moe_w_rg': moe_w_rg, 'moe_b_rg': moe_b_rg, 'moe_w_re': moe_w_re, 'moe_b_re': moe_b_re,
        'moe_w_gate': moe_w_gate, 'moe_w_up': moe_w_up, 'moe_w_down': moe_w_down,
        'g_final': g_final,
    }


def reference(x, c, ctx, c_ctx, w_ada, b_ada, g_mix, g_ffn, w_in, w_out,
              a_conv_w, a_conv_b, a_w_r, a_b_r, a_w_i, a_b_i, a_lam,
              b_lq1, b_lk1, b_lq2, b_lk2, b_sub_g,
              c_conv_w, c_conv_b, c_ln_g, c_ln_b, c_w_pw, c_b_pw,
              d_conv_w, d_conv_b, d_w_f1, d_b_f1, d_freq, d_w_f2, d_b_f2, d_w_f3, d_decay, d_bias,
              moe_w_rg, moe_b_rg, moe_w_re, moe_b_re, moe_w_gate, moe_w_up, moe_w_down,
              g_final):
    out_dtype = x.dtype
    B_, L, D = x.shape
    Lc = ctx.shape[1]
    n_lat = B_ * L
    rope_cos, rope_sin = axial_rope_tables(L)
    s_lat = jax.nn.silu(c.astype(jnp.float32))
    s_ctx = jax.nn.silu(c_ctx.astype(jnp.float32))
    xc = ctx
    for l in range(DEPTH):
        last = l == DEPTH - 1
        mod = jnp.split((s_lat @ w_ada[l] + b_ada[l])[:, None, :], N_MOD, -1)
        mod_c = jnp.split(s_ctx @ w_ada[l] + b_ada[l], N_MOD, -1)
        lam_init = 0.8 - 0.6 * math.exp(-0.3 * l)
        conf = (c_conv_w[l], c_conv_b[l], c_ln_g[l], c_ln_b[l], c_w_pw[l], c_b_pw[l])
        hy = (d_w_f1[l], d_b_f1[l], d_freq[l], d_w_f2[l], d_b_f2[l], d_w_f3[l], d_decay[l])
        h = modulate(rms_norm(x, g_mix[l]), mod[0], mod[1])
        hc = modulate(rms_norm(xc, g_mix[l]), mod_c[0], mod_c[1])
        p = h @ w_in[l]
        p_c = hc @ w_in[l][:, :(OFF_C if last else D_IN)]
        y_a, yc_a = rglru_mixer(p[..., :OFF_B], p_c[..., :OFF_B], a_conv_w[l], a_conv_b[l],
                                a_w_r[l], a_b_r[l], a_w_i[l], a_b_i[l], a_lam[l], not last)
        y_b, yc_b = diff_attn_mixer(p[..., OFF_B:OFF_C], p_c[..., OFF_B:OFF_C], rope_cos, rope_sin,
                                    b_lq1[l], b_lk1[l], b_lq2[l], b_lk2[l], b_sub_g[l], lam_init, not last)
        y_c = conformer_conv(p[..., OFF_C:OFF_D], *conf)
        y_d = hyena_mixer(p[..., OFF_D:], hyena_filters(L, *hy), d_conv_w[l], d_conv_b[l], d_bias[l])
        x = x + mod[2] * (jnp.concatenate([y_a, y_b, y_c, y_d], -1) @ w_out[l])
        if not last:
            yc_c = conformer_conv(p_c[..., OFF_C:OFF_D], *conf)
            yc_d = hyena_mixer(p_c[..., OFF_D:], hyena_filters(Lc, *hy), d_conv_w[l], d_conv_b[l], d_bias[l])
            xc = xc + mod_c[2] * (jnp.concatenate([yc_a, yc_b, yc_c, yc_d], -1) @ w_out[l])
        moe = (moe_w_rg[l], moe_b_rg[l], moe_w_re[l], moe_b_re[l], moe_w_gate[l], moe_w_up[l], moe_w_down[l])
        h = modulate(rms_norm(x, g_ffn[l]), mod[3], mod[4]).reshape(n_lat, D)
        if last:
            x = x + mod[5] * hier_moe(h, *moe).reshape(B_, L, D)
        else:
            hc = modulate(rms_norm(xc, g_ffn[l]), mod_c[3], mod_c[4]).reshape(B_ * Lc, D)
            f = hier_moe(jnp.concatenate([h, hc], 0), *moe)
            x = x + mod[5] * f[:n_lat].reshape(B_, L, D)
            xc = xc + mod_c[5] * f[n_lat:].reshape(B_, Lc, D)
    return rms_norm(x, g_final).astype(out_dtype)
```

```python
import math
import os
from contextlib import ExitStack
import numpy as np
import ml_dtypes
import concourse.bass as bass
import concourse.mybir as mybir
from concourse.bass_utils import run_bass_kernel_spmd

F32 = mybir.dt.float32
BF16 = mybir.dt.bfloat16
I32 = mybir.dt.int32
ALU = mybir.AluOpType
AF = mybir.ActivationFunctionType
AX = mybir.AxisListType

D = 1024
L = 2048
LC = 256
NB_ = 4
DEPTH = 4
PAD = 16
C0 = PAD
L0 = PAD + LC + PAD
PW = L0 + L + PAD
TOK = LC + L
NTOK = NB_ * TOK
EPS = 1e-6
FULL_TILES = [(0, 512), (512, 512), (1024, 512), (1536, 512), (2048, PW - 2048)]
VT = [(C0, LC)] + [(L0 + 512 * i, 512) for i in range(4)]
TT = [(C0 + 128 * i, True) for i in range(2)] + [(L0 + 128 * i, False) for i in range(16)]
N_DMA_SEMS = 12
MOE_S = 512
NE = 32


class Buf:
    __slots__ = ("t", "last_w", "readers", "name")

    def __init__(self, t, name=""):
        self.t = t
        self.last_w = None
        self.readers = []
        self.name = name

    def __getitem__(self, idx):
        return self.t[idx]


class K:
    def __init__(self, nc, es):
        self.nc = nc
        self.es = es
        self.eng = {"pe": nc.tensor, "act": nc.scalar, "dve": nc.vector, "pool": nc.gpsimd, "sp": nc.sync}
        self.sem = {}
        self.cnt = {}
        for e in self.eng:
            self.sem[e] = es.enter_context(nc.semaphore("s_" + e))
            self.cnt[e] = 0
        self.dsem = {}
        self.dcnt = {}
        self.dnext = {}
        for q in ("sp", "act", "pool"):
            self.dsem[q] = [es.enter_context(nc.semaphore("d_%s%d" % (q, i))) for i in range(N_DMA_SEMS)]
            self.dcnt[q] = [0] * N_DMA_SEMS
            self.dnext[q] = 0
        self.seen = {e: {} for e in self.eng}
        self.n_wait = 0
        self.n_inst = 0
        self.uid = 0

    def nm(self, s):
        self.uid += 1
        return "%s_%d" % (s, self.uid)

    def sb(self, name, shape, dt, es=None):
        t = (es or self.es).enter_context(self.nc.sbuf_tensor(self.nm(name), shape, dt))
        return Buf(t, name)

    def ps(self, name, shape, dt=F32, es=None):
        t = (es or self.es).enter_context(self.nc.psum_tensor(self.nm(name), shape, dt))
        return Buf(t, name)

    def dram(self, name, shape, dt, kind="Internal"):
        t = self.nc.dram_tensor(name, shape, dt, kind=kind)
        return Buf(t, name)

    def _semh(self, key):
        if key[0] == "e":
            return self.sem[key[1]]
        return self.dsem[key[1]][key[2]]

    def _wait(self, e, tok):
        key, val = tok
        if self.seen[e].get(key, 0) >= val:
            return
        self.eng[e].wait_ge(self._semh(key), val)
        self.seen[e][key] = val
        self.n_wait += 1

    def _deps(self, e, reads, writes):
        for b in reads:
            if b.last_w is not None:
                self._dep1(e, b.last_w, "raw")
        for b in writes:
            if b.last_w is not None:
                self._dep1(e, b.last_w, "waw")
            for r in b.readers:
                self._dep1(e, r, "war")

    def _dep1(self, e, tok, kind):
        key = tok[0]
        if key[0] == "e" and key[1] == e:
            if e == "pe" or kind != "raw":
                return
        self._wait(e, tok)

    def _commit(self, tok, reads, writes):
        for b in reads:
            if len(b.readers) > 24:
                b.readers = b.readers[-24:] if False else b.readers
            b.readers.append(tok)
        for b in writes:
            b.last_w = tok
            b.readers = []

    def op(self, e, fn, reads=(), writes=()):
        self._deps(e, reads, writes)
        inst = fn(self.eng[e])
        self.cnt[e] += 1
        inst.then_inc(self.sem[e], 1)
        self.n_inst += 1
        tok = (("e", e), self.cnt[e])
        self._commit(tok, reads, writes)
        return tok

    def _dq(self, q, reads, writes):
        self._deps(q, reads, writes)
        i = self.dnext[q]
        self.dnext[q] = (i + 1) % N_DMA_SEMS
        key = ("d", q, i)
        if self.dcnt[q][i] > 0:
            self._wait(q, (key, self.dcnt[q][i]))
        return i, key

    def dma(self, q, out, in_, reads=(), writes=(), **kw):
        i, key = self._dq(q, reads, writes)
        inst = self.eng[q].dma_start(out=out, in_=in_, **kw)
        self.dcnt[q][i] += 16
        inst.then_inc(self.dsem[q][i], 16)
        tok = (key, self.dcnt[q][i])
        self._commit(tok, reads, writes)
        self.n_inst += 1
        return tok

    def idma(self, out, out_off, in_, in_off, reads=(), writes=(), **kw):
        q = "pool"
        i, key = self._dq(q, reads, writes)
        inst = self.eng[q].indirect_dma_start(out=out, out_offset=out_off, in_=in_, in_offset=in_off, **kw)
        self.dcnt[q][i] += 16
        inst.then_inc(self.dsem[q][i], 16)
        tok = (key, self.dcnt[q][i])
        self._commit(tok, reads, writes)
        self.n_inst += 1
        return tok

    def wait_all(self, e):
        for e2 in self.eng:
            if self.cnt[e2] > 0 and e2 != e:
                self._wait(e, (("e", e2), self.cnt[e2]))
        for q in self.dsem:
            for i in range(N_DMA_SEMS):
                if self.dcnt[q][i] > 0:
                    self._wait(e, (("d", q, i), self.dcnt[q][i]))

    def barrier(self):
        for e in self.eng:
            self.wait_all(e)


def _bf(a):
    return np.asarray(a, np.float32).astype(ml_dtypes.bfloat16)


def host_consts():
    c = {}
    c["ident_f"] = np.eye(128, dtype=np.float32)
    c["ident_b"] = _bf(np.eye(128))
    c["ones_f"] = np.ones((128, 128), np.float32)
    c["ones_b"] = _bf(np.ones((128, 128)))
    blk = np.zeros((128, 128), np.float32)
    blk[:64, :64] = 1
    blk[64:, 64:] = 1
    c["blk64_f"] = blk
    c["utri_b"] = _bf(np.triu(np.ones((128, 128)), 1))
    inv = 10000.0 ** (-np.arange(8, dtype=np.float32) / 8)
    t = np.arange(L)
    row = (t // 64).astype(np.float32)
    col = (t % 64).astype(np.float32)
    ang = np.concatenate([row[:, None] * inv, col[:, None] * inv], -1)
    cosf = np.ones((128, PW), np.float32)
    sinf = np.zeros((128, PW), np.float32)
    rot = np.zeros((128, 128), np.float32)
    for ch in range(128):
        d = ch % 32
        cosf[ch, L0:L0 + L] = np.cos(ang[:, d % 16])
        s = np.sin(ang[:, d % 16])
        if d < 16:
            sinf[ch, L0:L0 + L] = -s
            rot[ch + 16, ch] = 1.0
        else:
            sinf[ch, L0:L0 + L] = s
            rot[ch - 16, ch] = 1.0
    c["rope_cos"] = cosf
    c["rope_sin"] = sinf
    c["rot_b"] = _bf(rot)
    for tag, Lx in (("l", L), ("c", LC)):
        N = 2 * Lx
        tt = np.arange(Lx, dtype=np.float64)
        ff = np.arange(Lx, dtype=np.float64) + 0.5
        th = 2 * np.pi * np.outer(tt, ff) / N
        nch = Lx // 128
        CF = np.cos(th)
        SF = np.sin(th)
        def fw(M):
            return _bf(M.reshape(nch, 128, nch, 128).transpose(2, 1, 0, 3).copy())
        c["cf_" + tag] = fw(CF)
        c["sf_" + tag] = fw(SF)
        def iv(M):
            return _bf(M.T.reshape(nch, 128, Lx).transpose(1, 0, 2).copy())
        c["ci_" + tag] = iv(CF * (2.0 / N))
        c["si_" + tag] = iv(-SF * (2.0 / N))
        tu = tt / max(Lx - 1, 1)
        bands = np.linspace(1e-4, 15, 16)
        a2 = (2.0 * math.pi / Lx) * tt[:, None] * bands[None, :]
        feats = np.concatenate([tu[:, None], np.cos(a2), -np.sin(a2)], -1)
        c["feats_" + tag] = feats.T.astype(np.float32).copy()
        c["ntu_" + tag] = (-tu).reshape(nch, 128).T.astype(np.float32).copy()
    io = np.zeros((128, 16), np.float32)
    for kc in range(8):
        io[:, kc] = kc * 128 + np.arange(128)
    for ec in range(4):
        io[:, 8 + ec] = ec * 128 + np.arange(128)
    c["iota_w"] = io
    c["iota_p"] = np.arange(128, dtype=np.float32).reshape(128, 1)
    c["iota_e"] = np.tile(np.arange(NE, dtype=np.float32)[None], (128, 1))
    c["zeros_i"] = np.zeros((128, 512), np.int32)
    tk = np.zeros((128, NB_ * 18), np.int32)
    for j in range(NB_):
        for ti in range(18):
            r0 = j * TOK + (ti * 128 if ti < 2 else LC + (ti - 2) * 128)
            tk[:, j * 18 + ti] = r0 + np.arange(128)
    c["tokid"] = tk
    c["blkS"] = np.tile((np.arange(80, dtype=np.float32) * MOE_S)[None], (128, 1))
    return c


def layout_inputs(inp, core, nseq=NB_):
    f = lambda a: np.ascontiguousarray(np.asarray(a, np.float32))
    b0 = core * NB_
    m = {}
    m["x"] = f(inp["x"][b0:b0 + nseq])
    m["ctx"] = f(inp["ctx"][b0:b0 + nseq])
    cv = np.concatenate([np.asarray(inp["c"], np.float32)[b0:b0 + NB_], np.asarray(inp["c_ctx"], np.float32)[None]], 0)
    m["cvT"] = f(cv.reshape(5, 8, 128).transpose(2, 1, 0))
    for n in ("w_ada", "b_ada", "g_mix", "g_ffn", "w_in", "w_out", "c_w_pw", "d_w_f1", "d_w_f2", "d_w_f3", "d_decay",
              "b_lq1", "b_lk1", "b_lq2", "b_lk2", "moe_w_gate", "moe_w_up", "moe_w_down"):
        m[n] = f(inp[n])
    m["g_final"] = f(np.asarray(inp["g_final"]).reshape(1, D))
    vA = np.zeros((DEPTH, 128, 32), np.float32)
    for d in range(2):
        for c in range(2):
            o = (d * 2 + c) * 8
            sl = slice(c * 128, c * 128 + 128)
            vA[:, :, o:o + 4] = np.asarray(inp["a_conv_w"])[:, d, :, sl].transpose(0, 2, 1)
            vA[:, :, o + 4] = np.asarray(inp["a_conv_b"])[:, d, sl]
            vA[:, :, o + 5] = np.asarray(inp["a_b_r"])[:, d, sl]
            vA[:, :, o + 6] = np.asarray(inp["a_b_i"])[:, d, sl]
            vA[:, :, o + 7] = np.asarray(inp["a_lam"])[:, d, sl]
    m["vecA"] = vA
    wb = np.zeros((DEPTH, 128, 8, 128), np.float32)
    for d in range(2):
        for g, nme in enumerate(("a_w_r", "a_w_i")):
            w = np.asarray(inp[nme])
            for c in range(2):
                i = (d * 2 + g) * 2 + c
                wb[:, 0:64, i, 0:64] = w[:, d, 2 * c]
                wb[:, 64:128, i, 64:128] = w[:, d, 2 * c + 1]
    m["wblkA"] = wb
    m["vecB"] = f(np.tile(np.asarray(inp["b_sub_g"]), (1, 2)).reshape(DEPTH, 128, 1))
    vC = np.zeros((DEPTH, 128, 2, 35), np.float32)
    for c in range(2):
        sl = slice(c * 128, c * 128 + 128)
        vC[:, :, c, 0:31] = np.asarray(inp["c_conv_w"])[:, :, sl].transpose(0, 2, 1)
        vC[:, :, c, 31] = np.asarray(inp["c_conv_b"])[:, sl]
        vC[:, :, c, 32] = np.asarray(inp["c_ln_g"])[:, sl]
        vC[:, :, c, 33] = np.asarray(inp["c_ln_b"])[:, sl]
        vC[:, :, c, 34] = np.asarray(inp["c_b_pw"])[:, sl]
    m["vecC"] = vC
    vD = np.zeros((DEPTH, 128, 6, 4), np.float32)
    for c in range(6):
        sl = slice(c * 128, c * 128 + 128)
        vD[:, :, c, 0:3] = np.asarray(inp["d_conv_w"])[:, :, sl].transpose(0, 2, 1)
        vD[:, :, c, 3] = np.asarray(inp["d_conv_b"])[:, sl]
    m["vecD"] = vD
    m["vecD2"] = f(np.asarray(inp["d_bias"]).reshape(DEPTH, 2, 2, 128).transpose(0, 3, 1, 2).reshape(DEPTH, 128, 4))
    m["hyv"] = f(np.stack([np.asarray(inp["d_b_f1"]), np.asarray(inp["d_freq"]), np.asarray(inp["d_b_f2"])], -1))
    m["wr"] = f(np.concatenate([np.asarray(inp["moe_w_rg"]), np.asarray(inp["moe_w_re"])], -1))
    m["br"] = f(np.concatenate([np.asarray(inp["moe_b_rg"]), np.asarray(inp["moe_b_re"])], -1))
    return m


DT_OF = {np.dtype("float32"): F32, np.dtype("int32"): I32, np.dtype(ml_dtypes.bfloat16): BF16}


class Prog:
    pass


def build(in_map, consts, nseq=NB_, layers=DEPTH, dbg=None, stop_after=None):
    nc = bass.Bass("TRN2", target_bir_lowering=False)
    T = Prog()
    T.din = {}
    for n, a in list(in_map.items()) + list(consts.items()):
        T.din[n] = nc.dram_tensor(n, list(a.shape), DT_OF[a.dtype], kind="ExternalInput")
    out_d = nc.dram_tensor("out", [nseq, L, D], F32, kind="ExternalOutput")
    es = ExitStack()
    k = K(nc, es)
    T.k = k
    dbg = dbg or {}
    dbg_t = {}

    def dbg_out(name, shape, dt=F32):
        dbg_t[name] = nc.dram_tensor("dbg_" + name, list(shape), dt, kind="ExternalOutput")
        return dbg_t[name]

    IN = lambda n: T.din[n].ap()
    ntok = nseq * TOK
    xres = k.dram("xres", [ntok, D], F32)
    hrow = k.dram("hrow", [ntok, D], BF16)
    modD = k.dram("modD", [DEPTH, 5, 6 * D], F32)
    NSLOT = ((2 * ntok + MOE_S - 1) // MOE_S + NE) * MOE_S
    yslot = k.dram("yslot", [NSLOT, D], F32)
    slot_tok = k.dram("slot_tok", [NSLOT, 1], I32)
    xs = k.dram("xs", [NSLOT, D], BF16)
    xs_z = Buf(xs.t)
    Hd = {"l": k.dram("Hd_l", [2, L, 512], F32), "c": k.dram("Hd_c", [2, LC, 512], F32)}
    hfil = {"l": k.dram("hfil_l", [L, 1024], F32), "c": k.dram("hfil_c", [LC, 1024], F32)}

    ident_b = k.sb("ident_b", [128, 128], BF16)
    ident_f = k.sb("ident_f", [128, 128], F32)
    ones_f = k.sb("ones_f", [128, 128], F32)
    blk64 = k.sb("blk64", [128, 128], F32)
    eps6 = k.sb("eps6", [128, 1], F32)
    eps5 = k.sb("eps5", [128, 1], F32)
    one1 = k.sb("one1", [128, 1], F32)
    for b_, n_ in ((ident_b, "ident_b"), (ident_f, "ident_f"), (ones_f, "ones_f"), (blk64, "blk64_f")):
        k.dma("sp", b_[:], IN(n_), writes=[b_])
    k.op("dve", lambda v: v.memset(eps6[:], 1e-6), writes=[eps6])
    k.op("dve", lambda v: v.memset(eps5[:], 1e-5), writes=[eps5])
    k.op("dve", lambda v: v.memset(one1[:], 1.0), writes=[one1])
    hT = k.sb("hT", [128, 8, PW], BF16)
    yT = k.sb("yT", [128, 8, PW], BF16)
    k.op("pool", lambda g: g.memset(hT[:], 0.0), writes=[hT])
    k.op("pool", lambda g: g.memset(yT[:], 0.0), writes=[yT])

    def rot(lst, st=[0]):
        st[0] += 1
        return lst[st[0] % len(lst)]

    xres_b = {(j, ti): Buf(xres.t) for j in range(nseq) for ti in range(18)}
    for j in range(nseq):
        k.dma("sp", xres.t.ap()[j * TOK:j * TOK + LC, :], IN("ctx")[j], writes=[xres_b[(j, 0)], xres_b[(j, 1)]])
        k.dma("sp", xres.t.ap()[j * TOK + LC:(j + 1) * TOK, :], IN("x")[j], writes=[xres_b[(j, ti)] for ti in range(2, 18)])

    def prologue():
        with ExitStack() as e2:
            cv = k.sb("cv", [128, 8, 5], F32, e2)
            sT = k.sb("sT", [128, 8, 5], BF16, e2)
            k.dma("sp", cv[:], IN("cvT"), writes=[cv])
            k.op("act", lambda a: a.activation(out=sT[:], in_=cv[:], func=AF.Silu), reads=[cv], writes=[sT])
            was = [k.sb("wa", [128, 8, 512], BF16, e2) for _ in range(2)]
            pms = [k.ps("pm", [128, 512], F32, e2) for _ in range(2)]
            bada = k.sb("bada", [5, 6 * D], F32, e2)
            modsb = k.sb("modsb", [5, 6 * D], F32, e2)
            for l in range(layers):
                k.dma("sp", bada[:], IN("b_ada")[l:l + 1, :].to_broadcast([5, 6 * D]), writes=[bada])
                for ct in range(12):
                    wa = was[ct % 2]
                    pm = pms[ct % 2]
                    k.dma("pool", wa[:], IN("w_ada")[l][:, ct * 512:(ct + 1) * 512].rearrange("(kc p) n -> p kc n", p=128), writes=[wa])

                    def f(pe, wa=wa, pm=pm):
                        for kc in range(8):
                            ins = pe.matmul(pm[0:5, :], sT[:, kc, :], wa[:, kc, :], start=(kc == 0), stop=(kc == 7))
                        return ins
                    k.op("pe", f, reads=[sT, wa], writes=[pm])
                    k.op("dve", lambda v, pm=pm, ct=ct: v.tensor_tensor(out=modsb[0:5, ct * 512:(ct + 1) * 512], in0=pm[0:5, :],
                                                                     in1=bada[0:5, ct * 512:(ct + 1) * 512], op=ALU.add),
                         reads=[pm, bada], writes=[modsb])
                k.dma("sp", modD.t.ap()[l], modsb[:], reads=[modsb], writes=[modD])
            k.barrier()

    prologue()
    if "mod" in dbg:
        o = dbg_out("mod", [DEPTH, 5, 6 * D])
        k.dma("sp", o.ap(), modD.t.ap(), reads=[modD])

    def mod_bc(dst, l, r, m_):
        k.dma("sp", dst[:], modD.t.ap()[l, r:r + 1, m_ * D:(m_ + 1) * D].to_broadcast([128, D]), reads=[modD], writes=[dst])

    def norm_mod(xt, A1, A0, tmp, hout, st, heng="pool"):
        ss, sd, rs = st
        k.op("act", lambda a: a.activation(out=tmp[:], in_=xt[:], func=AF.Square, accum_out=ss[:, 0:1]), reads=[xt], writes=[tmp, ss])
        k.op("act", lambda a: a.activation(out=sd[:], in_=ss[:], func=AF.Sqrt, scale=1.0 / D, bias=eps6[:, 0:1]), reads=[ss, eps6], writes=[sd])
        k.op("dve", lambda v: v.reciprocal(out=rs[:], in_=sd[:]), reads=[sd], writes=[rs])
        k.op("dve", lambda v: v.scalar_tensor_tensor(out=tmp[:], in0=xt[:], scalar=rs[:, 0:1], in1=A1[:], op0=ALU.mult, op1=ALU.mult),
             reads=[xt, rs, A1], writes=[tmp])
        k.op(heng, lambda g: g.tensor_tensor(out=hout[:], in0=tmp[:], in1=A0[:], op=ALU.add), reads=[tmp, A0], writes=[hout])

    def load_mods(e2, l, r, mi_scale, mi_shift, gname):
        A1 = k.sb("A1", [128, D], F32, e2)
        A0 = k.sb("A0", [128, D], F32, e2)
        gt = k.sb("gt", [128, D], F32, e2)
        mod_bc(A1, l, r, mi_scale)
        mod_bc(A0, l, r, mi_shift)
        k.dma("sp", gt[:], IN(gname)[l:l + 1, :].to_broadcast([128, D]), writes=[gt])
        k.op("dve", lambda v: v.scalar_tensor_tensor(out=A1[:], in0=A1[:], scalar=1.0, in1=gt[:], op0=ALU.add, op1=ALU.mult),
             reads=[A1, gt], writes=[A1])
        return A1, A0

    def tok_row(j, ti):
        col, isctx = TT[ti]
        return j * TOK + (ti * 128 if isctx else LC + (ti - 2) * 128)

    def stage1(l, j):
        with ExitStack() as e2:
            A1, A0 = load_mods(e2, l, j, 1, 0, "g_mix")
            A1c, A0c = load_mods(e2, l, 4, 1, 0, "g_mix")
            xts = [k.sb("xt", [128, D], F32, e2) for _ in range(2)]
            hbs = [k.sb("hb", [128, D], BF16, e2) for _ in range(2)]
            tmp = k.sb("tmp", [128, D], F32, e2)
            sts = [[k.sb("st", [128, 1], F32, e2) for _ in range(3)] for _ in range(2)]
            psts = [k.ps("pst", [128, 8, 128], BF16, e2) for _ in range(2)]
            for ti, (col, isctx) in enumerate(TT):
                xt, hb, st, pst = xts[ti % 2], hbs[ti % 2], sts[ti % 2], psts[ti % 2]
                r0 = tok_row(j, ti)
                k.dma("sp", xt[:], xres.t.ap()[r0:r0 + 128, :], reads=[xres_b[(j, ti)]], writes=[xt])
                norm_mod(xt, A1c if isctx else A1, A0c if isctx else A0, tmp, hb, st)

                def f(pe, hb=hb, pst=pst):
                    for kc in range(8):
                        ins = pe.transpose(pst[:, kc, :], hb[:, kc * 128:(kc + 1) * 128], ident_b[:, :])
                    return ins
                k.op("pe", f, reads=[hb, ident_b], writes=[pst])
                k.op("act", lambda a, pst=pst, col=col: a.copy(hT[:, :, col:col + 128], pst[:, :, :]), reads=[pst], writes=[hT])
            k.barrier()

    def proj(wg, wcol0, pp, evac):
        for (c0, n) in FULL_TILES:
            ps = rot(pp)

            def f(pe, ps=ps, c0=c0, n=n):
                for kc in range(8):
                    ins = pe.matmul(ps[:, 0:n], wg[:, kc, wcol0:wcol0 + 128], hT[:, kc, c0:c0 + n], start=(kc == 0), stop=(kc == 7))
                return ins
            k.op("pe", f, reads=[wg, hT], writes=[ps])
            evac(ps, c0, n)

    def load_w_in(e2, l, c_lo, c_n, nm="wg"):
        wg = k.sb(nm, [128, 8, c_n], BF16, e2)
        k.dma("pool", wg[:], IN("w_in")[l][:, c_lo:c_lo + c_n].rearrange("(kc p) n -> p kc n", p=128), writes=[wg])
        return wg

    def mixer_a(l, j):
        with ExitStack() as e2:
            wg = load_w_in(e2, l, 0, 512)
            pp = [k.ps("ppa", [128, 512], F32, e2) for _ in range(4)]
            pq = [k.ps("pqa", [128, 512], F32, e2) for _ in range(4)]
            xr = [k.sb("xr", [128, PW], BF16, e2) for _ in range(2)]
            xg = [k.sb("xg", [128, PW], F32, e2) for _ in range(2)]
            hsum = [k.sb("hsum", [128, PW], F32, e2) for _ in range(2)]
            hrev = k.sb("hrev", [128, PW], F32, e2)
            vA = k.sb("vA", [128, 32], F32, e2)
            k.dma("sp", vA[:], IN("vecA")[l], writes=[vA])
            wblk = k.sb("wblk", [128, 8, 128], BF16, e2)
            k.dma("pool", wblk[:], IN("wblkA")[l], writes=[wblk])
            lam4 = k.sb("lam4", [128, 4], F32, e2)
            c1 = k.sb("c1", [128, 4], F32, e2)
            c2 = k.sb("c2", [128, 4], F32, e2)
            k.op("act", lambda a: a.activation(out=lam4[:], in_=vA[:, 7::8], func=AF.Exp, scale=-1.0), reads=[vA], writes=[lam4])
            k.op("act", lambda a: a.activation(out=lam4[:], in_=lam4[:], func=AF.Ln, bias=one1[:, 0:1]), reads=[lam4, one1], writes=[lam4])
            k.op("dve", lambda v: v.tensor_scalar(out=c1[:], in0=lam4[:], scalar1=-8.0, scalar2=None, op0=ALU.mult), reads=[lam4], writes=[c1])
            k.op("dve", lambda v: v.tensor_scalar(out=c2[:], in0=lam4[:], scalar1=-16.0, scalar2=None, op0=ALU.mult), reads=[lam4], writes=[c2])
            if os.environ.get('KSTOP') == '0a':
                k.barrier(); return
            dg = k.sb("dgA", [128, 16, 128], BF16, e2)
            for dc in range(4):
                for kk in range(4):
                    k.op("dve", lambda v, dc=dc, kk=kk: v.tensor_scalar(out=dg[:, dc * 4 + kk, :], in0=ident_f[:], scalar1=vA[:, dc * 8 + kk:dc * 8 + kk + 1],
                                                                        scalar2=None, op0=ALU.mult), reads=[ident_f, vA], writes=[dg])
            if os.environ.get('KSTOP') == '0b':
                k.barrier(); return
            for oc in range(4):
                if oc < 2:
                    proj(wg, oc * 128, pp, lambda ps, c0, n, oc=oc: k.op("act", lambda a: a.copy(xr[oc][:, c0:c0 + n], ps[:, 0:n]), reads=[ps], writes=[xr[oc]]))
                else:
                    proj(wg, oc * 128, pp, lambda ps, c0, n, oc=oc: k.op("dve", lambda v: v.tensor_copy(xg[oc - 2][:, c0:c0 + n], ps[:, 0:n]), reads=[ps], writes=[xg[oc - 2]]))
            if os.environ.get('KSTOP') == '1':
                k.barrier(); return
            tl = [k.sb("tA%d" % i, [128, 512], F32, e2) for i in range(12)]
            ubs = [k.sb("ubA", [128, 512], BF16, e2) for _ in range(2)]
            for c in range(2):
                for d in range(2):
                    dc = d * 2 + c
                    hb = hsum[c] if d == 0 else hrev
                    order = [0, 1, 2, 3, 4] if d == 0 else [0, 4, 3, 2, 1]
                    for oi, vi in enumerate(order):
                        c0, n = VT[vi]
                        half = (oi % 2) * 6
                        uf, r_, i_, a_, e_, iu = tl[half:half + 6]
                        ub = ubs[oi % 2]
                        pu = rot(pq)

                        def fconv(pe, pu=pu, c0=c0, n=n, dc=dc, d=d, c=c):
                            for kk in range(4):
                                off = kk - 3 if d == 0 else kk
                                ins = pe.matmul(pu[:, 0:n], dg[:, dc * 4 + kk, :], xr[c][:, c0 + off:c0 + off + n], start=(kk == 0), stop=(kk == 3))
                            return ins
                        k.op("pe", fconv, reads=[dg, xr[c]], writes=[pu])
                        if os.environ.get('KSTOP') == '15':
                            k.barrier(); return
                        cb = vA[:, dc * 8 + 4:dc * 8 + 5]
                        k.op("act", lambda a, pu=pu, n=n, uf=uf, cb=cb: a.activation(out=uf[:, 0:n], in_=pu[:, 0:n], func=AF.Identity, bias=cb), reads=[pu, vA], writes=[uf])
                        if os.environ.get('KSTOP') == '16':
                            k.barrier(); return
                        k.op("dve", lambda v, uf=uf, n=n, ub=ub: v.tensor_copy(ub[:, 0:n], uf[:, 0:n]), reads=[uf], writes=[ub])
                        if os.environ.get('KSTOP') == '2':
                            k.barrier(); return
                        pr = rot(pq)
                        pi = rot(pq)
                        k.op("pe", lambda pe, pr=pr, n=n, ub=ub, d=d, c=c: pe.matmul(pr[:, 0:n], wblk[:, (d * 2 + 0) * 2 + c, :], ub[:, 0:n], start=True, stop=True), reads=[wblk, ub], writes=[pr])
                        k.op("pe", lambda pe, pi=pi, n=n, ub=ub, d=d, c=c: pe.matmul(pi[:, 0:n], wblk[:, (d * 2 + 1) * 2 + c, :], ub[:, 0:n], start=True, stop=True), reads=[wblk, ub], writes=[pi])
                        k.op("act", lambda a, pr=pr, n=n, r_=r_, dc=dc: a.activation(out=r_[:, 0:n], in_=pr[:, 0:n], func=AF.Sigmoid, bias=vA[:, dc * 8 + 5:dc * 8 + 6]), reads=[pr, vA], writes=[r_])
                        k.op("act", lambda a, pi=pi, n=n, i_=i_, dc=dc: a.activation(out=i_[:, 0:n], in_=pi[:, 0:n], func=AF.Sigmoid, bias=vA[:, dc * 8 + 6:dc * 8 + 7]), reads=[pi, vA], writes=[i_])
                        k.op("act", lambda a, n=n, r_=r_, a_=a_, dc=dc: a.activation(out=a_[:, 0:n], in_=r_[:, 0:n], func=AF.Exp, scale=c1[:, dc:dc + 1]), reads=[r_, c1], writes=[a_])
                        k.op("act", lambda a, n=n, r_=r_, e_=e_, dc=dc: a.activation(out=e_[:, 0:n], in_=r_[:, 0:n], func=AF.Exp, scale=c2[:, dc:dc + 1]), reads=[r_, c2], writes=[e_])
                        k.op("dve", lambda v, n=n, e_=e_: v.tensor_scalar(out=e_[:, 0:n], in0=e_[:, 0:n], scalar1=1.0, scalar2=-1.0, op0=ALU.min, op1=ALU.mult), reads=[e_], writes=[e_])
                        k.op("act", lambda a, n=n, e_=e_: a.activation(out=e_[:, 0:n], in_=e_[:, 0:n], func=AF.Sqrt, bias=one1[:, 0:1]), reads=[e_, one1], writes=[e_])
                        k.op("pool", lambda g, n=n, i_=i_, uf=uf, iu=iu: g.tensor_tensor(out=iu[:, 0:n], in0=i_[:, 0:n], in1=uf[:, 0:n], op=ALU.mult), reads=[i_, uf], writes=[iu])
                        k.op("dve", lambda v, n=n, e_=e_, iu=iu: v.tensor_tensor(out=iu[:, 0:n], in0=iu[:, 0:n], in1=e_[:, 0:n], op=ALU.mult), reads=[iu, e_], writes=[iu])
                        if os.environ.get('KSTOP') == '3':
                            k.barrier(); return
                        if vi == 0:
                            init = 0.0
                        elif d == 0:
                            init = hb[:, c0 - 1:c0] if vi > 1 else hb[:, C0 + LC - 1:C0 + LC]
                        else:
                            init = hb[:, c0 + n:c0 + n + 1] if vi < 4 else hb[:, C0:C0 + 1]
                        if d == 0:
                            k.op("dve", lambda v, n=n, c0=c0, a_=a_, iu=iu, hb=hb, init=init: v.tensor_tensor_scan(
                                out=hb[:, c0:c0 + n], data0=a_[:, 0:n], data1=iu[:, 0:n], initial=init, op0=ALU.mult, op1=ALU.add), reads=[a_, iu, hb], writes=[hb])
                        else:
                            k.op("dve", lambda v, n=n, c0=c0, a_=a_, iu=iu, hb=hb, init=init: v.tensor_tensor_scan(
                                out=hb[:, c0:c0 + n][:, ::-1], data0=a_[:, 0:n][:, ::-1], data1=iu[:, 0:n][:, ::-1], initial=init, op0=ALU.mult, op1=ALU.add),
                                reads=[a_, iu, hb], writes=[hb])
                if os.environ.get('KSTOP') == '4':
                    k.barrier(); return
                for (c0, n) in VT:
                    k.op("act", lambda a, c=c, c0=c0, n=n: a.activation(out=xg[c][:, c0:c0 + n], in_=xg[c][:, c0:c0 + n], func=AF.Gelu_apprx_tanh), reads=[xg[c]], writes=[xg[c]])
                    k.op("pool", lambda g, c=c, c0=c0, n=n: g.tensor_tensor(out=hsum[c][:, c0:c0 + n], in0=hsum[c][:, c0:c0 + n], in1=hrev[:, c0:c0 + n], op=ALU.add), reads=[hsum[c], hrev], writes=[hsum[c]])
                    k.op("dve", lambda v, c=c, c0=c0, n=n: v.tensor_tensor(out=yT[:, c, c0:c0 + n], in0=hsum[c][:, c0:c0 + n], in1=xg[c][:, c0:c0 + n], op=ALU.mult), reads=[hsum[c], xg[c]], writes=[yT])
            k.barrier()
    T.stage1, T.mixer_a = stage1, mixer_a

    def mixer_c(l, j):
        with ExitStack() as e2:
            wg = load_w_in(e2, l, 1280, 512)
            pp = [k.ps("ppc", [128, 512], F32, e2) for _ in range(8)]
            vC = k.sb("vC", [128, 2, 35], F32, e2)
            k.dma("sp", vC[:], IN("vecC")[l], writes=[vC])
            wpw = k.sb("wpw", [128, 2, 256], BF16, e2)
            k.dma("pool", wpw[:], IN("c_w_pw")[l].rearrange("(ic p) j -> p ic j", p=128), writes=[wpw])
            dg = k.sb("dgC", [128, 62, 128], BF16, e2)
            for c in range(2):
                for kk in range(31):
                    k.op("dve", lambda v, c=c, kk=kk: v.tensor_scalar(out=dg[:, c * 31 + kk, :], in0=ident_f[:], scalar1=vC[:, c, kk:kk + 1], scalar2=None, op0=ALU.mult),
                         reads=[ident_f, vC], writes=[dg])
            ub = [k.sb("ubC", [128, PW], BF16, e2) for _ in range(2)]
            sgt = [k.sb("sgt", [128, 512], F32, e2) for _ in range(2)]
            for c in range(2):
                for ti, (c0, n) in enumerate(FULL_TILES):
                    pv, pg = rot(pp), rot(pp)

                    def f(pe, pv=pv, pg=pg, c0=c0, n=n, c=c):
                        for kc in range(8):
                            pe.matmul(pv[:, 0:n], wg[:, kc, c * 128:c * 128 + 128], hT[:, kc, c0:c0 + n], start=(kc == 0), stop=(kc == 7))
                        for kc in range(8):
                            ins = pe.matmul(pg[:, 0:n], wg[:, kc, 256 + c * 128:256 + c * 128 + 128], hT[:, kc, c0:c0 + n], start=(kc == 0), stop=(kc == 7))
                        return ins
                    k.op("pe", f, reads=[wg, hT], writes=[pv, pg])
                    sg = sgt[ti % 2]
                    k.op("act", lambda a, pg=pg, n=n, sg=sg: a.activation(out=sg[:, 0:n], in_=pg[:, 0:n], func=AF.Sigmoid), reads=[pg], writes=[sg])
                    k.op("dve", lambda v, pv=pv, n=n, sg=sg, c=c, c0=c0: v.tensor_tensor(out=ub[c][:, c0:c0 + n], in0=pv[:, 0:n], in1=sg[:, 0:n], op=ALU.mult), reads=[pv, sg], writes=[ub[c]])
            tl = [k.sb("tC%d" % i, [128, 512], F32, e2) for i in range(10)]
            slb = [k.sb("slC", [128, 512], BF16, e2) for _ in range(2)]
            for (c0, n) in VT:
                cv = tl[0:2]
                sq = tl[2:4]
                mean, msq, var, t1 = tl[4:8]
                for c in range(2):
                    pc = rot(pp)

                    def f(pe, pc=pc, c0=c0, n=n, c=c):
                        for kk in range(31):
                            ins = pe.matmul(pc[:, 0:n], dg[:, c * 31 + kk, :], ub[c][:, c0 + kk - 15:c0 + kk - 15 + n], start=(kk == 0), stop=(kk == 30))
                        return ins
                    k.op("pe", f, reads=[dg, ub[c]], writes=[pc])
                    k.op("act", lambda a, pc=pc, n=n, c=c: a.activation(out=cv[c][:, 0:n], in_=pc[:, 0:n], func=AF.Identity, bias=vC[:, c, 31:32]), reads=[pc, vC], writes=[cv[c]])
                    k.op("pool", lambda g, n=n, c=c: g.tensor_tensor(out=sq[c][:, 0:n], in0=cv[c][:, 0:n], in1=cv[c][:, 0:n], op=ALU.mult), reads=[cv[c]], writes=[sq[c]])
                p1, p2 = rot(pp), rot(pp)

                def f(pe, p1=p1, p2=p2, n=n):
                    pe.matmul(p1[:, 0:n], ones_f[:, :], cv[0][:, 0:n], start=True, stop=False)
                    pe.matmul(p1[:, 0:n], ones_f[:, :], cv[1][:, 0:n], start=False, stop=True)
                    pe.matmul(p2[:, 0:n], ones_f[:, :], sq[0][:, 0:n], start=True, stop=False)
                    return pe.matmul(p2[:, 0:n], ones_f[:, :], sq[1][:, 0:n], start=False, stop=True)
                k.op("pe", f, reads=[ones_f, cv[0], cv[1], sq[0], sq[1]], writes=[p1, p2])
                k.op("act", lambda a, p1=p1, n=n: a.activation(out=mean[:, 0:n], in_=p1[:, 0:n], func=AF.Copy, scale=1.0 / 256), reads=[p1], writes=[mean])
                k.op("pool", lambda g, n=n: g.tensor_tensor(out=msq[:, 0:n], in0=mean[:, 0:n], in1=mean[:, 0:n], op=ALU.mult), reads=[mean], writes=[msq])
                k.op("dve", lambda v, p2=p2, n=n: v.scalar_tensor_tensor(out=var[:, 0:n], in0=p2[:, 0:n], scalar=1.0 / 256, in1=msq[:, 0:n], op0=ALU.mult, op1=ALU.subtract), reads=[p2, msq], writes=[var])
                k.op("act", lambda a, n=n: a.activation(out=var[:, 0:n], in_=var[:, 0:n], func=AF.Sqrt, bias=eps5[:, 0:1]), reads=[var, eps5], writes=[var])
                k.op("dve", lambda v, n=n: v.reciprocal(out=var[:, 0:n], in_=var[:, 0:n]), reads=[var], writes=[var])
                for c in range(2):
                    k.op("dve", lambda v, n=n, c=c: v.tensor_tensor(out=t1[:, 0:n], in0=cv[c][:, 0:n], in1=mean[:, 0:n], op=ALU.subtract), reads=[cv[c], mean], writes=[t1])
                    k.op("pool", lambda g, n=n: g.tensor_tensor(out=t1[:, 0:n], in0=t1[:, 0:n], in1=var[:, 0:n], op=ALU.mult), reads=[t1, var], writes=[t1])
                    k.op("act", lambda a, n=n, c=c: a.activation(out=slb[c][:, 0:n], in_=t1[:, 0:n], func=AF.Silu, scale=vC[:, c, 32:33], bias=vC[:, c, 33:34]), reads=[t1, vC], writes=[slb[c]])
                for jc in range(2):
                    po = rot(pp)

                    def f(pe, po=po, n=n, jc=jc):
                        pe.matmul(po[:, 0:n], wpw[:, 0, jc * 128:jc * 128 + 128], slb[0][:, 0:n], start=True, stop=False)
                        return pe.matmul(po[:, 0:n], wpw[:, 1, jc * 128:jc * 128 + 128], slb[1][:, 0:n], start=False, stop=True)
                    k.op("pe", f, reads=[wpw, slb[0], slb[1]], writes=[po])
                    k.op("act", lambda a, po=po, n=n, jc=jc, c0=c0: a.activation(out=yT[:, 4 + jc, c0:c0 + n], in_=po[:, 0:n], func=AF.Identity, bias=vC[:, jc, 34:35]), reads=[po, vC], writes=[yT])
            k.barrier()

    def wout_phase(l, j, last):
        with ExitStack() as e2:
            wo = k.sb("wo", [128, 8, D], BF16, e2)
            k.dma("pool", wo[:], IN("w_out")[l].rearrange("(kc p) n -> p kc n", p=128), writes=[wo])
            A2 = k.sb("A2", [128, D], F32, e2)
            A2c = k.sb("A2c", [128, D], F32, e2)
            mod_bc(A2, l, j, 2)
            mod_bc(A2c, l, 4, 2)
            xts = [k.sb("xtw", [128, D], F32, e2) for _ in range(2)]
            tws = [k.sb("tw", [128, D], F32, e2) for _ in range(2)]
            pps = [k.ps("ppw", [128, D], F32, e2) for _ in range(2)]
            for ti, (col, isctx) in enumerate(TT):
                if last and isctx:
                    continue
                xt, tw, pw = xts[ti % 2], tws[ti % 2], pps[ti % 2]
                r0 = tok_row(j, ti)
                k.dma("sp", xt[:], xres.t.ap()[r0:r0 + 128, :], reads=[xres_b[(j, ti)]], writes=[xt])

                def f(pe, pw=pw, col=col):
                    for nh in range(2):
                        for kc in range(8):
                            ins = pe.matmul(pw[:, nh * 512:(nh + 1) * 512], yT[:, kc, col:col + 128], wo[:, kc, nh * 512:(nh + 1) * 512], start=(kc == 0), stop=(kc == 7))
                    return ins
                k.op("pe", f, reads=[yT, wo], writes=[pw])
                Ax = A2c if isctx else A2
                k.op("dve", lambda v, pw=pw, tw=tw, Ax=Ax: v.tensor_tensor(out=tw[:], in0=pw[:, :], in1=Ax[:], op=ALU.mult), reads=[pw, Ax], writes=[tw])
                k.op("pool", lambda g, tw=tw, xt=xt: g.tensor_tensor(out=tw[:], in0=tw[:], in1=xt[:], op=ALU.add), reads=[tw, xt], writes=[tw])
                k.dma("sp", xres.t.ap()[r0:r0 + 128, :], tw[:], reads=[tw], writes=[xres_b[(j, ti)]])
            k.barrier()

    def mixer_b(l, j):
        lam_init = 0.8 - 0.6 * math.exp(-0.3 * l)
        scale = 32.0 ** -0.5
        with ExitStack() as e2:
            qk = [k.sb("qk", [128, PW], BF16, e2) for _ in range(4)]
            va = k.sb("va", [128, 18, 4, 128], BF16, e2)
            k.op("pool", lambda g: g.memset(va[:], 1.0), writes=[va])
            neglam = k.sb("neglam", [128, 1], F32, e2)
            sgv = k.sb("sgv", [128, 1], F32, e2)
            with ExitStack() as e3:
                wg = load_w_in(e3, l, 512, 768)
                rotb = k.sb("rotb", [128, 128], BF16, e3)
                k.dma("sp", rotb[:], IN("rot_b"), writes=[rotb])
                cosT = k.sb("cosT", [128, PW], F32, e3)
                sinT = k.sb("sinT", [128, PW], F32, e3)
                k.dma("sp", cosT[:], IN("rope_cos"), writes=[cosT])
                k.dma("sp", sinT[:], IN("rope_sin"), writes=[sinT])
                pp = [k.ps("ppb", [128, 512], F32, e3) for _ in range(8)]
                lq = k.sb("lq", [128, 4, 32], F32, e3)
                for i_, nme in enumerate(("b_lq1", "b_lk1", "b_lq2", "b_lk2")):
                    k.dma("sp", lq[:, i_, :], IN(nme)[l:l + 1, :].to_broadcast([128, 32]), writes=[lq])
                s12 = k.sb("s12", [128, 2], F32, e3)
                pr_ = k.sb("pr_", [128, 32], F32, e3)
                for i_ in range(2):
                    k.op("dve", lambda v, i_=i_: v.tensor_tensor(out=pr_[:], in0=lq[:, 2 * i_, :], in1=lq[:, 2 * i_ + 1, :], op=ALU.mult), reads=[lq], writes=[pr_])
                    k.op("dve", lambda v, i_=i_: v.reduce_sum(out=s12[:, i_:i_ + 1], in_=pr_[:], axis=AX.X), reads=[pr_], writes=[s12])
                k.op("act", lambda a: a.activation(out=s12[:], in_=s12[:], func=AF.Exp), reads=[s12], writes=[s12])
                k.op("dve", lambda v: v.tensor_tensor(out=neglam[:], in0=s12[:, 1:2], in1=s12[:, 0:1], op=ALU.subtract), reads=[s12], writes=[neglam])
                k.op("dve", lambda v: v.tensor_scalar(out=neglam[:], in0=neglam[:], scalar1=-lam_init, scalar2=None, op0=ALU.add), reads=[neglam], writes=[neglam])
                k.dma("sp", sgv[:], IN("vecB")[l], writes=[sgv])
                k.op("dve", lambda v: v.tensor_scalar(out=sgv[:], in0=sgv[:], scalar1=1.0 - lam_init, scalar2=None, op0=ALU.mult), reads=[sgv], writes=[sgv])
                if os.environ.get('KSTOP') == 'ba':
                    k.barrier(); return
                qbs = [k.sb("qb", [128, 512], BF16, e3) for _ in range(2)]
                t1s = [k.sb("t1b", [128, 512], F32, e3) for _ in range(2)]
                t2s = [k.sb("t2b", [128, 512], F32, e3) for _ in range(2)]
                cnt = 0
                for oc in range(4):
                    for (c0, n) in FULL_TILES:
                        ps, p2 = rot(pp), rot(pp)
                        qb, t1, t2 = qbs[cnt % 2], t1s[cnt % 2], t2s[cnt % 2]
                        cnt += 1

                        def f(pe, ps=ps, c0=c0, n=n, oc=oc):
                            for kc in range(8):
                                ins = pe.matmul(ps[:, 0:n], wg[:, kc, oc * 128:oc * 128 + 128], hT[:, kc, c0:c0 + n], start=(kc == 0), stop=(kc == 7))
                            return ins
                        k.op("pe", f, reads=[wg, hT], writes=[ps])
                        k.op("act", lambda a, ps=ps, n=n, qb=qb: a.copy(qb[:, 0:n], ps[:, 0:n]), reads=[ps], writes=[qb])
                        k.op("dve", lambda v, ps=ps, n=n, t1=t1, c0=c0: v.tensor_tensor(out=t1[:, 0:n], in0=ps[:, 0:n], in1=cosT[:, c0:c0 + n], op=ALU.mult), reads=[ps, cosT, qb], writes=[t1])
                        k.op("pe", lambda pe, p2=p2, n=n, qb=qb: pe.matmul(p2[:, 0:n], rotb[:, :], qb[:, 0:n], start=True, stop=True), reads=[rotb, qb], writes=[p2])
                        k.op("dve", lambda v, p2=p2, n=n, t2=t2, c0=c0: v.tensor_tensor(out=t2[:, 0:n], in0=p2[:, 0:n], in1=sinT[:, c0:c0 + n], op=ALU.mult), reads=[p2, sinT], writes=[t2])
                        k.op("dve" if os.environ.get("KPOOL") else "pool", lambda g, n=n, t1=t1, t2=t2, oc=oc, c0=c0: g.tensor_tensor(out=qk[oc][:, c0:c0 + n], in0=t1[:, 0:n], in1=t2[:, 0:n], op=ALU.add), reads=[t1, t2], writes=[qk[oc]])
                        if os.environ.get('KSTOP') == 'bb':
                            k.barrier(); return
                if os.environ.get('KSTOP') == 'b0':
                    k.barrier(); return
                for ti, (col, isctx) in enumerate(TT):
                    ps = rot(pp)

                    def f(pe, ps=ps, col=col):
                        for kc in range(8):
                            ins = pe.matmul(ps[:, 0:256], hT[:, kc, col:col + 128], wg[:, kc, 512:768], start=(kc == 0), stop=(kc == 7))
                        return ins
                    k.op("pe", f, reads=[wg, hT], writes=[ps])
                    for h in range(4):
                        dst = va[:, ti, h, 0:64] if h % 2 == 0 else va[:, ti, h, 64:128]
                        k.op("dve", lambda v, ps=ps, h=h, dst=dst: v.tensor_copy(dst, ps[:, h * 64:(h + 1) * 64]), reads=[ps], writes=[va])
                k.barrier()
            if os.environ.get('KSTOP') == 'b1':
                return
            with ExitStack() as e3:
                pS = [k.ps("pS", [128, 1024], F32, e3) for _ in range(2)]
                pO = k.ps("pO", [128, 1024], F32, e3)
                pX = [k.ps("pX", [128, 512], F32, e3) for _ in range(2)]
                Eb = [k.sb("Eb", [128, 1024], BF16, e3) for _ in range(2)]
                ob = [k.sb("ob", [128, PW], F32, e3) for _ in range(2)]
                o1 = k.sb("o1", [128, PW], F32, e3)
                rdt = [k.sb("rdt", [128, 512], F32, e3) for _ in range(2)]
                for h in range(4):
                    ch, hl = h // 2, h % 2
                    nr = slice(0, 64) if hl == 0 else slice(64, 128)
                    dr = slice(64, 128) if hl == 0 else slice(0, 64)
                    for c in range(2):
                        base = hl * 64 + c * 32
                        dest = ob[ch] if c == 0 else o1
                        qq, kk_ = qk[ch], qk[2 + ch]
                        segs = [(L0, 1024, 18), (L0 + 1024, 1024, 18), (C0, 256, 2)]
                        for (q0, qn, nkt) in segs:
                            nsub = (qn + 511) // 512
                            def emit_S(kt, q0=q0, qn=qn, nsub=nsub, base=base, qq=qq, kk_=kk_):
                                kcol = TT[kt][0]
                                ps_ = pS[kt % 2]

                                def f(pe):
                                    for i_ in range(nsub):
                                        w_ = min(512, qn - i_ * 512)
                                        ins = pe.matmul(ps_[:, i_ * 512:i_ * 512 + w_], kk_[base:base + 32, kcol:kcol + 128], qq[base:base + 32, q0 + i_ * 512:q0 + i_ * 512 + w_],
                                                        start=True, stop=True, tile_position=(base, 0))
                                    return ins
                                k.op("pe", f, reads=[qq, kk_], writes=[ps_])

                            def emit_E(kt, qn=qn):
                                ps_, E_ = pS[kt % 2], Eb[kt % 2]
                                k.op("act", lambda a: a.activation(out=E_[:, 0:qn], in_=ps_[:, 0:qn], func=AF.Exp, scale=scale), reads=[ps_], writes=[E_])

                            def emit_PV(kt, nkt=nkt, qn=qn, nsub=nsub, h=h):
                                E_ = Eb[kt % 2]

                                def f2(pe):
                                    for i_ in range(nsub):
                                        w_ = min(512, qn - i_ * 512)
                                        ins = pe.matmul(pO[:, i_ * 512:i_ * 512 + w_], va[:, kt, h, :], E_[:, i_ * 512:i_ * 512 + w_], start=(kt == 0), stop=(kt == nkt - 1))
                                    return ins
                                k.op("pe", f2, reads=[va, E_], writes=[pO])
                            emit_S(0)
                            for kt in range(nkt):
                                emit_E(kt)
                                if kt + 1 < nkt:
                                    emit_S(kt + 1)
                                emit_PV(kt)
                            for i_ in range(nsub):
                                w_ = min(512, qn - i_ * 512)
                                rd = rdt[i_ % 2]
                                k.op("dve", lambda v, rd=rd, i_=i_, w_=w_, dr=dr: v.reciprocal(out=rd[dr, 0:w_], in_=pO[dr, i_ * 512:i_ * 512 + w_]), reads=[pO], writes=[rd])
                                k.op("dve", lambda v, rd=rd, i_=i_, w_=w_, dr=dr, nr=nr, dest=dest, q0=q0: v.tensor_tensor(
                                    out=dest[nr, q0 + i_ * 512:q0 + i_ * 512 + w_], in0=pO[nr, i_ * 512:i_ * 512 + w_], in1=rd[dr, 0:w_], op=ALU.mult), reads=[pO, rd], writes=[dest])
                    for (c0, n) in VT:
                        k.op("dve", lambda v, c0=c0, n=n, nr=nr, ch=ch: v.scalar_tensor_tensor(out=ob[ch][nr, c0:c0 + n], in0=o1[nr, c0:c0 + n], scalar=neglam[nr, 0:1], in1=ob[ch][nr, c0:c0 + n],
                                                                                            op0=ALU.mult, op1=ALU.add), reads=[o1, neglam, ob[ch]], writes=[ob[ch]])
                sqs = [k.sb("sqb", [128, 512], F32, e3) for _ in range(2)]
                for ch in range(2):
                    for vi, (c0, n) in enumerate(VT):
                        sq, px = sqs[vi % 2], pX[vi % 2]
                        k.op("pool", lambda g, sq=sq, c0=c0, n=n, ch=ch: g.tensor_tensor(out=sq[:, 0:n], in0=ob[ch][:, c0:c0 + n], in1=ob[ch][:, c0:c0 + n], op=ALU.mult), reads=[ob[ch]], writes=[sq])
                        k.op("pe", lambda pe, sq=sq, px=px, n=n: pe.matmul(px[:, 0:n], blk64[:, :], sq[:, 0:n], start=True, stop=True), reads=[blk64, sq], writes=[px])
                        k.op("act", lambda a, sq=sq, px=px, n=n: a.activation(out=sq[:, 0:n], in_=px[:, 0:n], func=AF.Sqrt, scale=1.0 / 64, bias=eps6[:, 0:1]), reads=[px, eps6], writes=[sq])
                        k.op("dve", lambda v, sq=sq, n=n: v.reciprocal(out=sq[:, 0:n], in_=sq[:, 0:n]), reads=[sq], writes=[sq])
                        k.op("dve", lambda v, sq=sq, c0=c0, n=n, ch=ch: v.scalar_tensor_tensor(out=yT[:, 2 + ch, c0:c0 + n], in0=ob[ch][:, c0:c0 + n], scalar=sgv[:, 0:1], in1=sq[:, 0:n],
                                                                                         op0=ALU.mult, op1=ALU.mult), reads=[ob[ch], sgv, sq], writes=[yT])
                k.barrier()

    SEGS = {"l": (L0, L, 16), "c": (C0, LC, 2)}

    def hyena_prep(l):
        TWO_PI = 2.0 * math.pi
        for tag, (col0, Lx, nch) in SEGS.items():
            with ExitStack() as e2:
                pp = [k.ps("pph", [128, 512], F32, e2) for _ in range(4)]
                pe_ = [k.ps("ppe", [128, 512], F32, e2) for _ in range(2)]
                feats = k.sb("feats", [33, Lx], F32, e2)
                k.dma("sp", feats[:], IN("feats_" + tag), writes=[feats])
                wf1 = k.sb("wf1", [33, 64], F32, e2)
                wf2 = k.sb("wf2", [64, 64], F32, e2)
                wf3 = k.sb("wf3", [64, 1024], F32, e2)
                k.dma("sp", wf1[:], IN("d_w_f1")[l], writes=[wf1])
                k.dma("sp", wf2[:], IN("d_w_f2")[l], writes=[wf2])
                k.dma("sp", wf3[:], IN("d_w_f3")[l], writes=[wf3])
                hv = k.sb("hv", [64, 3], F32, e2)
                k.dma("sp", hv[:], IN("hyv")[l], writes=[hv])
                fb = k.sb("fb", [64, 2], F32, e2)
                k.op("dve", lambda v: v.tensor_tensor(out=fb[:, 0:1], in0=hv[:, 0:1], in1=hv[:, 1:2], op=ALU.mult), reads=[hv], writes=[fb])
                k.op("dve", lambda v: v.tensor_tensor(out=fb[:, 1:2], in0=hv[:, 2:3], in1=hv[:, 1:2], op=ALU.mult), reads=[hv], writes=[fb])
                ntu = k.sb("ntu", [128, nch], F32, e2)
                k.dma("sp", ntu[:], IN("ntu_" + tag), writes=[ntu])
                dec = k.sb("dec", [128, 1024], F32, e2)
                dec2 = k.sb("dec2", [128, 1024], F32, e2)
                k.dma("sp", dec[:], IN("d_decay")[l:l + 1, :].to_broadcast([128, 1024]), writes=[dec])
                k.op("dve", lambda v: v.tensor_scalar(out=dec2[:], in0=dec[:], scalar1=-1.0, scalar2=None, op0=ALU.mult), reads=[dec], writes=[dec2])
                k.op("dve", lambda v: v.tensor_tensor(out=dec[:], in0=dec[:], in1=dec2[:], op=ALU.max), reads=[dec, dec2], writes=[dec])
                f1 = k.sb("f1", [64, Lx], F32, e2)
                f2 = k.sb("f2", [64, Lx], F32, e2)
                arg = k.sb("arg", [64, 512], F32, e2)
                ki = k.sb("ki", [64, 512], I32, e2)
                kf = k.sb("kf", [64, 512], F32, e2)
                nt = min(512, Lx)
                for (src, w_, dst, bcol, K_) in ((feats, wf1, f1, 0, 33), (f1, wf2, f2, 1, 64)):
                    for t0 in range(0, Lx, nt):
                        ps = rot(pp)
                        k.op("pe", lambda pe, ps=ps, src=src, w_=w_, t0=t0, K_=K_: pe.matmul(ps[0:64, 0:nt], w_[0:K_, :], src[0:K_, t0:t0 + nt], start=True, stop=True), reads=[w_, src], writes=[ps])
                        k.op("act", lambda a, ps=ps, bcol=bcol: a.activation(out=arg[:, 0:nt], in_=ps[0:64, 0:nt], func=AF.Identity, scale=hv[:, 1:2], bias=fb[:, bcol:bcol + 1]), reads=[ps, hv, fb], writes=[arg])
                        k.op("dve", lambda v: v.tensor_scalar(out=ki[:, 0:nt], in0=arg[:, 0:nt], scalar1=1.0 / TWO_PI, scalar2=None, op0=ALU.mult), reads=[arg], writes=[ki])
                        k.op("dve", lambda v: v.tensor_copy(kf[:, 0:nt], ki[:, 0:nt]), reads=[ki], writes=[kf])
                        k.op("dve", lambda v: v.scalar_tensor_tensor(out=arg[:, 0:nt], in0=kf[:, 0:nt], scalar=-TWO_PI, in1=arg[:, 0:nt], op0=ALU.mult, op1=ALU.add), reads=[kf, arg], writes=[arg])
                        k.op("dve", lambda v: v.tensor_scalar(out=arg[:, 0:nt], in0=arg[:, 0:nt], scalar1=3.14159, scalar2=-3.14159, op0=ALU.min, op1=ALU.max), reads=[arg], writes=[arg])
                        k.op("act", lambda a, dst=dst, t0=t0: a.activation(out=dst[:, t0:t0 + nt], in_=arg[:, 0:nt], func=AF.Sin), reads=[arg], writes=[dst])
                hrs = [k.sb("hr", [128, 1024], F32, e2) for _ in range(2)]
                ed = k.sb("ed", [128, 1024], F32, e2)
                sq = k.sb("sqh", [128, 1024], F32, e2)
                hsd = k.sb("hsd", [128, nch, 2, 512], BF16, e2)
                pen = pe_[0]
                for tc in range(nch):
                    hr = hrs[tc % 2]
                    k.op("act", lambda a, tc=tc: a.activation(out=ed[:], in_=dec[:], func=AF.Exp, scale=ntu[:, tc:tc + 1]), reads=[dec, ntu], writes=[ed])
                    for hh in range(2):
                        ps = rot(pp)
                        k.op("pe", lambda pe, ps=ps, tc=tc, hh=hh: pe.matmul(ps[:, :], f2[0:64, tc * 128:(tc + 1) * 128], wf3[0:64, hh * 512:(hh + 1) * 512], start=True, stop=True), reads=[f2, wf3], writes=[ps])
                        k.op("dve", lambda v, ps=ps, hr=hr, hh=hh: v.tensor_tensor(out=hr[:, hh * 512:(hh + 1) * 512], in0=ps[:, :], in1=ed[:, hh * 512:(hh + 1) * 512], op=ALU.mult), reads=[ps, ed], writes=[hr])
                    if tc == 0:
                        for o_ in range(2):
                            k.op("dve", lambda v, o_=o_, hr=hr: v.memset(hr[0:1, o_ * 512 + 256:o_ * 512 + 512], 0.0), reads=[hr], writes=[hr])
                    k.op("pool", lambda g, hr=hr: g.tensor_tensor(out=sq[:], in0=hr[:], in1=hr[:], op=ALU.mult), reads=[hr], writes=[sq])

                    def f(pe, tc=tc):
                        for o_ in range(2):
                            for dd in range(2):
                                ins = pe.matmul(pe_[o_][:, 0:256], ones_f[:, :], sq[:, o_ * 512 + dd * 256:o_ * 512 + dd * 256 + 256],
                                                start=(tc == 0 and dd == 0), stop=(tc == nch - 1 and dd == 1))
                        return ins
                    k.op("pe", f, reads=[ones_f, sq], writes=[pe_[0], pe_[1]])
                    for o_ in range(2):
                        fw_ = hr[:, o_ * 512:o_ * 512 + 256]
                        bw_ = hr[:, o_ * 512 + 256:o_ * 512 + 512]
                        k.op("pool", lambda g, tc=tc, o_=o_, fw_=fw_, bw_=bw_: g.tensor_tensor(out=hsd[:, tc, 0, o_ * 256:(o_ + 1) * 256], in0=fw_, in1=bw_, op=ALU.add), reads=[hr], writes=[hsd])
                        k.op("dve", lambda v, tc=tc, o_=o_, fw_=fw_, bw_=bw_: v.tensor_tensor(out=hsd[:, tc, 1, o_ * 256:(o_ + 1) * 256], in0=bw_, in1=fw_, op=ALU.subtract), reads=[hr], writes=[hsd])
                rn = k.sb("rn", [128, 512], F32, e2)
                for o_ in range(2):
                    k.op("act", lambda a, o_=o_: a.activation(out=rn[:, o_ * 256:(o_ + 1) * 256], in_=pe_[o_][:, 0:256], func=AF.Sqrt, bias=eps6[:, 0:1]), reads=[pe_[o_], eps6], writes=[rn])
                k.op("dve", lambda v: v.reciprocal(out=rn[:], in_=rn[:]), reads=[rn], writes=[rn])
                cfs = [k.sb("cfp", [128, nch, 128], BF16, e2) for _ in range(2)]
                sfs = [k.sb("sfp", [128, nch, 128], BF16, e2) for _ in range(2)]
                hks = [k.sb("hk", [128, 2, 512], F32, e2) for _ in range(2)]
                for fc in range(nch):
                    cf, sf, hk = cfs[fc % 2], sfs[fc % 2], hks[fc % 2]
                    k.dma("sp", cf[:], IN("cf_" + tag)[fc], writes=[cf])
                    k.dma("sp", sf[:], IN("sf_" + tag)[fc], writes=[sf])
                    pr_, pi_ = rot(pp), rot(pp)

                    def f(pe, cf=cf, sf=sf, pr_=pr_, pi_=pi_):
                        for tc in range(nch):
                            pe.matmul(pr_[:, :], cf[:, tc, :], hsd[:, tc, 0, :], start=(tc == 0), stop=(tc == nch - 1))
                        for tc in range(nch):
                            ins = pe.matmul(pi_[:, :], sf[:, tc, :], hsd[:, tc, 1, :], start=(tc == 0), stop=(tc == nch - 1))
                        return ins
                    k.op("pe", f, reads=[cf, sf, hsd], writes=[pr_, pi_])
                    k.op("dve", lambda v, hk=hk, pr_=pr_: v.tensor_tensor(out=hk[:, 0, :], in0=pr_[:, :], in1=rn[:], op=ALU.mult), reads=[pr_, rn], writes=[hk])
                    k.op("dve", lambda v, hk=hk, pi_=pi_: v.tensor_tensor(out=hk[:, 1, :], in0=pi_[:, :], in1=rn[:], op=ALU.mult), reads=[pi_, rn], writes=[hk])
                    k.dma("sp", Hd[tag].t.ap()[:, fc * 128:(fc + 1) * 128, :].rearrange("r p n -> p r n"), hk[:], reads=[hk], writes=[Hd[tag]])
                k.barrier()

    def mixer_d(l, j):
        with ExitStack() as e2:
            pdb = [k.sb("pdb", [128, PW], BF16, e2) for _ in range(6)]
            z = [k.sb("zD", [128, PW], F32, e2) for _ in range(2)]
            vD = k.sb("vD", [128, 6, 4], F32, e2)
            vD2 = k.sb("vD2", [128, 4], F32, e2)
            k.dma("sp", vD[:], IN("vecD")[l], writes=[vD])
            k.dma("sp", vD2[:], IN("vecD2")[l], writes=[vD2])
            dg = k.sb("dgD", [128, 18, 128], BF16, e2)
            for c in range(6):
                for kk in range(3):
                    k.op("dve", lambda v, c=c, kk=kk: v.tensor_scalar(out=dg[:, c * 3 + kk, :], in0=ident_f[:], scalar1=vD[:, c, kk:kk + 1], scalar2=None, op0=ALU.mult), reads=[ident_f, vD], writes=[dg])
            with ExitStack() as e3:
                wg = load_w_in(e3, l, 1792, 768)
                pp = [k.ps("ppd0", [128, 512], F32, e3) for _ in range(4)]
                for oc in range(6):
                    proj(wg, oc * 128, pp, lambda ps, c0, n, oc=oc: k.op("act" if oc % 2 == 0 else "dve",
                         (lambda a: a.copy(pdb[oc][:, c0:c0 + n], ps[:, 0:n])) if oc % 2 == 0 else (lambda v: v.tensor_copy(pdb[oc][:, c0:c0 + n], ps[:, 0:n])), reads=[ps], writes=[pdb[oc]]))
                k.barrier()
            pp = [k.ps("ppd", [128, 512], F32, e2) for _ in range(6)]
            ptr = [k.ps("ptr", [128, 4, 128], BF16, e2) for _ in range(2)]

            def sconv(c, c0, n, ps):
                def f(pe):
                    for kk in range(3):
                        ins = pe.matmul(ps[:, 0:n], dg[:, c * 3 + kk, :], pdb[c][:, c0 + kk - 1:c0 + kk - 1 + n], start=(kk == 0), stop=(kk == 2))
                    return ins
                k.op("pe", f, reads=[dg, pdb[c]], writes=[ps])
            for c in range(2):
                for (c0, n) in VT:
                    ps = rot(pp)
                    sconv(c, c0, n, ps)
                    k.op("act", lambda a, ps=ps, c=c, c0=c0, n=n: a.activation(out=z[c][:, c0:c0 + n], in_=ps[:, 0:n], func=AF.Identity, bias=vD[:, c, 3:4]), reads=[ps, vD], writes=[z[c]])
            zb = k.sb("zb", [128, 2, L], BF16, e2)
            zT = k.sb("zT", [128, 16, 256], BF16, e2)
            Y = k.sb("Yd", [128, 32, 256], BF16, e2)
            cfs = [k.sb("cfd", [128, 16, 128], BF16, e2) for _ in range(2)]
            sfs = [k.sb("sfd", [128, 16, 128], BF16, e2) for _ in range(2)]
            hks = [k.sb("hkd", [128, 2, 256], F32, e2) for _ in range(2)]
            tms = [k.sb("tmd", [128, 256], F32, e2) for _ in range(4)]
            cis = [k.sb("cid", [128, 4, 512], BF16, e2) for _ in range(2)]
            sis = [k.sb("sid", [128, 4, 512], BF16, e2) for _ in range(2)]
            gts = [k.sb("gtd", [128, 512], F32, e2) for _ in range(2)]
            tts = [k.sb("ttd", [128, 512], F32, e2) for _ in range(2)]
            for o_ in range(2):
                for tag, (col0, Lx, nch) in SEGS.items():
                    for c in range(2):
                        k.op("act", lambda a, c=c, col0=col0, Lx=Lx: a.copy(zb[:, c, 0:Lx], z[c][:, col0:col0 + Lx]), reads=[z[c]], writes=[zb])
                    for tc in range(nch):
                        pt = ptr[tc % 2]

                        def f(pe, pt=pt, tc=tc):
                            for c in range(2):
                                ins = pe.transpose(pt[:, c, :], zb[:, c, tc * 128:(tc + 1) * 128], ident_b[:, :])
                            return ins
                        k.op("pe", f, reads=[zb, ident_b], writes=[pt])
                        k.op("dve", lambda v, pt=pt, tc=tc: v.tensor_copy(zT[:, tc, :], pt[:, 0:2, :]), reads=[pt], writes=[zT])
                    for fc in range(nch):
                        cf, sf, hk = cfs[fc % 2], sfs[fc % 2], hks[fc % 2]
                        k.dma("sp", cf[:, 0:nch, :], IN("cf_" + tag)[fc], writes=[cf])
                        k.dma("sp", sf[:, 0:nch, :], IN("sf_" + tag)[fc], writes=[sf])
                        k.dma("sp", hk[:], Hd[tag].t.ap()[:, fc * 128:(fc + 1) * 128, o_ * 256:(o_ + 1) * 256].rearrange("r p n -> p r n"), reads=[Hd[tag]], writes=[hk])
                        pa, pb = rot(pp), rot(pp)

                        def f(pe, cf=cf, sf=sf, pa=pa, pb=pb, nch=nch):
                            for tc in range(nch):
                                pe.matmul(pa[:, 0:256], cf[:, tc, :], zT[:, tc, :], start=(tc == 0), stop=(tc == nch - 1))
                            for tc in range(nch):
                                ins = pe.matmul(pb[:, 0:256], sf[:, tc, :], zT[:, tc, :], start=(tc == 0), stop=(tc == nch - 1))
                            return ins
                        k.op("pe", f, reads=[cf, sf, zT], writes=[pa, pb])
                        t1, t2, t3, t4 = tms
                        k.op("dve", lambda v, pa=pa, hk=hk: v.tensor_tensor(out=t1[:], in0=pa[:, 0:256], in1=hk[:, 0, :], op=ALU.mult), reads=[pa, hk], writes=[t1])
                        k.op("dve", lambda v, pb=pb, hk=hk: v.tensor_tensor(out=t2[:], in0=pb[:, 0:256], in1=hk[:, 1, :], op=ALU.mult), reads=[pb, hk], writes=[t2])
                        k.op("pool", lambda g, fc=fc: g.tensor_tensor(out=Y[:, fc, :], in0=t1[:], in1=t2[:], op=ALU.add), reads=[t1, t2], writes=[Y])
                        k.op("dve", lambda v, pa=pa, hk=hk: v.tensor_tensor(out=t3[:], in0=pa[:, 0:256], in1=hk[:, 1, :], op=ALU.mult), reads=[pa, hk], writes=[t3])
                        k.op("dve", lambda v, pb=pb, hk=hk: v.tensor_tensor(out=t4[:], in0=pb[:, 0:256], in1=hk[:, 0, :], op=ALU.mult), reads=[pb, hk], writes=[t4])
                        k.op("pool", lambda g, fc=fc, nch=nch: g.tensor_tensor(out=Y[:, nch + fc, :], in0=t3[:], in1=t4[:], op=ALU.subtract), reads=[t3, t4], writes=[Y])
                    nt = min(512, Lx)
                    for ti_, t0 in enumerate(range(0, Lx, nt)):
                        py = [rot(pp), rot(pp)]
                        ngrp = (nch + 3) // 4
                        for g_ in range(ngrp):
                            ci, si = cis[g_ % 2], sis[g_ % 2]
                            nf = min(4, nch - g_ * 4)
                            k.dma("sp", ci[:, 0:nf, 0:nt], IN("ci_" + tag)[:, g_ * 4:g_ * 4 + nf, t0:t0 + nt], writes=[ci])
                            k.dma("sp", si[:, 0:nf, 0:nt], IN("si_" + tag)[:, g_ * 4:g_ * 4 + nf, t0:t0 + nt], writes=[si])

                            def f(pe, ci=ci, si=si, g_=g_, nf=nf, py=py, nch=nch, ngrp=ngrp):
                                for c in range(2):
                                    for ff in range(nf):
                                        fc = g_ * 4 + ff
                                        pe.matmul(py[c][:, 0:nt], Y[:, fc, c * 128:(c + 1) * 128], ci[:, ff, 0:nt], start=(fc == 0), stop=False)
                                        ins = pe.matmul(py[c][:, 0:nt], Y[:, nch + fc, c * 128:(c + 1) * 128], si[:, ff, 0:nt], start=False, stop=(fc == nch - 1))
                                return ins
                            k.op("pe", f, reads=[Y, ci, si], writes=[py[0], py[1]])
                        c0 = col0 + t0
                        for c in range(2):
                            gc = 2 + 2 * o_ + c
                            pg = rot(pp)
                            sconv(gc, c0, nt, pg)
                            gt, tt_ = gts[c], tts[c]
                            k.op("act", lambda a, pg=pg, gt=gt, gc=gc: a.activation(out=gt[:, 0:nt], in_=pg[:, 0:nt], func=AF.Identity, bias=vD[:, gc, 3:4]), reads=[pg, vD], writes=[gt])
                            k.op("dve", lambda v, c=c, tt_=tt_, c0=c0, py=py, o_=o_: v.scalar_tensor_tensor(out=tt_[:, 0:nt], in0=z[c][:, c0:c0 + nt], scalar=vD2[:, o_ * 2 + c:o_ * 2 + c + 1], in1=py[c][:, 0:nt],
                                                                                                op0=ALU.mult, op1=ALU.add), reads=[z[c], vD2, py[c]], writes=[tt_])
                            if o_ == 0:
                                k.op("pool", lambda g, c=c, tt_=tt_, gt=gt, c0=c0: g.tensor_tensor(out=z[c][:, c0:c0 + nt], in0=tt_[:, 0:nt], in1=gt[:, 0:nt], op=ALU.mult), reads=[tt_, gt, zb], writes=[z[c]])
                            else:
                                k.op("pool", lambda g, c=c, tt_=tt_, gt=gt, c0=c0: g.tensor_tensor(out=yT[:, 6 + c, c0:c0 + nt], in0=tt_[:, 0:nt], in1=gt[:, 0:nt], op=ALU.mult), reads=[tt_, gt], writes=[yT])
            k.barrier()

    def ffn_phase(l, last):
        tiles = [(j, ti) for j in range(nseq) for ti in (range(2, 18) if last else range(18))]
        nt = len(tiles)
        NBk = (2 * nt * 128) // MOE_S + NE
        wgT = IN("moe_w_gate").rearrange("l e (p j) n -> (l e p) (j n)", j=8)
        wuT = IN("moe_w_up").rearrange("l e (p j) n -> (l e p) (j n)", j=8)
        wdT = IN("moe_w_down").rearrange("l e (p j) n -> (l e p) (j n)", j=4)
        hrow_b = [Buf(hrow.t) for _ in range(nt)]
        xs_w = [Buf(xs.t) for _ in range(nt)]
        ysl_b = [Buf(yslot.t) for _ in range(NBk)]
        with ExitStack() as e2:
            zt = k.sb("zt", [128, 2 * D], BF16, e2)
            k.op("pool", lambda g: g.memset(zt[:], 0.0), writes=[zt])
            for i_ in range(2 * NBk):
                k.dma("sp", xs.t.ap()[0:NBk * MOE_S, :].rearrange("(p r) d -> p r d", p=128)[:, 2 * i_:2 * i_ + 2, :], zt[:].rearrange("p (r d) -> p r d", r=2), reads=[zt], writes=[xs_z])
            GT = k.sb("GT", [128, nt, 2], F32, e2)
            DST = k.sb("DST", [128, nt, 2], F32, e2)
            DSTi = k.sb("DSTi", [128, nt, 2], I32, e2)
            run = k.sb("run", [128, 32], F32, e2)
            tokid = k.sb("tokid", [128, NB_ * 18], I32, e2)
            iow = k.sb("iow", [128, 16], F32, e2)
            blkS = k.sb("blkS", [128, 80], F32, e2)
            nbi = k.sb("nbi", [128, 32], I32, e2)
            pad_ = k.sb("pad_", [128, 32], F32, e2)
            pend = k.sb("pend", [128, 32], F32, e2)
            pst_ = k.sb("pst_", [128, 32], F32, e2)
            tq = k.sb("tq", [128, 32], F32, e2)
            tq2 = k.sb("tq2", [128, 32], F32, e2)
            bacc = k.sb("bacc", [128, NBk], F32, e2)
            be1 = k.sb("be1", [128, NBk], F32, e2)
            be2 = k.sb("be2", [128, NBk], F32, e2)
            WI = k.sb("WI", [128, NBk], I32, e2)
            zi = k.sb("zi", [128, 512], I32, e2)
            hb2s = [k.sb("hb2", [128, D], BF16, e2) for _ in range(2)]
            eR = ExitStack()
            OH1 = k.sb("OH1", [128, nt, 32], F32, eR)
            OH2 = k.sb("OH2", [128, nt, 32], F32, eR)
            RK = k.sb("RK", [128, nt, 32], F32, eR)
            T32 = k.sb("T32", [128, nt, 32], F32, eR)
            WIf = k.sb("WIf", [128, NBk], F32, eR)
            k.dma("sp", tokid[:], IN("tokid"), writes=[tokid])
            k.dma("sp", iow[:], IN("iota_w"), writes=[iow])
            k.dma("sp", blkS[:], IN("blkS"), writes=[blkS])
            k.op("dve", lambda v: v.memset(run[:], 0.0), writes=[run])
            with ExitStack() as e3:
                A1 = k.sb("A1f", [128, D], F32, e3)
                A0 = k.sb("A0f", [128, D], F32, e3)
                A1c = k.sb("A1cf", [128, D], F32, e3)
                A0c = k.sb("A0cf", [128, D], F32, e3)
                gtl = k.sb("gtf", [128, D], F32, e3)
                k.dma("sp", gtl[:], IN("g_ffn")[l:l + 1, :].to_broadcast([128, D]), writes=[gtl])

                def fill(A1_, A0_, r):
                    mod_bc(A1_, l, r, 4)
                    mod_bc(A0_, l, r, 3)
                    k.op("dve", lambda v: v.scalar_tensor_tensor(out=A1_[:], in0=A1_[:], scalar=1.0, in1=gtl[:], op0=ALU.add, op1=ALU.mult), reads=[A1_, gtl], writes=[A1_])
                if not last:
                    fill(A1c, A0c, 4)
                wr = k.sb("wr", [128, 8, 36], F32, e3)
                k.dma("sp", wr[:], IN("wr")[l].rearrange("(kc p) n -> p kc n", p=128), writes=[wr])
                brb = k.sb("brb", [128, 36], F32, e3)
                k.dma("sp", brb[:], IN("br")[l:l + 1, :].to_broadcast([128, 36]), writes=[brb])
                utri = k.sb("utri", [128, 128], BF16, e3)
                onesb = k.sb("onesb", [128, 128], BF16, e3)
                k.dma("sp", utri[:], IN("utri_b"), writes=[utri])
                k.dma("sp", onesb[:], IN("ones_b"), writes=[onesb])
                xts = [k.sb("xtm", [128, D], F32, e3) for _ in range(2)]
                hfs = [k.sb("hfm", [128, D], F32, e3) for _ in range(1)] * 2
                hbs = [k.sb("hbm", [128, D], BF16, e3) for _ in range(2)]
                hTfs = [k.sb("hTf", [128, 8, 128], F32, e3) for _ in range(2)]
                tmp = k.sb("tmpm", [128, D], F32, e3)
                sts = [[k.sb("stm", [128, 1], F32, e3) for _ in range(3)] for _ in range(2)]
                ptfs = [k.ps("ptf", [128, 8, 128], F32, e3) for _ in range(2)]
                plgs = [k.ps("plg", [128, 512], F32, e3) for _ in range(2)]
                pR1 = k.ps("pR1", [128, 512], F32, e3)
                pR2 = k.ps("pR2", [128, 512], F32, e3)
                LG = k.sb("LG", [128, nt, 36], F32, e3)
                gm = k.sb("gm", [128, 8], F32, e3)
                ohg = k.sb("ohg", [128, 4], F32, e3)
                eg = k.sb("eg", [128, 4], F32, e3)
                les = k.sb("les", [128, 8], F32, e3)
                oh1 = k.sb("oh1", [128, 8], F32, e3)
                msk = k.sb("msk", [128, 8], F32, e3)
                oh2 = k.sb("oh2", [128, 8], F32, e3)
                Mb = k.sb("Mb", [128, 32], BF16, e3)
                curj = None
                for t, (j, ti) in enumerate(tiles):
                    col, isctx = TT[ti]
                    if j != curj:
                        fill(A1, A0, j)
                        curj = j
                    xt, hf, hb, hTf, st, ptf, plg = xts[t % 2], hfs[t % 2], hbs[t % 2], hTfs[t % 2], sts[t % 2], ptfs[t % 2], plgs[t % 2]
                    r0 = tok_row(j, ti)
                    xb_ = xres_b[(j, ti)]
                    k.dma("sp", xt[:], xres.t.ap()[r0:r0 + 128, :], reads=[xb_], writes=[xt])
                    norm_mod(xt, A1c if isctx else A1, A0c if isctx else A0, tmp, hf, st)
                    k.op("dve", lambda v, hf=hf, hb=hb: v.tensor_copy(hb[:], hf[:]), reads=[hf], writes=[hb])
                    k.dma("sp", hrow.t.ap()[r0:r0 + 128, :], hb[:], reads=[hb], writes=[hrow_b[t]])

                    def f(pe, hf=hf, ptf=ptf):
                        for kc in range(8):
                            ins = pe.transpose(ptf[:, kc, :], hf[:, kc * 128:(kc + 1) * 128], ident_f[:, :])
                        return ins
                    k.op("pe", f, reads=[hf, ident_f], writes=[ptf])
                    k.op("dve", lambda v, ptf=ptf, hTf=hTf: v.tensor_copy(hTf[:], ptf[:, :, :]), reads=[ptf], writes=[hTf])

                    def f(pe, hTf=hTf, plg=plg):
                        for kc in range(8):
                            ins = pe.matmul(plg[:, 0:36], hTf[:, kc, :], wr[:, kc, :], start=(kc == 0), stop=(kc == 7))
                        return ins
                    k.op("pe", f, reads=[hTf, wr], writes=[plg])
                    V = lambda fn, rd, wrr: k.op("dve", fn, reads=rd, writes=wrr)
                    V(lambda v, plg=plg, t=t: v.tensor_tensor(out=LG[:, t, :], in0=plg[:, 0:36], in1=brb[:], op=ALU.add), [plg, brb], [LG])
                S1_ = lambda nm: k.sb(nm, [128, nt, 1], F32, e3)
                GMx, SGs, PGs, M1s, M2s, DEs, P1s = [S1_("r1_%d" % i_) for i_ in range(7)]
                OHG = k.sb("OHG", [128, nt, 4], F32, e3)
                EG = k.sb("EG", [128, nt, 4], F32, e3)
                LES = k.sb("LES", [128, nt, 8], F32, e3)
                T8 = k.sb("T8", [128, nt, 8], F32, e3)
                O1s = k.sb("O1s", [128, nt, 8], F32, e3)
                MSK = k.sb("MSK", [128, nt, 8], F32, e3)
                O2s = k.sb("O2s", [128, nt, 8], F32, e3)
                MbA = k.sb("MbA", [128, nt, 32], BF16, e3)
                bc = lambda ap_, n_: ap_.to_broadcast([128, nt, n_])
                V(lambda v: v.reduce_max(out=GMx[:], in_=LG[:, :, 0:4], axis=AX.X), [LG], [GMx])
                V(lambda v: v.tensor_tensor(out=OHG[:], in0=LG[:, :, 0:4], in1=bc(GMx[:, :, 0:1], 4), op=ALU.is_equal), [LG, GMx], [OHG])
                V(lambda v: v.tensor_tensor(out=EG[:], in0=LG[:, :, 0:4], in1=bc(GMx[:, :, 0:1], 4), op=ALU.subtract), [LG, GMx], [EG])
                k.op("act", lambda a: a.activation(out=EG[:], in_=EG[:], func=AF.Exp), reads=[EG], writes=[EG])
                V(lambda v: v.reduce_sum(out=SGs[:], in_=EG[:], axis=AX.X), [EG], [SGs])
                V(lambda v: v.reciprocal(out=PGs[:], in_=SGs[:]), [SGs], [PGs])
                V(lambda v: v.tensor_tensor(out=LES[:], in0=LG[:, :, 4:12], in1=bc(OHG[:, :, 0:1], 8), op=ALU.mult), [LG, OHG], [LES])
                for g_ in range(1, 4):
                    V(lambda v, g_=g_: v.tensor_tensor(out=T8[:], in0=LG[:, :, 4 + 8 * g_:12 + 8 * g_], in1=bc(OHG[:, :, g_:g_ + 1], 8), op=ALU.mult), [LG, OHG], [T8])
                    V(lambda v: v.tensor_tensor(out=LES[:], in0=LES[:], in1=T8[:], op=ALU.add), [LES, T8], [LES])
                V(lambda v: v.reduce_max(out=M1s[:], in_=LES[:], axis=AX.X), [LES], [M1s])
                V(lambda v: v.tensor_tensor(out=O1s[:], in0=LES[:], in1=bc(M1s[:, :, 0:1], 8), op=ALU.is_equal), [LES, M1s], [O1s])
                V(lambda v: v.scalar_tensor_tensor(out=MSK[:], in0=O1s[:], scalar=-1e30, in1=LES[:], op0=ALU.mult, op1=ALU.add), [O1s, LES], [MSK])
                V(lambda v: v.reduce_max(out=M2s[:], in_=MSK[:], axis=AX.X), [MSK], [M2s])
                V(lambda v: v.tensor_tensor(out=O2s[:], in0=MSK[:], in1=bc(M2s[:, :, 0:1], 8), op=ALU.is_equal), [MSK, M2s], [O2s])
                V(lambda v: v.tensor_tensor(out=DEs[:], in0=M2s[:], in1=M1s[:], op=ALU.subtract), [M1s, M2s], [DEs])
                k.op("act", lambda a: a.activation(out=DEs[:], in_=DEs[:], func=AF.Exp), reads=[DEs], writes=[DEs])
                V(lambda v: v.tensor_scalar(out=P1s[:], in0=DEs[:], scalar1=1.0, scalar2=None, op0=ALU.add), [DEs], [P1s])
                V(lambda v: v.reciprocal(out=P1s[:], in_=P1s[:]), [P1s], [P1s])
                V(lambda v: v.tensor_tensor(out=GT[:, :, 0:1], in0=P1s[:], in1=PGs[:], op=ALU.mult), [P1s, PGs], [GT])
                V(lambda v: v.tensor_tensor(out=GT[:, :, 1:2], in0=GT[:, :, 0:1], in1=DEs[:], op=ALU.mult), [GT, DEs], [GT])
                for g_ in range(4):
                    V(lambda v, g_=g_: v.tensor_tensor(out=OH1[:, :, 8 * g_:8 * g_ + 8], in0=O1s[:], in1=bc(OHG[:, :, g_:g_ + 1], 8), op=ALU.mult), [O1s, OHG], [OH1])
                    V(lambda v, g_=g_: v.tensor_tensor(out=OH2[:, :, 8 * g_:8 * g_ + 8], in0=O2s[:], in1=bc(OHG[:, :, g_:g_ + 1], 8), op=ALU.mult), [O2s, OHG], [OH2])
                V(lambda v: v.tensor_tensor(out=MbA[:], in0=OH1[:], in1=OH2[:], op=ALU.add), [OH1, OH2], [MbA])
                for t in range(nt):
                    k.op("pe", lambda pe, t=t: pe.matmul(pR1[:, 0:32], utri[:, :], MbA[:, t, :], start=True, stop=True), reads=[utri, MbA], writes=[pR1])
                    k.op("pe", lambda pe, t=t: pe.matmul(pR2[:, 0:32], onesb[:, :], MbA[:, t, :], start=True, stop=True), reads=[onesb, MbA], writes=[pR2])
                    V(lambda v, t=t: v.tensor_tensor(out=RK[:, t, :], in0=pR1[:, 0:32], in1=run[:], op=ALU.add), [pR1, run], [RK])
                    V(lambda v: v.tensor_tensor(out=run[:], in0=pR2[:, 0:32], in1=run[:], op=ALU.add), [pR2, run], [run])
                k.barrier()
            V = lambda fn, rd, wrr: k.op("dve", fn, reads=rd, writes=wrr)
            V(lambda v: v.tensor_scalar(out=pad_[:], in0=run[:], scalar1=1.0 / MOE_S, scalar2=(MOE_S - 1.0) / MOE_S - 0.499, op0=ALU.mult, op1=ALU.add), [run], [pad_])
            V(lambda v: v.tensor_copy(nbi[:], pad_[:]), [pad_], [nbi])
            V(lambda v: v.tensor_copy(pad_[:], nbi[:]), [nbi], [pad_])
            V(lambda v: v.tensor_scalar(out=pad_[:], in0=pad_[:], scalar1=float(MOE_S), scalar2=None, op0=ALU.mult), [pad_], [pad_])
            V(lambda v: v.tensor_tensor_scan(out=pend[:], data0=ones_f[:, 0:32], data1=pad_[:], initial=0.0, op0=ALU.mult, op1=ALU.add), [ones_f, pad_], [pend])
            V(lambda v: v.tensor_tensor(out=pst_[:], in0=pend[:], in1=pad_[:], op=ALU.subtract), [pend, pad_], [pst_])
            V(lambda v: v.tensor_tensor(out=RK[:], in0=RK[:], in1=pst_[:, :].unsqueeze(1).to_broadcast([128, nt, 32]), op=ALU.add), [RK, pst_], [RK])
            V(lambda v: v.tensor_tensor(out=T32[:], in0=RK[:], in1=OH1[:], op=ALU.mult), [RK, OH1], [T32])
            V(lambda v: v.reduce_sum(out=DST[:, :, 0:1], in_=T32[:], axis=AX.X), [T32], [DST])
            V(lambda v: v.tensor_tensor(out=T32[:], in0=RK[:], in1=OH2[:], op=ALU.mult), [RK, OH2], [T32])
            V(lambda v: v.reduce_sum(out=DST[:, :, 1:2], in_=T32[:], axis=AX.X), [T32], [DST])
            V(lambda v: v.tensor_copy(DSTi[:], DST[:]), [DST], [DSTi])
            V(lambda v: v.memset(bacc[:], 0.0), [], [bacc])
            for e_ in range(NE):
                V(lambda v, e_=e_: v.scalar_tensor_tensor(out=bacc[:], in0=blkS[:, 0:NBk], scalar=pend[:, e_:e_ + 1], in1=bacc[:], op0=ALU.is_ge, op1=ALU.add), [blkS, pend, bacc], [bacc])
            V(lambda v: v.tensor_scalar(out=bacc[:], in0=bacc[:], scalar1=float(NE - 1), scalar2=None, op0=ALU.min), [bacc], [bacc])
            V(lambda v: v.tensor_scalar(out=be1[:], in0=bacc[:], scalar1=128.0, scalar2=float(l * NE * 128), op0=ALU.mult, op1=ALU.add), [bacc], [be1])
            V(lambda v: v.tensor_scalar(out=WIf[:], in0=be1[:], scalar1=iow[:, 0:1], scalar2=None, op0=ALU.add), [be1, iow], [WIf])
            V(lambda v: v.tensor_copy(WI[:], WIf[:]), [WIf], [WI])
            for t, (j, ti) in enumerate(tiles):
                hb2 = hb2s[t % 2]
                r0 = tok_row(j, ti)
                k.dma("sp", hb2[:], hrow.t.ap()[r0:r0 + 128, :], reads=[hrow_b[t]], writes=[hb2])
                for q_ in range(2):
                    k.idma(xs.t.ap(), bass.IndirectOffsetOnAxis(ap=DSTi[:, t, q_:q_ + 1], axis=0), hb2[:, :], None, reads=[DSTi, hb2, xs_z], writes=[xs_w[t]])
            if "moe" in dbg:
                o = dbg_out("dst", [128, nt, 2])
                k.dma("sp", o.ap(), DST[:], reads=[DST])
                o = dbg_out("gt", [128, nt, 2])
                k.dma("sp", o.ap(), GT[:], reads=[GT])
                o = dbg_out("blke", [128, NBk])
                k.dma("sp", o.ap(), bacc[:], reads=[bacc])
                o = dbg_out("oh1", [128, nt, 32])
                k.dma("sp", o.ap(), OH1[:], reads=[OH1])
            k.barrier()
            eR.close()
            with ExitStack() as e3:
                wgs = [k.sb("mwg", [128, 8, 512], BF16, e3) for _ in range(2)]
                wus = [k.sb("mwu", [128, 8, 512], BF16, e3) for _ in range(2)]
                wds = [k.sb("mwd", [128, 4, D], BF16, e3) for _ in range(2)]
                stoks = [k.sb("stok", [128, 4], I32, e3) for _ in range(2)]
                xbs = [k.sb("mxb", [128, 4, D], BF16, e3) for _ in range(2)]
                xbTs = [k.sb("mxbT", [128, 8, 512], BF16, e3) for _ in range(2)]
                hids = [k.sb("mhid", [128, 4, 512], BF16, e3) for _ in range(2)]
                sgs = [k.sb("msg", [128, 512], F32, e3) for _ in range(2)]
                ybs = [k.sb("myb", [128, D], F32, e3) for _ in range(2)]
                ptT = [k.ps("mptT", [128, 512], BF16, e3) for _ in range(2)]
                pgu = [k.ps("mpgu", [128, 512], F32, e3) for _ in range(4)]
                pyy = [k.ps("mpy", [128, 512], F32, e3) for _ in range(2)]
                cnt = 0
                for b in range(0 if os.environ.get('KSKIPBLK') else NBk):
                    wg_, wu_, wd_, stok, xb, xbT, hid = wgs[b % 2], wus[b % 2], wds[b % 2], stoks[b % 2], xbs[b % 2], xbTs[b % 2], hids[b % 2]
                    k.dma("sp", xb[:], xs.t.ap()[b * MOE_S:(b + 1) * MOE_S, :].rearrange("(p a) d -> p a d", a=4), reads=xs_w + [xs_z], writes=[xb])
                    k.idma(wg_[:].rearrange("p j n -> p (j n)"), None, wgT, bass.IndirectOffsetOnAxis(ap=WI[:, b:b + 1], axis=0), reads=[WI], writes=[wg_])
                    k.idma(wu_[:].rearrange("p j n -> p (j n)"), None, wuT, bass.IndirectOffsetOnAxis(ap=WI[:, b:b + 1], axis=0), reads=[WI], writes=[wu_])
                    k.idma(wd_[:].rearrange("p j n -> p (j n)"), None, wdT, bass.IndirectOffsetOnAxis(ap=WI[:, b:b + 1], axis=0), reads=[WI], writes=[wd_])
                    for kc in range(8):
                        pt = ptT[kc % 2]

                        def f(pe, pt=pt, kc=kc, xb=xb):
                            for a_ in range(4):
                                ins = pe.transpose(pt[:, a_ * 128:(a_ + 1) * 128], xb[:, a_, kc::8], ident_b[:, :])
                            return ins
                        k.op("pe", f, reads=[xb, ident_b], writes=[pt])
                        if kc % 2 == 0:
                            k.op("act", lambda a, pt=pt, kc=kc, xbT=xbT: a.copy(xbT[:, kc, :], pt[:, :]), reads=[pt], writes=[xbT])
                        else:
                            k.op("dve", lambda v, pt=pt, kc=kc, xbT=xbT: v.tensor_copy(xbT[:, kc, :], pt[:, :]), reads=[pt], writes=[xbT])
                    for ec in range(4):
                        pg, pu = pgu[(2 * ec) % 4], pgu[(2 * ec + 1) % 4]
                        sg = sgs[ec % 2]

                        def f(pe, pg=pg, pu=pu, ec=ec, wg_=wg_, wu_=wu_, xbT=xbT):
                            for kc in range(8):
                                pe.matmul(pg[:, :], wg_[:, kc, ec::4], xbT[:, kc, :], start=(kc == 0), stop=(kc == 7))
                            for kc in range(8):
                                ins = pe.matmul(pu[:, :], wu_[:, kc, ec::4], xbT[:, kc, :], start=(kc == 0), stop=(kc == 7))
                            return ins
                        k.op("pe", f, reads=[wg_, wu_, xbT], writes=[pg, pu])
                        k.op("act", lambda a, pg=pg, sg=sg: a.activation(out=sg[:], in_=pg[:, :], func=AF.Silu), reads=[pg], writes=[sg])
                        k.op("dve", lambda v, pu=pu, sg=sg, hid=hid, ec=ec: v.tensor_tensor(out=hid[:, ec, :], in0=pu[:, :], in1=sg[:], op=ALU.mult), reads=[pu, sg], writes=[hid])
                    for a_ in range(4):
                        yb = ybs[a_ % 2]
                        for nh in range(2):
                            py = pyy[nh]

                            def f(pe, py=py, a_=a_, nh=nh, hid=hid, wd_=wd_):
                                for ec in range(4):
                                    ins = pe.matmul(py[:, :], hid[:, ec, a_ * 128:(a_ + 1) * 128], wd_[:, ec, nh * 512:(nh + 1) * 512], start=(ec == 0), stop=(ec == 3))
                                return ins
                            k.op("pe", f, reads=[hid, wd_], writes=[py])
                            if nh == 0:
                                k.op("act", lambda a, py=py, yb=yb: a.copy(yb[:, 0:512], py[:, :]), reads=[py], writes=[yb])
                            else:
                                k.op("dve", lambda v, py=py, yb=yb: v.tensor_copy(yb[:, 512:1024], py[:, :]), reads=[py], writes=[yb])
                        k.dma("sp", yslot.t.ap()[b * MOE_S:(b + 1) * MOE_S, :].rearrange("(p a) d -> p a d", a=4)[:, a_, :], yb[:], reads=[yb], writes=[ysl_b[b]])
                k.barrier()
            with ExitStack() as e3:
                A5 = k.sb("A5", [128, D], F32, e3)
                A5c = k.sb("A5c", [128, D], F32, e3)
                if not last:
                    mod_bc(A5c, l, 4, 5)
                xts = [k.sb("xtc", [128, D], F32, e3) for _ in range(2)]
                o1s = [k.sb("o1c", [128, D], F32, e3) for _ in range(2)]
                o2s = [k.sb("o2c", [128, D], F32, e3) for _ in range(2)]
                curj = None
                for t, (j, ti) in enumerate(tiles):
                    col, isctx = TT[ti]
                    if j != curj:
                        mod_bc(A5, l, j, 5)
                        curj = j
                    xt, o1_, o2_ = xts[t % 2], o1s[t % 2], o2s[t % 2]
                    r0 = tok_row(j, ti)
                    xb_ = xres_b[(j, ti)]
                    k.dma("sp", xt[:], xres.t.ap()[r0:r0 + 128, :], reads=[xb_], writes=[xt])
                    k.idma(o1_[:], None, yslot.t.ap(), bass.IndirectOffsetOnAxis(ap=DSTi[:, t, 0:1], axis=0), reads=[DSTi] + ysl_b, writes=[o1_])
                    k.idma(o2_[:], None, yslot.t.ap(), bass.IndirectOffsetOnAxis(ap=DSTi[:, t, 1:2], axis=0), reads=[DSTi] + ysl_b, writes=[o2_])
                    k.op("dve", lambda v, o1_=o1_, t=t: v.tensor_scalar(out=o1_[:], in0=o1_[:], scalar1=GT[:, t, 0:1], scalar2=None, op0=ALU.mult), reads=[o1_, GT], writes=[o1_])
                    k.op("dve", lambda v, o1_=o1_, o2_=o2_, t=t: v.scalar_tensor_tensor(out=o1_[:], in0=o2_[:], scalar=GT[:, t, 1:2], in1=o1_[:], op0=ALU.mult, op1=ALU.add), reads=[o1_, o2_, GT], writes=[o1_])
                    Ax = A5c if isctx else A5
                    k.op("pool", lambda g, o1_=o1_, Ax=Ax: g.tensor_tensor(out=o1_[:], in0=o1_[:], in1=Ax[:], op=ALU.mult), reads=[o1_, Ax], writes=[o1_])
                    k.op("dve", lambda v, o1_=o1_, xt=xt: v.tensor_tensor(out=o1_[:], in0=o1_[:], in1=xt[:], op=ALU.add), reads=[o1_, xt], writes=[o1_])
                    k.dma("sp", xres.t.ap()[r0:r0 + 128, :], o1_[:], reads=[o1_], writes=[xb_])
                k.barrier()

    def final_phase(j):
        with ExitStack() as e2:
            gf = k.sb("gf", [128, D], F32, e2)
            z0 = k.sb("z0", [128, D], F32, e2)
            k.dma("sp", gf[:], IN("g_final")[0:1, :].to_broadcast([128, D]), writes=[gf])
            k.op("dve", lambda v: v.memset(z0[:], 0.0), writes=[z0])
            xts = [k.sb("xtf", [128, D], F32, e2) for _ in range(2)]
            hos = [k.sb("hof", [128, D], F32, e2) for _ in range(2)]
            tmp = k.sb("tmpf", [128, D], F32, e2)
            sts = [[k.sb("stf", [128, 1], F32, e2) for _ in range(3)] for _ in range(2)]
            for ti in range(2, 18):
                xt, ho, st = xts[ti % 2], hos[ti % 2], sts[ti % 2]
                r0 = tok_row(j, ti)
                k.dma("sp", xt[:], xres.t.ap()[r0:r0 + 128, :], reads=[xres_b[(j, ti)]], writes=[xt])
                norm_mod(xt, gf, z0, tmp, ho, st)
                k.dma("sp", out_d.ap()[j, (ti - 2) * 128:(ti - 1) * 128, :], ho[:], reads=[ho])
            k.barrier()

    def finish():
        if "hT" in dbg:
            o = dbg_out("hT", [128, 8, PW], BF16)
            k.dma("sp", o.ap(), hT[:], reads=[hT])
        if "yT" in dbg:
            o = dbg_out("yT", [128, 8, PW], BF16)
            k.dma("sp", o.ap(), yT[:], reads=[yT])
        if "Hd" in dbg:
            for tg_, Lx_ in (("l", L), ("c", LC)):
                o = dbg_out("Hd_" + tg_, [2, Lx_, 512])
                k.dma("sp", o.ap(), Hd[tg_].t.ap(), reads=[Hd[tg_]])
        if "xres" in dbg:
            o = dbg_out("xres", [ntok, D])
            k.dma("sp", o.ap(), xres.t.ap(), reads=list(xres_b.values()))
        k.wait_all("sp")
        k.barrier()
        es.close()
        return nc, dbg_t

    steps = stop_after or "all"
    for l in range(layers):
        last = (l == DEPTH - 1)
        if steps in ("d", "all", "m"):
            hyena_prep(l)
        for j in range(nseq):
            stage1(l, j)
            if steps == "s1":
                return finish()
            if steps in ("a", "all", "w", "m"):
                mixer_a(l, j)
            if steps == "a":
                return finish()
            if steps in ("c", "all", "w", "m"):
                mixer_c(l, j)
            if steps == "c":
                return finish()
            if steps in ("b", "all", "m"):
                mixer_b(l, j)
            if steps == "b":
                return finish()
            if steps in ("d", "all", "m"):
                mixer_d(l, j)
            if steps == "d":
                return finish()
            wout_phase(l, j, last)
            if steps == "w":
                return finish()
        if steps in ("all", "m"):
            ffn_phase(l, last)
        if steps == "m":
            return finish()
    for j in range(nseq):
        final_phase(j)
    return finish()


def kernel(**inputs):
    consts = host_consts()
    n_cores = 8
    maps = []
    for c in range(n_cores):
        m = layout_inputs(inputs, c)
        maps.append(m)
    nc, _ = build(maps[0], consts)
    in_maps = []
    for m in maps:
        im = dict(m)
        im.update(consts)
        in_maps.append(im)
    res = run_bass_kernel_spmd(nc, in_maps, core_ids=list(range(n_cores)))
    out = np.concatenate([np.asarray(r["out"], np.float32) for r in res.results], axis=0)
    return out
```

```python
import math
import os
from contextlib import ExitStack
import numpy as np
import ml_dtypes
import concourse.bass as bass
import concourse.mybir as mybir
from concourse.bass_utils import run_bass_kernel_spmd

F32 = mybir.dt.float32
BF16 = mybir.dt.bfloat16
I32 = mybir.dt.int32
ALU = mybir.AluOpType
AF = mybir.ActivationFunctionType
AX = mybir.AxisListType

D = 1024
L = 2048
LC = 256
NB_ = 4
DEPTH = 4
PAD = 16
C0 = PAD
L0 = PAD + LC + PAD
PW = L0 + L + PAD
TOK = LC + L
NTOK = NB_ * TOK
EPS = 1e-6
FULL_TILES = [(0, 512), (512, 512), (1024, 512), (1536, 512), (2048, PW - 2048)]
VT = [(C0, LC)] + [(L0 + 512 * i, 512) for i in range(4)]
TT = [(C0 + 128 * i, True) for i in range(2)] + [(L0 + 128 * i, False) for i in range(16)]
N_DMA_SEMS = 12
MOE_S = 512
NE = 32


class Buf:
    __slots__ = ("t", "last_w", "readers", "name")

    def __init__(self, t, name=""):
        self.t = t
        self.last_w = None
        self.readers = []
        self.name = name

    def __getitem__(self, idx):
        return self.t[idx]


class K:
    def __init__(self, nc, es):
        self.nc = nc
        self.es = es
        self.eng = {"pe": nc.tensor, "act": nc.scalar, "dve": nc.vector, "pool": nc.gpsimd, "sp": nc.sync}
        self.sem = {}
        self.cnt = {}
        for e in self.eng:
            self.sem[e] = es.enter_context(nc.semaphore("s_" + e))
            self.cnt[e] = 0
        self.dsem = {}
        self.dcnt = {}
        self.dnext = {}
        for q in ("sp", "act", "pool"):
            self.dsem[q] = [es.enter_context(nc.semaphore("d_%s%d" % (q, i))) for i in range(N_DMA_SEMS)]
            self.dcnt[q] = [0] * N_DMA_SEMS
            self.dnext[q] = 0
        self.seen = {e: {} for e in self.eng}
        self.n_wait = 0
        self.n_inst = 0
        self.uid = 0

    def nm(self, s):
        self.uid += 1
        return "%s_%d" % (s, self.uid)

    def sb(self, name, shape, dt, es=None):
        t = (es or self.es).enter_context(self.nc.sbuf_tensor(self.nm(name), shape, dt))
        return Buf(t, name)

    def ps(self, name, shape, dt=F32, es=None):
        t = (es or self.es).enter_context(self.nc.psum_tensor(self.nm(name), shape, dt))
        return Buf(t, name)

    def dram(self, name, shape, dt, kind="Internal"):
        t = self.nc.dram_tensor(name, shape, dt, kind=kind)
        return Buf(t, name)

    def _semh(self, key):
        if key[0] == "e":
            return self.sem[key[1]]
        return self.dsem[key[1]][key[2]]

    def _wait(self, e, tok):
        key, val = tok
        if self.seen[e].get(key, 0) >= val:
            return
        self.eng[e].wait_ge(self._semh(key), val)
        self.seen[e][key] = val
        self.n_wait += 1

    def _deps(self, e, reads, writes):
        for b in reads:
            if b.last_w is not None:
                self._dep1(e, b.last_w, "raw")
        for b in writes:
            if b.last_w is not None:
                self._dep1(e, b.last_w, "waw")
            for r in b.readers:
                self._dep1(e, r, "war")

    def _dep1(self, e, tok, kind):
        key = tok[0]
        if key[0] == "e" and key[1] == e:
            if e == "pe" or kind != "raw":
                return
        self._wait(e, tok)

    def _commit(self, tok, reads, writes):
        for b in reads:
            if len(b.readers) > 24:
                b.readers = b.readers[-24:] if False else b.readers
            b.readers.append(tok)
        for b in writes:
            b.last_w = tok
            b.readers = []

    def op(self, e, fn, reads=(), writes=()):
        self._deps(e, reads, writes)
        inst = fn(self.eng[e])
        self.cnt[e] += 1
        inst.then_inc(self.sem[e], 1)
        self.n_inst += 1
        tok = (("e", e), self.cnt[e])
        self._commit(tok, reads, writes)
        return tok

    def _dq(self, q, reads, writes):
        self._deps(q, reads, writes)
        i = self.dnext[q]
        self.dnext[q] = (i + 1) % N_DMA_SEMS
        key = ("d", q, i)
        if self.dcnt[q][i] > 0:
            self._wait(q, (key, self.dcnt[q][i]))
        return i, key

    def dma(self, q, out, in_, reads=(), writes=(), **kw):
        i, key = self._dq(q, reads, writes)
        inst = self.eng[q].dma_start(out=out, in_=in_, **kw)
        self.dcnt[q][i] += 16
        inst.then_inc(self.dsem[q][i], 16)
        tok = (key, self.dcnt[q][i])
        self._commit(tok, reads, writes)
        self.n_inst += 1
        return tok

    def idma(self, out, out_off, in_, in_off, reads=(), writes=(), **kw):
        q = "pool"
        i, key = self._dq(q, reads, writes)
        inst = self.eng[q].indirect_dma_start(out=out, out_offset=out_off, in_=in_, in_offset=in_off, **kw)
        self.dcnt[q][i] += 16
        inst.then_inc(self.dsem[q][i], 16)
        tok = (key, self.dcnt[q][i])
        self._commit(tok, reads, writes)
        self.n_inst += 1
        return tok

    def wait_all(self, e):
        for e2 in self.eng:
            if self.cnt[e2] > 0 and e2 != e:
                self._wait(e, (("e", e2), self.cnt[e2]))
        for q in self.dsem:
            for i in range(N_DMA_SEMS):
                if self.dcnt[q][i] > 0:
                    self._wait(e, (("d", q, i), self.dcnt[q][i]))

    def barrier(self):
        for e in self.eng:
            self.wait_all(e)


def _bf(a):
    return np.asarray(a, np.float32).astype(ml_dtypes.bfloat16)


def host_consts():
    c = {}
    c["ident_f"] = np.eye(128, dtype=np.float32)
    c["ident_b"] = _bf(np.eye(128))
    c["ones_f"] = np.ones((128, 128), np.float32)
    c["ones_b"] = _bf(np.ones((128, 128)))
    blk = np.zeros((128, 128), np.float32)
    blk[:64, :64] = 1
    blk[64:, 64:] = 1
    c["blk64_f"] = blk
    c["utri_b"] = _bf(np.triu(np.ones((128, 128)), 1))
    inv = 10000.0 ** (-np.arange(8, dtype=np.float32) / 8)
    t = np.arange(L)
    row = (t // 64).astype(np.float32)
    col = (t % 64).astype(np.float32)
    ang = np.concatenate([row[:, None] * inv, col[:, None] * inv], -1)
    cosf = np.ones((128, PW), np.float32)
    sinf = np.zeros((128, PW), np.float32)
    rot = np.zeros((128, 128), np.float32)
    for ch in range(128):
        d = ch % 32
        cosf[ch, L0:L0 + L] = np.cos(ang[:, d % 16])
        s = np.sin(ang[:, d % 16])
        if d < 16:
            sinf[ch, L0:L0 + L] = -s
            rot[ch + 16, ch] = 1.0
        else:
            sinf[ch, L0:L0 + L] = s
            rot[ch - 16, ch] = 1.0
    c["rope_cos"] = cosf
    c["rope_sin"] = sinf
    c["rot_b"] = _bf(rot)
    for tag, Lx in (("l", L), ("c", LC)):
        N = 2 * Lx
        tt = np.arange(Lx, dtype=np.float64)
        ff = np.arange(Lx, dtype=np.float64) + 0.5
        th = 2 * np.pi * np.outer(tt, ff) / N
        nch = Lx // 128
        CF = np.cos(th)
        SF = np.sin(th)
        def fw(M):
            return _bf(M.reshape(nch, 128, nch, 128).transpose(2, 1, 0, 3).copy())
        c["cf_" + tag] = fw(CF)
        c["sf_" + tag] = fw(SF)
        def iv(M):
            return _bf(M.T.reshape(nch, 128, Lx).transpose(1, 0, 2).copy())
        c["ci_" + tag] = iv(CF * (2.0 / N))
        c["si_" + tag] = iv(-SF * (2.0 / N))
        tu = tt / max(Lx - 1, 1)
        bands = np.linspace(1e-4, 15, 16)
        a2 = (2.0 * math.pi / Lx) * tt[:, None] * bands[None, :]
        feats = np.concatenate([tu[:, None], np.cos(a2), -np.sin(a2)], -1)
        c["feats_" + tag] = feats.T.astype(np.float32).copy()
        c["ntu_" + tag] = (-tu).reshape(nch, 128).T.astype(np.float32).copy()
    io = np.zeros((128, 16), np.float32)
    for kc in range(8):
        io[:, kc] = kc * 128 + np.arange(128)
    for ec in range(4):
        io[:, 8 + ec] = ec * 128 + np.arange(128)
    c["iota_w"] = io
    c["iota_p"] = np.arange(128, dtype=np.float32).reshape(128, 1)
    c["iota_e"] = np.tile(np.arange(NE, dtype=np.float32)[None], (128, 1))
    c["zeros_i"] = np.zeros((128, 512), np.int32)
    tk = np.zeros((128, NB_ * 18), np.int32)
    for j in range(NB_):
        for ti in range(18):
            r0 = j * TOK + (ti * 128 if ti < 2 else LC + (ti - 2) * 128)
            tk[:, j * 18 + ti] = r0 + np.arange(128)
    c["tokid"] = tk
    c["blkS"] = np.tile((np.arange(80, dtype=np.float32) * MOE_S)[None], (128, 1))
    return c


def layout_inputs(inp, core, nseq=NB_):
    f = lambda a: np.ascontiguousarray(np.asarray(a, np.float32))
    b0 = core * NB_
    m = {}
    m["x"] = f(inp["x"][b0:b0 + nseq])
    m["ctx"] = f(inp["ctx"][b0:b0 + nseq])
    cv = np.concatenate([np.asarray(inp["c"], np.float32)[b0:b0 + NB_], np.asarray(inp["c_ctx"], np.float32)[None]], 0)
    m["cvT"] = f(cv.reshape(5, 8, 128).transpose(2, 1, 0))
    for n in ("w_ada", "b_ada", "g_mix", "g_ffn", "w_in", "w_out", "c_w_pw", "d_w_f1", "d_w_f2", "d_w_f3", "d_decay",
              "b_lq1", "b_lk1", "b_lq2", "b_lk2", "moe_w_gate", "moe_w_up", "moe_w_down"):
        m[n] = f(inp[n])
    m["g_final"] = f(np.asarray(inp["g_final"]).reshape(1, D))
    vA = np.zeros((DEPTH, 128, 32), np.float32)
    for d in range(2):
        for c in range(2):
            o = (d * 2 + c) * 8
            sl = slice(c * 128, c * 128 + 128)
            vA[:, :, o:o + 4] = np.asarray(inp["a_conv_w"])[:, d, :, sl].transpose(0, 2, 1)
            vA[:, :, o + 4] = np.asarray(inp["a_conv_b"])[:, d, sl]
            vA[:, :, o + 5] = np.asarray(inp["a_b_r"])[:, d, sl]
            vA[:, :, o + 6] = np.asarray(inp["a_b_i"])[:, d, sl]
            vA[:, :, o + 7] = np.asarray(inp["a_lam"])[:, d, sl]
    m["vecA"] = vA
    wb = np.zeros((DEPTH, 128, 8, 128), np.float32)
    for d in range(2):
        for g, nme in enumerate(("a_w_r", "a_w_i")):
            w = np.asarray(inp[nme])
            for c in range(2):
                i = (d * 2 + g) * 2 + c
                wb[:, 0:64, i, 0:64] = w[:, d, 2 * c]
                wb[:, 64:128, i, 64:128] = w[:, d, 2 * c + 1]
    m["wblkA"] = wb
    m["vecB"] = f(np.tile(np.asarray(inp["b_sub_g"]), (1, 2)).reshape(DEPTH, 128, 1))
    vC = np.zeros((DEPTH, 128, 2, 35), np.float32)
    for c in range(2):
        sl = slice(c * 128, c * 128 + 128)
        vC[:, :, c, 0:31] = np.asarray(inp["c_conv_w"])[:, :, sl].transpose(0, 2, 1)
        vC[:, :, c, 31] = np.asarray(inp["c_conv_b"])[:, sl]
        vC[:, :, c, 32] = np.asarray(inp["c_ln_g"])[:, sl]
        vC[:, :, c, 33] = np.asarray(inp["c_ln_b"])[:, sl]
        vC[:, :, c, 34] = np.asarray(inp["c_b_pw"])[:, sl]
    m["vecC"] = vC
    vD = np.zeros((DEPTH, 128, 6, 4), np.float32)
    for c in range(6):
        sl = slice(c * 128, c * 128 + 128)
        vD[:, :, c, 0:3] = np.asarray(inp["d_conv_w"])[:, :, sl].transpose(0, 2, 1)
        vD[:, :, c, 3] = np.asarray(inp["d_conv_b"])[:, sl]
    m["vecD"] = vD
    m["vecD2"] = f(np.asarray(inp["d_bias"]).reshape(DEPTH, 2, 2, 128).transpose(0, 3, 1, 2).reshape(DEPTH, 128, 4))
    m["hyv"] = f(np.stack([np.asarray(inp["d_b_f1"]), np.asarray(inp["d_freq"]), np.asarray(inp["d_b_f2"])], -1))
    m["wr"] = f(np.concatenate([np.asarray(inp["moe_w_rg"]), np.asarray(inp["moe_w_re"])], -1))
    m["br"] = f(np.concatenate([np.asarray(inp["moe_b_rg"]), np.asarray(inp["moe_b_re"])], -1))
    return m


DT_OF = {np.dtype("float32"): F32, np.dtype("int32"): I32, np.dtype(ml_dtypes.bfloat16): BF16}


class Prog:
    pass


def build(in_map, consts, nseq=NB_, layers=DEPTH, dbg=None, stop_after=None):
    nc = bass.Bass("TRN2", target_bir_lowering=False)
    T = Prog()
    T.din = {}
    for n, a in list(in_map.items()) + list(consts.items()):
        T.din[n] = nc.dram_tensor(n, list(a.shape), DT_OF[a.dtype], kind="ExternalInput")
    out_d = nc.dram_tensor("out", [nseq, L, D], F32, kind="ExternalOutput")
    es = ExitStack()
    k = K(nc, es)
    T.k = k
    dbg = dbg or {}
    dbg_t = {}

    def dbg_out(name, shape, dt=F32):
        dbg_t[name] = nc.dram_tensor("dbg_" + name, list(shape), dt, kind="ExternalOutput")
        return dbg_t[name]

    IN = lambda n: T.din[n].ap()
    ntok = nseq * TOK
    xres = k.dram("xres", [ntok, D], F32)
    hrow = k.dram("hrow", [ntok, D], BF16)
    modD = k.dram("modD", [DEPTH, 5, 6 * D], F32)
    NSLOT = ((2 * ntok + MOE_S - 1) // MOE_S + NE) * MOE_S
    yslot = k.dram("yslot", [NSLOT, D], F32)
    slot_tok = k.dram("slot_tok", [NSLOT, 1], I32)
    xs = k.dram("xs", [NSLOT, D], BF16)
    xs_z = Buf(xs.t)
    Hd = {"l": k.dram("Hd_l", [2, L, 512], F32), "c": k.dram("Hd_c", [2, LC, 512], F32)}
    hfil = {"l": k.dram("hfil_l", [L, 1024], F32), "c": k.dram("hfil_c", [LC, 1024], F32)}

    ident_b = k.sb("ident_b", [128, 128], BF16)
    ident_f = k.sb("ident_f", [128, 128], F32)
    ones_f = k.sb("ones_f", [128, 128], F32)
    blk64 = k.sb("blk64", [128, 128], F32)
    eps6 = k.sb("eps6", [128, 1], F32)
    eps5 = k.sb("eps5", [128, 1], F32)
    one1 = k.sb("one1", [128, 1], F32)
    for b_, n_ in ((ident_b, "ident_b"), (ident_f, "ident_f"), (ones_f, "ones_f"), (blk64, "blk64_f")):
        k.dma("sp", b_[:], IN(n_), writes=[b_])
    k.op("dve", lambda v: v.memset(eps6[:], 1e-6), writes=[eps6])
    k.op("dve", lambda v: v.memset(eps5[:], 1e-5), writes=[eps5])
    k.op("dve", lambda v: v.memset(one1[:], 1.0), writes=[one1])
    hT = k.sb("hT", [128, 8, PW], BF16)
    yT = k.sb("yT", [128, 8, PW], BF16)
    k.op("pool", lambda g: g.memset(hT[:], 0.0), writes=[hT])
    k.op("pool", lambda g: g.memset(yT[:], 0.0), writes=[yT])

    def rot(lst, st=[0]):
        st[0] += 1
        return lst[st[0] % len(lst)]

    xres_b = {(j, ti): Buf(xres.t) for j in range(nseq) for ti in range(18)}
    for j in range(nseq):
        k.dma("sp", xres.t.ap()[j * TOK:j * TOK + LC, :], IN("ctx")[j], writes=[xres_b[(j, 0)], xres_b[(j, 1)]])
        k.dma("sp", xres.t.ap()[j * TOK + LC:(j + 1) * TOK, :], IN("x")[j], writes=[xres_b[(j, ti)] for ti in range(2, 18)])

    def prologue():
        with ExitStack() as e2:
            cv = k.sb("cv", [128, 8, 5], F32, e2)
            sT = k.sb("sT", [128, 8, 5], BF16, e2)
            k.dma("sp", cv[:], IN("cvT"), writes=[cv])
            k.op("act", lambda a: a.activation(out=sT[:], in_=cv[:], func=AF.Silu), reads=[cv], writes=[sT])
            was = [k.sb("wa", [128, 8, 512], BF16, e2) for _ in range(2)]
            pms = [k.ps("pm", [128, 512], F32, e2) for _ in range(2)]
            bada = k.sb("bada", [5, 6 * D], F32, e2)
            modsb = k.sb("modsb", [5, 6 * D], F32, e2)
            for l in range(layers):
                k.dma("sp", bada[:], IN("b_ada")[l:l + 1, :].to_broadcast([5, 6 * D]), writes=[bada])
                for ct in range(12):
                    wa = was[ct % 2]
                    pm = pms[ct % 2]
                    k.dma("pool", wa[:], IN("w_ada")[l][:, ct * 512:(ct + 1) * 512].rearrange("(kc p) n -> p kc n", p=128), writes=[wa])

                    def f(pe, wa=wa, pm=pm):
                        for kc in range(8):
                            ins = pe.matmul(pm[0:5, :], sT[:, kc, :], wa[:, kc, :], start=(kc == 0), stop=(kc == 7))
                        return ins
                    k.op("pe", f, reads=[sT, wa], writes=[pm])
                    k.op("dve", lambda v, pm=pm, ct=ct: v.tensor_tensor(out=modsb[0:5, ct * 512:(ct + 1) * 512], in0=pm[0:5, :],
                                                                     in1=bada[0:5, ct * 512:(ct + 1) * 512], op=ALU.add),
                         reads=[pm, bada], writes=[modsb])
                k.dma("sp", modD.t.ap()[l], modsb[:], reads=[modsb], writes=[modD])
            k.barrier()

    prologue()
    if "mod" in dbg:
        o = dbg_out("mod", [DEPTH, 5, 6 * D])
        k.dma("sp", o.ap(), modD.t.ap(), reads=[modD])

    def mod_bc(dst, l, r, m_):
        k.dma("sp", dst[:], modD.t.ap()[l, r:r + 1, m_ * D:(m_ + 1) * D].to_broadcast([128, D]), reads=[modD], writes=[dst])

    def norm_mod(xt, A1, A0, tmp, hout, st, heng="pool"):
        ss, sd, rs = st
        k.op("act", lambda a: a.activation(out=tmp[:], in_=xt[:], func=AF.Square, accum_out=ss[:, 0:1]), reads=[xt], writes=[tmp, ss])
        k.op("act", lambda a: a.activation(out=sd[:], in_=ss[:], func=AF.Sqrt, scale=1.0 / D, bias=eps6[:, 0:1]), reads=[ss, eps6], writes=[sd])
        k.op("dve", lambda v: v.reciprocal(out=rs[:], in_=sd[:]), reads=[sd], writes=[rs])
        k.op("dve", lambda v: v.scalar_tensor_tensor(out=tmp[:], in0=xt[:], scalar=rs[:, 0:1], in1=A1[:], op0=ALU.mult, op1=ALU.mult),
             reads=[xt, rs, A1], writes=[tmp])
        k.op(heng, lambda g: g.tensor_tensor(out=hout[:], in0=tmp[:], in1=A0[:], op=ALU.add), reads=[tmp, A0], writes=[hout])

    def load_mods(e2, l, r, mi_scale, mi_shift, gname):
        A1 = k.sb("A1", [128, D], F32, e2)
        A0 = k.sb("A0", [128, D], F32, e2)
        gt = k.sb("gt", [128, D], F32, e2)
        mod_bc(A1, l, r, mi_scale)
        mod_bc(A0, l, r, mi_shift)
        k.dma("sp", gt[:], IN(gname)[l:l + 1, :].to_broadcast([128, D]), writes=[gt])
        k.op("dve", lambda v: v.scalar_tensor_tensor(out=A1[:], in0=A1[:], scalar=1.0, in1=gt[:], op0=ALU.add, op1=ALU.mult),
             reads=[A1, gt], writes=[A1])
        return A1, A0

    def tok_row(j, ti):
        col, isctx = TT[ti]
        return j * TOK + (ti * 128 if isctx else LC + (ti - 2) * 128)

    def stage1(l, j):
        with ExitStack() as e2:
            A1, A0 = load_mods(e2, l, j, 1, 0, "g_mix")
            A1c, A0c = load_mods(e2, l, 4, 1, 0, "g_mix")
            xts = [k.sb("xt", [128, D], F32, e2) for _ in range(2)]
            hbs = [k.sb("hb", [128, D], BF16, e2) for _ in range(2)]
            tmp = k.sb("tmp", [128, D], F32, e2)
            sts = [[k.sb("st", [128, 1], F32, e2) for _ in range(3)] for _ in range(2)]
            psts = [k.ps("pst", [128, 8, 128], BF16, e2) for _ in range(2)]
            for ti, (col, isctx) in enumerate(TT):
                xt, hb, st, pst = xts[ti % 2], hbs[ti % 2], sts[ti % 2], psts[ti % 2]
                r0 = tok_row(j, ti)
                k.dma("sp", xt[:], xres.t.ap()[r0:r0 + 128, :], reads=[xres_b[(j, ti)]], writes=[xt])
                norm_mod(xt, A1c if isctx else A1, A0c if isctx else A0, tmp, hb, st)

                def f(pe, hb=hb, pst=pst):
                    for kc in range(8):
                        ins = pe.transpose(pst[:, kc, :], hb[:, kc * 128:(kc + 1) * 128], ident_b[:, :])
                    return ins
                k.op("pe", f, reads=[hb, ident_b], writes=[pst])
                k.op("act", lambda a, pst=pst, col=col: a.copy(hT[:, :, col:col + 128], pst[:, :, :]), reads=[pst], writes=[hT])
            k.barrier()

    def proj(wg, wcol0, pp, evac):
        for (c0, n) in FULL_TILES:
            ps = rot(pp)

            def f(pe, ps=ps, c0=c0, n=n):
                for kc in range(8):
                    ins = pe.matmul(ps[:, 0:n], wg[:, kc, wcol0:wcol0 + 128], hT[:, kc, c0:c0 + n], start=(kc == 0), stop=(kc == 7))
                return ins
            k.op("pe", f, reads=[wg, hT], writes=[ps])
            evac(ps, c0, n)

    def load_w_in(e2, l, c_lo, c_n, nm="wg"):
        wg = k.sb(nm, [128, 8, c_n], BF16, e2)
        k.dma("pool", wg[:], IN("w_in")[l][:, c_lo:c_lo + c_n].rearrange("(kc p) n -> p kc n", p=128), writes=[wg])
        return wg

    def mixer_a(l, j):
        with ExitStack() as e2:
            wg = load_w_in(e2, l, 0, 512)
            pp = [k.ps("ppa", [128, 512], F32, e2) for _ in range(4)]
            pq = [k.ps("pqa", [128, 512], F32, e2) for _ in range(4)]
            xr = [k.sb("xr", [128, PW], BF16, e2) for _ in range(2)]
            xg = [k.sb("xg", [128, PW], F32, e2) for _ in range(2)]
            hsum = [k.sb("hsum", [128, PW], F32, e2) for _ in range(2)]
            hrev = k.sb("hrev", [128, PW], F32, e2)
            vA = k.sb("vA", [128, 32], F32, e2)
            k.dma("sp", vA[:], IN("vecA")[l], writes=[vA])
            wblk = k.sb("wblk", [128, 8, 128], BF16, e2)
            k.dma("pool", wblk[:], IN("wblkA")[l], writes=[wblk])
            lam4 = k.sb("lam4", [128, 4], F32, e2)
            c1 = k.sb("c1", [128, 4], F32, e2)
            c2 = k.sb("c2", [128, 4], F32, e2)
            k.op("act", lambda a: a.activation(out=lam4[:], in_=vA[:, 7::8], func=AF.Exp, scale=-1.0), reads=[vA], writes=[lam4])
            k.op("act", lambda a: a.activation(out=lam4[:], in_=lam4[:], func=AF.Ln, bias=one1[:, 0:1]), reads=[lam4, one1], writes=[lam4])
            k.op("dve", lambda v: v.tensor_scalar(out=c1[:], in0=lam4[:], scalar1=-8.0, scalar2=None, op0=ALU.mult), reads=[lam4], writes=[c1])
            k.op("dve", lambda v: v.tensor_scalar(out=c2[:], in0=lam4[:], scalar1=-16.0, scalar2=None, op0=ALU.mult), reads=[lam4], writes=[c2])
            if os.environ.get('KSTOP') == '0a':
                k.barrier(); return
            dg = k.sb("dgA", [128, 16, 128], BF16, e2)
            for dc in range(4):
                for kk in range(4):
                    k.op("dve", lambda v, dc=dc, kk=kk: v.tensor_scalar(out=dg[:, dc * 4 + kk, :], in0=ident_f[:], scalar1=vA[:, dc * 8 + kk:dc * 8 + kk + 1],
                                                                        scalar2=None, op0=ALU.mult), reads=[ident_f, vA], writes=[dg])
            if os.environ.get('KSTOP') == '0b':
                k.barrier(); return
            for oc in range(4):
                if oc < 2:
                    proj(wg, oc * 128, pp, lambda ps, c0, n, oc=oc: k.op("act", lambda a: a.copy(xr[oc][:, c0:c0 + n], ps[:, 0:n]), reads=[ps], writes=[xr[oc]]))
                else:
                    proj(wg, oc * 128, pp, lambda ps, c0, n, oc=oc: k.op("dve", lambda v: v.tensor_copy(xg[oc - 2][:, c0:c0 + n], ps[:, 0:n]), reads=[ps], writes=[xg[oc - 2]]))
            if os.environ.get('KSTOP') == '1':
                k.barrier(); return
            tl = [k.sb("tA%d" % i, [128, 512], F32, e2) for i in range(12)]
            ubs = [k.sb("ubA", [128, 512], BF16, e2) for _ in range(2)]
            for c in range(2):
                for d in range(2):
                    dc = d * 2 + c
                    hb = hsum[c] if d == 0 else hrev
                    order = [0, 1, 2, 3, 4] if d == 0 else [0, 4, 3, 2, 1]
                    for oi, vi in enumerate(order):
                        c0, n = VT[vi]
                        half = (oi % 2) * 6
                        uf, r_, i_, a_, e_, iu = tl[half:half + 6]
                        ub = ubs[oi % 2]
                        pu = rot(pq)

                        def fconv(pe, pu=pu, c0=c0, n=n, dc=dc, d=d, c=c):
                            for kk in range(4):
                                off = kk - 3 if d == 0 else kk
                                ins = pe.matmul(pu[:, 0:n], dg[:, dc * 4 + kk, :], xr[c][:, c0 + off:c0 + off + n], start=(kk == 0), stop=(kk == 3))
                            return ins
                        k.op("pe", fconv, reads=[dg, xr[c]], writes=[pu])
                        if os.environ.get('KSTOP') == '15':
                            k.barrier(); return
                        cb = vA[:, dc * 8 + 4:dc * 8 + 5]
                        k.op("act", lambda a, pu=pu, n=n, uf=uf, cb=cb: a.activation(out=uf[:, 0:n], in_=pu[:, 0:n], func=AF.Identity, bias=cb), reads=[pu, vA], writes=[uf])
                        if os.environ.get('KSTOP') == '16':
                            k.barrier(); return
                        k.op("dve", lambda v, uf=uf, n=n, ub=ub: v.tensor_copy(ub[:, 0:n], uf[:, 0:n]), reads=[uf], writes=[ub])
                        if os.environ.get('KSTOP') == '2':
                            k.barrier(); return
                        pr = rot(pq)
                        pi = rot(pq)
                        k.op("pe", lambda pe, pr=pr, n=n, ub=ub, d=d, c=c: pe.matmul(pr[:, 0:n], wblk[:, (d * 2 + 0) * 2 + c, :], ub[:, 0:n], start=True, stop=True), reads=[wblk, ub], writes=[pr])
                        k.op("pe", lambda pe, pi=pi, n=n, ub=ub, d=d, c=c: pe.matmul(pi[:, 0:n], wblk[:, (d * 2 + 1) * 2 + c, :], ub[:, 0:n], start=True, stop=True), reads=[wblk, ub], writes=[pi])
                        k.op("act", lambda a, pr=pr, n=n, r_=r_, dc=dc: a.activation(out=r_[:, 0:n], in_=pr[:, 0:n], func=AF.Sigmoid, bias=vA[:, dc * 8 + 5:dc * 8 + 6]), reads=[pr, vA], writes=[r_])
                        k.op("act", lambda a, pi=pi, n=n, i_=i_, dc=dc: a.activation(out=i_[:, 0:n], in_=pi[:, 0:n], func=AF.Sigmoid, bias=vA[:, dc * 8 + 6:dc * 8 + 7]), reads=[pi, vA], writes=[i_])
                        k.op("act", lambda a, n=n, r_=r_, a_=a_, dc=dc: a.activation(out=a_[:, 0:n], in_=r_[:, 0:n], func=AF.Exp, scale=c1[:, dc:dc + 1]), reads=[r_, c1], writes=[a_])
                        k.op("act", lambda a, n=n, r_=r_, e_=e_, dc=dc: a.activation(out=e_[:, 0:n], in_=r_[:, 0:n], func=AF.Exp, scale=c2[:, dc:dc + 1]), reads=[r_, c2], writes=[e_])
                        k.op("dve", lambda v, n=n, e_=e_: v.tensor_scalar(out=e_[:, 0:n], in0=e_[:, 0:n], scalar1=1.0, scalar2=-1.0, op0=ALU.min, op1=ALU.mult), reads=[e_], writes=[e_])
                        k.op("act", lambda a, n=n, e_=e_: a.activation(out=e_[:, 0:n], in_=e_[:, 0:n], func=AF.Sqrt, bias=one1[:, 0:1]), reads=[e_, one1], writes=[e_])
                        k.op("pool", lambda g, n=n, i_=i_, uf=uf, iu=iu: g.tensor_tensor(out=iu[:, 0:n], in0=i_[:, 0:n], in1=uf[:, 0:n], op=ALU.mult), reads=[i_, uf], writes=[iu])
                        k.op("dve", lambda v, n=n, e_=e_, iu=iu: v.tensor_tensor(out=iu[:, 0:n], in0=iu[:, 0:n], in1=e_[:, 0:n], op=ALU.mult), reads=[iu, e_], writes=[iu])
                        if os.environ.get('KSTOP') == '3':
                            k.barrier(); return
                        if vi == 0:
                            init = 0.0
                        elif d == 0:
                            init = hb[:, c0 - 1:c0] if vi > 1 else hb[:, C0 + LC - 1:C0 + LC]
                        else:
                            init = hb[:, c0 + n:c0 + n + 1] if vi < 4 else hb[:, C0:C0 + 1]
                        if d == 0:
                            k.op("dve", lambda v, n=n, c0=c0, a_=a_, iu=iu, hb=hb, init=init: v.tensor_tensor_scan(
                                out=hb[:, c0:c0 + n], data0=a_[:, 0:n], data1=iu[:, 0:n], initial=init, op0=ALU.mult, op1=ALU.add), reads=[a_, iu, hb], writes=[hb])
                        else:
                            k.op("dve", lambda v, n=n, c0=c0, a_=a_, iu=iu, hb=hb, init=init: v.tensor_tensor_scan(
                                out=hb[:, c0:c0 + n][:, ::-1], data0=a_[:, 0:n][:, ::-1], data1=iu[:, 0:n][:, ::-1], initial=init, op0=ALU.mult, op1=ALU.add),
                                reads=[a_, iu, hb], writes=[hb])
                if os.environ.get('KSTOP') == '4':
                    k.barrier(); return
                for (c0, n) in VT:
                    k.op("act", lambda a, c=c, c0=c0, n=n: a.activation(out=xg[c][:, c0:c0 + n], in_=xg[c][:, c0:c0 + n], func=AF.Gelu_apprx_tanh), reads=[xg[c]], writes=[xg[c]])
                    k.op("pool", lambda g, c=c, c0=c0, n=n: g.tensor_tensor(out=hsum[c][:, c0:c0 + n], in0=hsum[c][:, c0:c0 + n], in1=hrev[:, c0:c0 + n], op=ALU.add), reads=[hsum[c], hrev], writes=[hsum[c]])
                    k.op("dve", lambda v, c=c, c0=c0, n=n: v.tensor_tensor(out=yT[:, c, c0:c0 + n], in0=hsum[c][:, c0:c0 + n], in1=xg[c][:, c0:c0 + n], op=ALU.mult), reads=[hsum[c], xg[c]], writes=[yT])
            k.barrier()
    T.stage1, T.mixer_a = stage1, mixer_a

    def mixer_c(l, j):
        with ExitStack() as e2:
            wg = load_w_in(e2, l, 1280, 512)
            pp = [k.ps("ppc", [128, 512], F32, e2) for _ in range(8)]
            vC = k.sb("vC", [128, 2, 35], F32, e2)
            k.dma("sp", vC[:], IN("vecC")[l], writes=[vC])
            wpw = k.sb("wpw", [128, 2, 256], BF16, e2)
            k.dma("pool", wpw[:], IN("c_w_pw")[l].rearrange("(ic p) j -> p ic j", p=128), writes=[wpw])
            dg = k.sb("dgC", [128, 62, 128], BF16, e2)
            for c in range(2):
                for kk in range(31):
                    k.op("dve", lambda v, c=c, kk=kk: v.tensor_scalar(out=dg[:, c * 31 + kk, :], in0=ident_f[:], scalar1=vC[:, c, kk:kk + 1], scalar2=None, op0=ALU.mult),
                         reads=[ident_f, vC], writes=[dg])
            ub = [k.sb("ubC", [128, PW], BF16, e2) for _ in range(2)]
            sgt = [k.sb("sgt", [128, 512], F32, e2) for _ in range(2)]
            for c in range(2):
                for ti, (c0, n) in enumerate(FULL_TILES):
                    pv, pg = rot(pp), rot(pp)

                    def f(pe, pv=pv, pg=pg, c0=c0, n=n, c=c):
                        for kc in range(8):
                            pe.matmul(pv[:, 0:n], wg[:, kc, c * 128:c * 128 + 128], hT[:, kc, c0:c0 + n], start=(kc == 0), stop=(kc == 7))
                        for kc in range(8):
                            ins = pe.matmul(pg[:, 0:n], wg[:, kc, 256 + c * 128:256 + c * 128 + 128], hT[:, kc, c0:c0 + n], start=(kc == 0), stop=(kc == 7))
                        return ins
                    k.op("pe", f, reads=[wg, hT], writes=[pv, pg])
                    sg = sgt[ti % 2]
                    k.op("act", lambda a, pg=pg, n=n, sg=sg: a.activation(out=sg[:, 0:n], in_=pg[:, 0:n], func=AF.Sigmoid), reads=[pg], writes=[sg])
                    k.op("dve", lambda v, pv=pv, n=n, sg=sg, c=c, c0=c0: v.tensor_tensor(out=ub[c][:, c0:c0 + n], in0=pv[:, 0:n], in1=sg[:, 0:n], op=ALU.mult), reads=[pv, sg], writes=[ub[c]])
            tl = [k.sb("tC%d" % i, [128, 512], F32, e2) for i in range(10)]
            slb = [k.sb("slC", [128, 512], BF16, e2) for _ in range(2)]
            for (c0, n) in VT:
                cv = tl[0:2]
                sq = tl[2:4]
                mean, msq, var, t1 = tl[4:8]
                for c in range(2):
                    pc = rot(pp)

                    def f(pe, pc=pc, c0=c0, n=n, c=c):
                        for kk in range(31):
                            ins = pe.matmul(pc[:, 0:n], dg[:, c * 31 + kk, :], ub[c][:, c0 + kk - 15:c0 + kk - 15 + n], start=(kk == 0), stop=(kk == 30))
                        return ins
                    k.op("pe", f, reads=[dg, ub[c]], writes=[pc])
                    k.op("act", lambda a, pc=pc, n=n, c=c: a.activation(out=cv[c][:, 0:n], in_=pc[:, 0:n], func=AF.Identity, bias=vC[:, c, 31:32]), reads=[pc, vC], writes=[cv[c]])
                    k.op("pool", lambda g, n=n, c=c: g.tensor_tensor(out=sq[c][:, 0:n], in0=cv[c][:, 0:n], in1=cv[c][:, 0:n], op=ALU.mult), reads=[cv[c]], writes=[sq[c]])
                p1, p2 = rot(pp), rot(pp)

                def f(pe, p1=p1, p2=p2, n=n):
                    pe.matmul(p1[:, 0:n], ones_f[:, :], cv[0][:, 0:n], start=True, stop=False)
                    pe.matmul(p1[:, 0:n], ones_f[:, :], cv[1][:, 0:n], start=False, stop=True)
                    pe.matmul(p2[:, 0:n], ones_f[:, :], sq[0][:, 0:n], start=True, stop=False)
                    return pe.matmul(p2[:, 0:n], ones_f[:, :], sq[1][:, 0:n], start=False, stop=True)
                k.op("pe", f, reads=[ones_f, cv[0], cv[1], sq[0], sq[1]], writes=[p1, p2])
                k.op("act", lambda a, p1=p1, n=n: a.activation(out=mean[:, 0:n], in_=p1[:, 0:n], func=AF.Copy, scale=1.0 / 256), reads=[p1], writes=[mean])
                k.op("pool", lambda g, n=n: g.tensor_tensor(out=msq[:, 0:n], in0=mean[:, 0:n], in1=mean[:, 0:n], op=ALU.mult), reads=[mean], writes=[msq])
                k.op("dve", lambda v, p2=p2, n=n: v.scalar_tensor_tensor(out=var[:, 0:n], in0=p2[:, 0:n], scalar=1.0 / 256, in1=msq[:, 0:n], op0=ALU.mult, op1=ALU.subtract), reads=[p2, msq], writes=[var])
                k.op("act", lambda a, n=n: a.activation(out=var[:, 0:n], in_=var[:, 0:n], func=AF.Sqrt, bias=eps5[:, 0:1]), reads=[var, eps5], writes=[var])
                k.op("dve", lambda v, n=n: v.reciprocal(out=var[:, 0:n], in_=var[:, 0:n]), reads=[var], writes=[var])
                for c in range(2):
                    k.op("dve", lambda v, n=n, c=c: v.tensor_tensor(out=t1[:, 0:n], in0=cv[c][:, 0:n], in1=mean[:, 0:n], op=ALU.subtract), reads=[cv[c], mean], writes=[t1])
                    k.op("pool", lambda g, n=n: g.tensor_tensor(out=t1[:, 0:n], in0=t1[:, 0:n], in1=var[:, 0:n], op=ALU.mult), reads=[t1, var], writes=[t1])
                    k.op("act", lambda a, n=n, c=c: a.activation(out=slb[c][:, 0:n], in_=t1[:, 0:n], func=AF.Silu, scale=vC[:, c, 32:33], bias=vC[:, c, 33:34]), reads=[t1, vC], writes=[slb[c]])
                for jc in range(2):
                    po = rot(pp)

                    def f(pe, po=po, n=n, jc=jc):
                        pe.matmul(po[:, 0:n], wpw[:, 0, jc * 128:jc * 128 + 128], slb[0][:, 0:n], start=True, stop=False)
                        return pe.matmul(po[:, 0:n], wpw[:, 1, jc * 128:jc * 128 + 128], slb[1][:, 0:n], start=False, stop=True)
                    k.op("pe", f, reads=[wpw, slb[0], slb[1]], writes=[po])
                    k.op("act", lambda a, po=po, n=n, jc=jc, c0=c0: a.activation(out=yT[:, 4 + jc, c0:c0 + n], in_=po[:, 0:n], func=AF.Identity, bias=vC[:, jc, 34:35]), reads=[po, vC], writes=[yT])
            k.barrier()

    def wout_phase(l, j, last):
        with ExitStack() as e2:
            wo = k.sb("wo", [128, 8, D], BF16, e2)
            k.dma("pool", wo[:], IN("w_out")[l].rearrange("(kc p) n -> p kc n", p=128), writes=[wo])
            A2 = k.sb("A2", [128, D], F32, e2)
            A2c = k.sb("A2c", [128, D], F32, e2)
            mod_bc(A2, l, j, 2)
            mod_bc(A2c, l, 4, 2)
            xts = [k.sb("xtw", [128, D], F32, e2) for _ in range(2)]
            tws = [k.sb("tw", [128, D], F32, e2) for _ in range(2)]
            pps = [k.ps("ppw", [128, D], F32, e2) for _ in range(2)]
            tl_ = [ti for ti, (col, isctx) in enumerate(TT) if not (last and isctx)]

            def ldw(i_):
                ti_ = tl_[i_]
                r0_ = tok_row(j, ti_)
                k.dma("sp", xts[ti_ % 2][:], xres.t.ap()[r0_:r0_ + 128, :], reads=[xres_b[(j, ti_)]], writes=[xts[ti_ % 2]])
            ldw(0)
            for i_w, ti in enumerate(tl_):
                col, isctx = TT[ti]
                if i_w + 1 < len(tl_):
                    ldw(i_w + 1)
                xt, tw, pw = xts[ti % 2], tws[ti % 2], pps[ti % 2]
                r0 = tok_row(j, ti)

                def f(pe, pw=pw, col=col):
                    for nh in range(2):
                        for kc in range(8):
                            ins = pe.matmul(pw[:, nh * 512:(nh + 1) * 512], yT[:, kc, col:col + 128], wo[:, kc, nh * 512:(nh + 1) * 512], start=(kc == 0), stop=(kc == 7))
                    return ins
                k.op("pe", f, reads=[yT, wo], writes=[pw])
                Ax = A2c if isctx else A2
                k.op("dve", lambda v, pw=pw, tw=tw, Ax=Ax: v.tensor_tensor(out=tw[:], in0=pw[:, :], in1=Ax[:], op=ALU.mult), reads=[pw, Ax], writes=[tw])
                k.op("pool", lambda g, tw=tw, xt=xt: g.tensor_tensor(out=tw[:], in0=tw[:], in1=xt[:], op=ALU.add), reads=[tw, xt], writes=[tw])
                k.dma("sp", xres.t.ap()[r0:r0 + 128, :], tw[:], reads=[tw], writes=[xres_b[(j, ti)]])
            k.barrier()

    def mixer_b(l, j):
        lam_init = 0.8 - 0.6 * math.exp(-0.3 * l)
        scale = 32.0 ** -0.5
        with ExitStack() as e2:
            qk = [k.sb("qk", [128, PW], BF16, e2) for _ in range(4)]
            va = k.sb("va", [128, 18, 4, 128], BF16, e2)
            k.op("pool", lambda g: g.memset(va[:], 1.0), writes=[va])
            neglam = k.sb("neglam", [128, 1], F32, e2)
            sgv = k.sb("sgv", [128, 1], F32, e2)
            with ExitStack() as e3:
                wg = load_w_in(e3, l, 512, 768)
                rotb = k.sb("rotb", [128, 128], BF16, e3)
                k.dma("sp", rotb[:], IN("rot_b"), writes=[rotb])
                cosT = k.sb("cosT", [128, PW], F32, e3)
                sinT = k.sb("sinT", [128, PW], F32, e3)
                k.dma("sp", cosT[:], IN("rope_cos"), writes=[cosT])
                k.dma("sp", sinT[:], IN("rope_sin"), writes=[sinT])
                pp = [k.ps("ppb", [128, 512], F32, e3) for _ in range(8)]
                lq = k.sb("lq", [128, 4, 32], F32, e3)
                for i_, nme in enumerate(("b_lq1", "b_lk1", "b_lq2", "b_lk2")):
                    k.dma("sp", lq[:, i_, :], IN(nme)[l:l + 1, :].to_broadcast([128, 32]), writes=[lq])
                s12 = k.sb("s12", [128, 2], F32, e3)
                pr_ = k.sb("pr_", [128, 32], F32, e3)
                for i_ in range(2):
                    k.op("dve", lambda v, i_=i_: v.tensor_tensor(out=pr_[:], in0=lq[:, 2 * i_, :], in1=lq[:, 2 * i_ + 1, :], op=ALU.mult), reads=[lq], writes=[pr_])
                    k.op("dve", lambda v, i_=i_: v.reduce_sum(out=s12[:, i_:i_ + 1], in_=pr_[:], axis=AX.X), reads=[pr_], writes=[s12])
                k.op("act", lambda a: a.activation(out=s12[:], in_=s12[:], func=AF.Exp), reads=[s12], writes=[s12])
                k.op("dve", lambda v: v.tensor_tensor(out=neglam[:], in0=s12[:, 1:2], in1=s12[:, 0:1], op=ALU.subtract), reads=[s12], writes=[neglam])
                k.op("dve", lambda v: v.tensor_scalar(out=neglam[:], in0=neglam[:], scalar1=-lam_init, scalar2=None, op0=ALU.add), reads=[neglam], writes=[neglam])
                k.dma("sp", sgv[:], IN("vecB")[l], writes=[sgv])
                k.op("dve", lambda v: v.tensor_scalar(out=sgv[:], in0=sgv[:], scalar1=1.0 - lam_init, scalar2=None, op0=ALU.mult), reads=[sgv], writes=[sgv])
                if os.environ.get('KSTOP') == 'ba':
                    k.barrier(); return
                qbs = [k.sb("qb", [128, 512], BF16, e3) for _ in range(2)]
                t1s = [k.sb("t1b", [128, 512], F32, e3) for _ in range(2)]
                t2s = [k.sb("t2b", [128, 512], F32, e3) for _ in range(2)]
                cnt = 0
                for oc in range(4):
                    for (c0, n) in FULL_TILES:
                        ps, p2 = rot(pp), rot(pp)
                        qb, t1, t2 = qbs[cnt % 2], t1s[cnt % 2], t2s[cnt % 2]
                        cnt += 1

                        def f(pe, ps=ps, c0=c0, n=n, oc=oc):
                            for kc in range(8):
                                ins = pe.matmul(ps[:, 0:n], wg[:, kc, oc * 128:oc * 128 + 128], hT[:, kc, c0:c0 + n], start=(kc == 0), stop=(kc == 7))
                            return ins
                        k.op("pe", f, reads=[wg, hT], writes=[ps])
                        k.op("act", lambda a, ps=ps, n=n, qb=qb: a.copy(qb[:, 0:n], ps[:, 0:n]), reads=[ps], writes=[qb])
                        k.op("dve", lambda v, ps=ps, n=n, t1=t1, c0=c0: v.tensor_tensor(out=t1[:, 0:n], in0=ps[:, 0:n], in1=cosT[:, c0:c0 + n], op=ALU.mult), reads=[ps, cosT, qb], writes=[t1])
                        k.op("pe", lambda pe, p2=p2, n=n, qb=qb: pe.matmul(p2[:, 0:n], rotb[:, :], qb[:, 0:n], start=True, stop=True), reads=[rotb, qb], writes=[p2])
                        k.op("dve", lambda v, p2=p2, n=n, t2=t2, c0=c0: v.tensor_tensor(out=t2[:, 0:n], in0=p2[:, 0:n], in1=sinT[:, c0:c0 + n], op=ALU.mult), reads=[p2, sinT], writes=[t2])
                        k.op("dve" if os.environ.get("KPOOL") else "pool", lambda g, n=n, t1=t1, t2=t2, oc=oc, c0=c0: g.tensor_tensor(out=qk[oc][:, c0:c0 + n], in0=t1[:, 0:n], in1=t2[:, 0:n], op=ALU.add), reads=[t1, t2], writes=[qk[oc]])
                        if os.environ.get('KSTOP') == 'bb':
                            k.barrier(); return
                if os.environ.get('KSTOP') == 'b0':
                    k.barrier(); return
                for ti, (col, isctx) in enumerate(TT):
                    ps = rot(pp)

                    def f(pe, ps=ps, col=col):
                        for kc in range(8):
                            ins = pe.matmul(ps[:, 0:256], hT[:, kc, col:col + 128], wg[:, kc, 512:768], start=(kc == 0), stop=(kc == 7))
                        return ins
                    k.op("pe", f, reads=[wg, hT], writes=[ps])
                    for h in range(4):
                        dst = va[:, ti, h, 0:64] if h % 2 == 0 else va[:, ti, h, 64:128]
                        k.op("dve", lambda v, ps=ps, h=h, dst=dst: v.tensor_copy(dst, ps[:, h * 64:(h + 1) * 64]), reads=[ps], writes=[va])
                k.barrier()
            if os.environ.get('KSTOP') == 'b1':
                return
            with ExitStack() as e3:
                pS = [k.ps("pS", [128, 1024], F32, e3) for _ in range(2)]
                pO = k.ps("pO", [128, 1024], F32, e3)
                pX = [k.ps("pX", [128, 512], F32, e3) for _ in range(2)]
                Eb = [k.sb("Eb", [128, 1024], BF16, e3) for _ in range(2)]
                ob = [k.sb("ob", [128, PW], F32, e3) for _ in range(2)]
                o1 = k.sb("o1", [128, PW], F32, e3)
                rdt = [k.sb("rdt", [128, 512], F32, e3) for _ in range(2)]
                for h in range(4):
                    ch, hl = h // 2, h % 2
                    nr = slice(0, 64) if hl == 0 else slice(64, 128)
                    dr = slice(64, 128) if hl == 0 else slice(0, 64)
                    for c in range(2):
                        base = hl * 64 + c * 32
                        dest = ob[ch] if c == 0 else o1
                        qq, kk_ = qk[ch], qk[2 + ch]
                        segs = [(L0, 1024, 18), (L0 + 1024, 1024, 18), (C0, 256, 2)]
                        for (q0, qn, nkt) in segs:
                            nsub = (qn + 511) // 512
                            def emit_S(kt, q0=q0, qn=qn, nsub=nsub, base=base, qq=qq, kk_=kk_):
                                kcol = TT[kt][0]
                                ps_ = pS[kt % 2]

                                def f(pe):
                                    for i_ in range(nsub):
                                        w_ = min(512, qn - i_ * 512)
                                        ins = pe.matmul(ps_[:, i_ * 512:i_ * 512 + w_], kk_[base:base + 32, kcol:kcol + 128], qq[base:base + 32, q0 + i_ * 512:q0 + i_ * 512 + w_],
                                                        start=True, stop=True, tile_position=(base, 0))
                                    return ins
                                k.op("pe", f, reads=[qq, kk_], writes=[ps_])

                            def emit_E(kt, qn=qn):
                                ps_, E_ = pS[kt % 2], Eb[kt % 2]
                                k.op("act", lambda a: a.activation(out=E_[:, 0:qn], in_=ps_[:, 0:qn], func=AF.Exp, scale=scale), reads=[ps_], writes=[E_])

                            def emit_PV(kt, nkt=nkt, qn=qn, nsub=nsub, h=h):
                                E_ = Eb[kt % 2]

                                def f2(pe):
                                    for i_ in range(nsub):
                                        w_ = min(512, qn - i_ * 512)
                                        ins = pe.matmul(pO[:, i_ * 512:i_ * 512 + w_], va[:, kt, h, :], E_[:, i_ * 512:i_ * 512 + w_], start=(kt == 0), stop=(kt == nkt - 1))
                                    return ins
                                k.op("pe", f2, reads=[va, E_], writes=[pO])
                            emit_S(0)
                            for kt in range(nkt):
                                emit_E(kt)
                                if kt + 1 < nkt:
                                    emit_S(kt + 1)
                                emit_PV(kt)
                            for i_ in range(nsub):
                                w_ = min(512, qn - i_ * 512)
                                rd = rdt[i_ % 2]
                                k.op("dve", lambda v, rd=rd, i_=i_, w_=w_, dr=dr: v.reciprocal(out=rd[dr, 0:w_], in_=pO[dr, i_ * 512:i_ * 512 + w_]), reads=[pO], writes=[rd])
                                k.op("dve", lambda v, rd=rd, i_=i_, w_=w_, dr=dr, nr=nr, dest=dest, q0=q0: v.tensor_tensor(
                                    out=dest[nr, q0 + i_ * 512:q0 + i_ * 512 + w_], in0=pO[nr, i_ * 512:i_ * 512 + w_], in1=rd[dr, 0:w_], op=ALU.mult), reads=[pO, rd], writes=[dest])
                    for (c0, n) in VT:
                        k.op("dve", lambda v, c0=c0, n=n, nr=nr, ch=ch: v.scalar_tensor_tensor(out=ob[ch][nr, c0:c0 + n], in0=o1[nr, c0:c0 + n], scalar=neglam[nr, 0:1], in1=ob[ch][nr, c0:c0 + n],
                                                                                            op0=ALU.mult, op1=ALU.add), reads=[o1, neglam, ob[ch]], writes=[ob[ch]])
                sqs = [k.sb("sqb", [128, 512], F32, e3) for _ in range(2)]
                for ch in range(2):
                    for vi, (c0, n) in enumerate(VT):
                        sq, px = sqs[vi % 2], pX[vi % 2]
                        k.op("pool", lambda g, sq=sq, c0=c0, n=n, ch=ch: g.tensor_tensor(out=sq[:, 0:n], in0=ob[ch][:, c0:c0 + n], in1=ob[ch][:, c0:c0 + n], op=ALU.mult), reads=[ob[ch]], writes=[sq])
                        k.op("pe", lambda pe, sq=sq, px=px, n=n: pe.matmul(px[:, 0:n], blk64[:, :], sq[:, 0:n], start=True, stop=True), reads=[blk64, sq], writes=[px])
                        k.op("act", lambda a, sq=sq, px=px, n=n: a.activation(out=sq[:, 0:n], in_=px[:, 0:n], func=AF.Sqrt, scale=1.0 / 64, bias=eps6[:, 0:1]), reads=[px, eps6], writes=[sq])
                        k.op("dve", lambda v, sq=sq, n=n: v.reciprocal(out=sq[:, 0:n], in_=sq[:, 0:n]), reads=[sq], writes=[sq])
                        k.op("dve", lambda v, sq=sq, c0=c0, n=n, ch=ch: v.scalar_tensor_tensor(out=yT[:, 2 + ch, c0:c0 + n], in0=ob[ch][:, c0:c0 + n], scalar=sgv[:, 0:1], in1=sq[:, 0:n],
                                                                                         op0=ALU.mult, op1=ALU.mult), reads=[ob[ch], sgv, sq], writes=[yT])
                k.barrier()

    SEGS = {"l": (L0, L, 16), "c": (C0, LC, 2)}

    def hyena_prep(l):
        TWO_PI = 2.0 * math.pi
        for tag, (col0, Lx, nch) in SEGS.items():
            with ExitStack() as e2:
                pp = [k.ps("pph", [128, 512], F32, e2) for _ in range(4)]
                pe_ = [k.ps("ppe", [128, 512], F32, e2) for _ in range(2)]
                feats = k.sb("feats", [33, Lx], F32, e2)
                k.dma("sp", feats[:], IN("feats_" + tag), writes=[feats])
                wf1 = k.sb("wf1", [33, 64], F32, e2)
                wf2 = k.sb("wf2", [64, 64], F32, e2)
                wf3 = k.sb("wf3", [64, 1024], F32, e2)
                k.dma("sp", wf1[:], IN("d_w_f1")[l], writes=[wf1])
                k.dma("sp", wf2[:], IN("d_w_f2")[l], writes=[wf2])
                k.dma("sp", wf3[:], IN("d_w_f3")[l], writes=[wf3])
                hv = k.sb("hv", [64, 3], F32, e2)
                k.dma("sp", hv[:], IN("hyv")[l], writes=[hv])
                fb = k.sb("fb", [64, 2], F32, e2)
                k.op("dve", lambda v: v.tensor_tensor(out=fb[:, 0:1], in0=hv[:, 0:1], in1=hv[:, 1:2], op=ALU.mult), reads=[hv], writes=[fb])
                k.op("dve", lambda v: v.tensor_tensor(out=fb[:, 1:2], in0=hv[:, 2:3], in1=hv[:, 1:2], op=ALU.mult), reads=[hv], writes=[fb])
                ntu = k.sb("ntu", [128, nch], F32, e2)
                k.dma("sp", ntu[:], IN("ntu_" + tag), writes=[ntu])
                dec = k.sb("dec", [128, 1024], F32, e2)
                dec2 = k.sb("dec2", [128, 1024], F32, e2)
                k.dma("sp", dec[:], IN("d_decay")[l:l + 1, :].to_broadcast([128, 1024]), writes=[dec])
                k.op("dve", lambda v: v.tensor_scalar(out=dec2[:], in0=dec[:], scalar1=-1.0, scalar2=None, op0=ALU.mult), reads=[dec], writes=[dec2])
                k.op("dve", lambda v: v.tensor_tensor(out=dec[:], in0=dec[:], in1=dec2[:], op=ALU.max), reads=[dec, dec2], writes=[dec])
                f1 = k.sb("f1", [64, Lx], F32, e2)
                f2 = k.sb("f2", [64, Lx], F32, e2)
                arg = k.sb("arg", [64, 512], F32, e2)
                ki = k.sb("ki", [64, 512], I32, e2)
                kf = k.sb("kf", [64, 512], F32, e2)
                nt = min(512, Lx)
                for (src, w_, dst, bcol, K_) in ((feats, wf1, f1, 0, 33), (f1, wf2, f2, 1, 64)):
                    for t0 in range(0, Lx, nt):
                        ps = rot(pp)
                        k.op("pe", lambda pe, ps=ps, src=src, w_=w_, t0=t0, K_=K_: pe.matmul(ps[0:64, 0:nt], w_[0:K_, :], src[0:K_, t0:t0 + nt], start=True, stop=True), reads=[w_, src], writes=[ps])
                        k.op("act", lambda a, ps=ps, bcol=bcol: a.activation(out=arg[:, 0:nt], in_=ps[0:64, 0:nt], func=AF.Identity, scale=hv[:, 1:2], bias=fb[:, bcol:bcol + 1]), reads=[ps, hv, fb], writes=[arg])
                        k.op("dve", lambda v: v.tensor_scalar(out=ki[:, 0:nt], in0=arg[:, 0:nt], scalar1=1.0 / TWO_PI, scalar2=None, op0=ALU.mult), reads=[arg], writes=[ki])
                        k.op("dve", lambda v: v.tensor_copy(kf[:, 0:nt], ki[:, 0:nt]), reads=[ki], writes=[kf])
                        k.op("dve", lambda v: v.scalar_tensor_tensor(out=arg[:, 0:nt], in0=kf[:, 0:nt], scalar=-TWO_PI, in1=arg[:, 0:nt], op0=ALU.mult, op1=ALU.add), reads=[kf, arg], writes=[arg])
                        k.op("dve", lambda v: v.tensor_scalar(out=arg[:, 0:nt], in0=arg[:, 0:nt], scalar1=3.14159, scalar2=-3.14159, op0=ALU.min, op1=ALU.max), reads=[arg], writes=[arg])
                        k.op("act", lambda a, dst=dst, t0=t0: a.activation(out=dst[:, t0:t0 + nt], in_=arg[:, 0:nt], func=AF.Sin), reads=[arg], writes=[dst])
                hrs = [k.sb("hr", [128, 1024], F32, e2) for _ in range(2)]
                ed = k.sb("ed", [128, 1024], F32, e2)
                sq = k.sb("sqh", [128, 1024], F32, e2)
                hsd = k.sb("hsd", [128, nch, 2, 512], BF16, e2)
                pen = pe_[0]
                for tc in range(nch):
                    hr = hrs[tc % 2]
                    k.op("act", lambda a, tc=tc: a.activation(out=ed[:], in_=dec[:], func=AF.Exp, scale=ntu[:, tc:tc + 1]), reads=[dec, ntu], writes=[ed])
                    for hh in range(2):
                        ps = rot(pp)
                        k.op("pe", lambda pe, ps=ps, tc=tc, hh=hh: pe.matmul(ps[:, :], f2[0:64, tc * 128:(tc + 1) * 128], wf3[0:64, hh * 512:(hh + 1) * 512], start=True, stop=True), reads=[f2, wf3], writes=[ps])
                        k.op("dve", lambda v, ps=ps, hr=hr, hh=hh: v.tensor_tensor(out=hr[:, hh * 512:(hh + 1) * 512], in0=ps[:, :], in1=ed[:, hh * 512:(hh + 1) * 512], op=ALU.mult), reads=[ps, ed], writes=[hr])
                    if tc == 0:
                        for o_ in range(2):
                            k.op("dve", lambda v, o_=o_, hr=hr: v.memset(hr[0:1, o_ * 512 + 256:o_ * 512 + 512], 0.0), reads=[hr], writes=[hr])
                    k.op("pool", lambda g, hr=hr: g.tensor_tensor(out=sq[:], in0=hr[:], in1=hr[:], op=ALU.mult), reads=[hr], writes=[sq])

                    def f(pe, tc=tc):
                        for o_ in range(2):
                            for dd in range(2):
                                ins = pe.matmul(pe_[o_][:, 0:256], ones_f[:, :], sq[:, o_ * 512 + dd * 256:o_ * 512 + dd * 256 + 256],
                                                start=(tc == 0 and dd == 0), stop=(tc == nch - 1 and dd == 1))
                        return ins
                    k.op("pe", f, reads=[ones_f, sq], writes=[pe_[0], pe_[1]])
                    for o_ in range(2):
                        fw_ = hr[:, o_ * 512:o_ * 512 + 256]
                        bw_ = hr[:, o_ * 512 + 256:o_ * 512 + 512]
                        k.op("pool", lambda g, tc=tc, o_=o_, fw_=fw_, bw_=bw_: g.tensor_tensor(out=hsd[:, tc, 0, o_ * 256:(o_ + 1) * 256], in0=fw_, in1=bw_, op=ALU.add), reads=[hr], writes=[hsd])
                        k.op("dve", lambda v, tc=tc, o_=o_, fw_=fw_, bw_=bw_: v.tensor_tensor(out=hsd[:, tc, 1, o_ * 256:(o_ + 1) * 256], in0=bw_, in1=fw_, op=ALU.subtract), reads=[hr], writes=[hsd])
                rn = k.sb("rn", [128, 512], F32, e2)
                for o_ in range(2):
                    k.op("act", lambda a, o_=o_: a.activation(out=rn[:, o_ * 256:(o_ + 1) * 256], in_=pe_[o_][:, 0:256], func=AF.Sqrt, bias=eps6[:, 0:1]), reads=[pe_[o_], eps6], writes=[rn])
                k.op("dve", lambda v: v.reciprocal(out=rn[:], in_=rn[:]), reads=[rn], writes=[rn])
                cfs = [k.sb("cfp", [128, nch, 128], BF16, e2) for _ in range(2)]
                sfs = [k.sb("sfp", [128, nch, 128], BF16, e2) for _ in range(2)]
                hks = [k.sb("hk", [128, 2, 512], F32, e2) for _ in range(2)]
                for fc in range(nch):
                    cf, sf, hk = cfs[fc % 2], sfs[fc % 2], hks[fc % 2]
                    k.dma("sp", cf[:], IN("cf_" + tag)[fc], writes=[cf])
                    k.dma("sp", sf[:], IN("sf_" + tag)[fc], writes=[sf])
                    pr_, pi_ = rot(pp), rot(pp)

                    def f(pe, cf=cf, sf=sf, pr_=pr_, pi_=pi_):
                        for tc in range(nch):
                            pe.matmul(pr_[:, :], cf[:, tc, :], hsd[:, tc, 0, :], start=(tc == 0), stop=(tc == nch - 1))
                        for tc in range(nch):
                            ins = pe.matmul(pi_[:, :], sf[:, tc, :], hsd[:, tc, 1, :], start=(tc == 0), stop=(tc == nch - 1))
                        return ins
                    k.op("pe", f, reads=[cf, sf, hsd], writes=[pr_, pi_])
                    k.op("dve", lambda v, hk=hk, pr_=pr_: v.tensor_tensor(out=hk[:, 0, :], in0=pr_[:, :], in1=rn[:], op=ALU.mult), reads=[pr_, rn], writes=[hk])
                    k.op("dve", lambda v, hk=hk, pi_=pi_: v.tensor_tensor(out=hk[:, 1, :], in0=pi_[:, :], in1=rn[:], op=ALU.mult), reads=[pi_, rn], writes=[hk])
                    k.dma("sp", Hd[tag].t.ap()[:, fc * 128:(fc + 1) * 128, :].rearrange("r p n -> p r n"), hk[:], reads=[hk], writes=[Hd[tag]])
                k.barrier()

    def mixer_d(l, j):
        with ExitStack() as e2:
            pdb = [k.sb("pdb", [128, PW], BF16, e2) for _ in range(6)]
            z = [k.sb("zD", [128, PW], F32, e2) for _ in range(2)]
            vD = k.sb("vD", [128, 6, 4], F32, e2)
            vD2 = k.sb("vD2", [128, 4], F32, e2)
            k.dma("sp", vD[:], IN("vecD")[l], writes=[vD])
            k.dma("sp", vD2[:], IN("vecD2")[l], writes=[vD2])
            dg = k.sb("dgD", [128, 18, 128], BF16, e2)
            for c in range(6):
                for kk in range(3):
                    k.op("dve", lambda v, c=c, kk=kk: v.tensor_scalar(out=dg[:, c * 3 + kk, :], in0=ident_f[:], scalar1=vD[:, c, kk:kk + 1], scalar2=None, op0=ALU.mult), reads=[ident_f, vD], writes=[dg])
            with ExitStack() as e3:
                wg = load_w_in(e3, l, 1792, 768)
                pp = [k.ps("ppd0", [128, 512], F32, e3) for _ in range(4)]
                for oc in range(6):
                    proj(wg, oc * 128, pp, lambda ps, c0, n, oc=oc: k.op("act" if oc % 2 == 0 else "dve",
                         (lambda a: a.copy(pdb[oc][:, c0:c0 + n], ps[:, 0:n])) if oc % 2 == 0 else (lambda v: v.tensor_copy(pdb[oc][:, c0:c0 + n], ps[:, 0:n])), reads=[ps], writes=[pdb[oc]]))
                k.barrier()
            pp = [k.ps("ppd", [128, 512], F32, e2) for _ in range(6)]
            ptr = [k.ps("ptr", [128, 4, 128], BF16, e2) for _ in range(2)]

            def sconv(c, c0, n, ps):
                def f(pe):
                    for kk in range(3):
                        ins = pe.matmul(ps[:, 0:n], dg[:, c * 3 + kk, :], pdb[c][:, c0 + kk - 1:c0 + kk - 1 + n], start=(kk == 0), stop=(kk == 2))
                    return ins
                k.op("pe", f, reads=[dg, pdb[c]], writes=[ps])
            for c in range(2):
                for (c0, n) in VT:
                    ps = rot(pp)
                    sconv(c, c0, n, ps)
                    k.op("act", lambda a, ps=ps, c=c, c0=c0, n=n: a.activation(out=z[c][:, c0:c0 + n], in_=ps[:, 0:n], func=AF.Identity, bias=vD[:, c, 3:4]), reads=[ps, vD], writes=[z[c]])
            zb = k.sb("zb", [128, 2, L], BF16, e2)
            zT = k.sb("zT", [128, 16, 256], BF16, e2)
            Y = k.sb("Yd", [128, 32, 256], BF16, e2)
            cfs = [k.sb("cfd", [128, 16, 128], BF16, e2) for _ in range(2)]
            sfs = [k.sb("sfd", [128, 16, 128], BF16, e2) for _ in range(2)]
            hks = [k.sb("hkd", [128, 2, 256], F32, e2) for _ in range(2)]
            tms = [k.sb("tmd", [128, 256], F32, e2) for _ in range(4)]
            cis = [k.sb("cid", [128, 4, 512], BF16, e2) for _ in range(2)]
            sis = [k.sb("sid", [128, 4, 512], BF16, e2) for _ in range(2)]
            gts = [k.sb("gtd", [128, 512], F32, e2) for _ in range(2)]
            tts = [k.sb("ttd", [128, 512], F32, e2) for _ in range(2)]
            for o_ in range(2):
                for tag, (col0, Lx, nch) in SEGS.items():
                    for c in range(2):
                        k.op("act", lambda a, c=c, col0=col0, Lx=Lx: a.copy(zb[:, c, 0:Lx], z[c][:, col0:col0 + Lx]), reads=[z[c]], writes=[zb])
                    for tc in range(nch):
                        pt = ptr[tc % 2]

                        def f(pe, pt=pt, tc=tc):
                            for c in range(2):
                                ins = pe.transpose(pt[:, c, :], zb[:, c, tc * 128:(tc + 1) * 128], ident_b[:, :])
                            return ins
                        k.op("pe", f, reads=[zb, ident_b], writes=[pt])
                        k.op("dve", lambda v, pt=pt, tc=tc: v.tensor_copy(zT[:, tc, :], pt[:, 0:2, :]), reads=[pt], writes=[zT])
                    for fc in range(nch):
                        cf, sf, hk = cfs[fc % 2], sfs[fc % 2], hks[fc % 2]
                        k.dma("sp", cf[:, 0:nch, :], IN("cf_" + tag)[fc], writes=[cf])
                        k.dma("sp", sf[:, 0:nch, :], IN("sf_" + tag)[fc], writes=[sf])
                        k.dma("sp", hk[:], Hd[tag].t.ap()[:, fc * 128:(fc + 1) * 128, o_ * 256:(o_ + 1) * 256].rearrange("r p n -> p r n"), reads=[Hd[tag]], writes=[hk])
                        pa, pb = rot(pp), rot(pp)

                        def f(pe, cf=cf, sf=sf, pa=pa, pb=pb, nch=nch):
                            for tc in range(nch):
                                pe.matmul(pa[:, 0:256], cf[:, tc, :], zT[:, tc, :], start=(tc == 0), stop=(tc == nch - 1))
                            for tc in range(nch):
                                ins = pe.matmul(pb[:, 0:256], sf[:, tc, :], zT[:, tc, :], start=(tc == 0), stop=(tc == nch - 1))
                            return ins
                        k.op("pe", f, reads=[cf, sf, zT], writes=[pa, pb])
                        t1, t2, t3, t4 = tms
                        k.op("dve", lambda v, pa=pa, hk=hk: v.tensor_tensor(out=t1[:], in0=pa[:, 0:256], in1=hk[:, 0, :], op=ALU.mult), reads=[pa, hk], writes=[t1])
                        k.op("dve", lambda v, pb=pb, hk=hk: v.tensor_tensor(out=t2[:], in0=pb[:, 0:256], in1=hk[:, 1, :], op=ALU.mult), reads=[pb, hk], writes=[t2])
                        k.op("pool", lambda g, fc=fc: g.tensor_tensor(out=Y[:, fc, :], in0=t1[:], in1=t2[:], op=ALU.add), reads=[t1, t2], writes=[Y])
                        k.op("dve", lambda v, pa=pa, hk=hk: v.tensor_tensor(out=t3[:], in0=pa[:, 0:256], in1=hk[:, 1, :], op=ALU.mult), reads=[pa, hk], writes=[t3])
                        k.op("dve", lambda v, pb=pb, hk=hk: v.tensor_tensor(out=t4[:], in0=pb[:, 0:256], in1=hk[:, 0, :], op=ALU.mult), reads=[pb, hk], writes=[t4])
                        k.op("pool", lambda g, fc=fc, nch=nch: g.tensor_tensor(out=Y[:, nch + fc, :], in0=t3[:], in1=t4[:], op=ALU.subtract), reads=[t3, t4], writes=[Y])
                    nt = min(512, Lx)
                    for ti_, t0 in enumerate(range(0, Lx, nt)):
                        py = [rot(pp), rot(pp)]
                        ngrp = (nch + 3) // 4
                        for g_ in range(ngrp):
                            ci, si = cis[g_ % 2], sis[g_ % 2]
                            nf = min(4, nch - g_ * 4)
                            k.dma("sp", ci[:, 0:nf, 0:nt], IN("ci_" + tag)[:, g_ * 4:g_ * 4 + nf, t0:t0 + nt], writes=[ci])
                            k.dma("sp", si[:, 0:nf, 0:nt], IN("si_" + tag)[:, g_ * 4:g_ * 4 + nf, t0:t0 + nt], writes=[si])

                            def f(pe, ci=ci, si=si, g_=g_, nf=nf, py=py, nch=nch, ngrp=ngrp):
                                for c in range(2):
                                    for ff in range(nf):
                                        fc = g_ * 4 + ff
                                        pe.matmul(py[c][:, 0:nt], Y[:, fc, c * 128:(c + 1) * 128], ci[:, ff, 0:nt], start=(fc == 0), stop=False)
                                        ins = pe.matmul(py[c][:, 0:nt], Y[:, nch + fc, c * 128:(c + 1) * 128], si[:, ff, 0:nt], start=False, stop=(fc == nch - 1))
                                return ins
                            k.op("pe", f, reads=[Y, ci, si], writes=[py[0], py[1]])
                        c0 = col0 + t0
                        for c in range(2):
                            gc = 2 + 2 * o_ + c
                            pg = rot(pp)
                            sconv(gc, c0, nt, pg)
                            gt, tt_ = gts[c], tts[c]
                            k.op("act", lambda a, pg=pg, gt=gt, gc=gc: a.activation(out=gt[:, 0:nt], in_=pg[:, 0:nt], func=AF.Identity, bias=vD[:, gc, 3:4]), reads=[pg, vD], writes=[gt])
                            k.op("dve", lambda v, c=c, tt_=tt_, c0=c0, py=py, o_=o_: v.scalar_tensor_tensor(out=tt_[:, 0:nt], in0=z[c][:, c0:c0 + nt], scalar=vD2[:, o_ * 2 + c:o_ * 2 + c + 1], in1=py[c][:, 0:nt],
                                                                                                op0=ALU.mult, op1=ALU.add), reads=[z[c], vD2, py[c]], writes=[tt_])
                            if o_ == 0:
                                k.op("pool", lambda g, c=c, tt_=tt_, gt=gt, c0=c0: g.tensor_tensor(out=z[c][:, c0:c0 + nt], in0=tt_[:, 0:nt], in1=gt[:, 0:nt], op=ALU.mult), reads=[tt_, gt, zb], writes=[z[c]])
                            else:
                                k.op("pool", lambda g, c=c, tt_=tt_, gt=gt, c0=c0: g.tensor_tensor(out=yT[:, 6 + c, c0:c0 + nt], in0=tt_[:, 0:nt], in1=gt[:, 0:nt], op=ALU.mult), reads=[tt_, gt], writes=[yT])
            k.barrier()

    def ffn_phase(l, last):
        tiles = [(j, ti) for j in range(nseq) for ti in (range(2, 18) if last else range(18))]
        nt = len(tiles)
        NBk = (2 * nt * 128) // MOE_S + NE
        wgT = IN("moe_w_gate").rearrange("l e (p j) n -> (l e p) (j n)", j=8)
        wuT = IN("moe_w_up").rearrange("l e (p j) n -> (l e p) (j n)", j=8)
        wdT = IN("moe_w_down").rearrange("l e (p j) n -> (l e p) (j n)", j=4)
        hrow_b = [Buf(hrow.t) for _ in range(nt)]
        xs_w = [Buf(xs.t) for _ in range(nt)]
        ysl_b = [Buf(yslot.t) for _ in range(NBk)]
        with ExitStack() as e2:
            zt = k.sb("zt", [128, 2 * D], BF16, e2)
            k.op("pool", lambda g: g.memset(zt[:], 0.0), writes=[zt])
            for i_ in range(2 * NBk):
                k.dma("sp", xs.t.ap()[0:NBk * MOE_S, :].rearrange("(p r) d -> p r d", p=128)[:, 2 * i_:2 * i_ + 2, :], zt[:].rearrange("p (r d) -> p r d", r=2), reads=[zt], writes=[xs_z])
            GT = k.sb("GT", [128, nt, 2], F32, e2)
            DST = k.sb("DST", [128, nt, 2], F32, e2)
            DSTi = k.sb("DSTi", [128, nt, 2], I32, e2)
            run = k.sb("run", [128, 32], F32, e2)
            tokid = k.sb("tokid", [128, NB_ * 18], I32, e2)
            iow = k.sb("iow", [128, 16], F32, e2)
            blkS = k.sb("blkS", [128, 80], F32, e2)
            nbi = k.sb("nbi", [128, 32], I32, e2)
            pad_ = k.sb("pad_", [128, 32], F32, e2)
            pend = k.sb("pend", [128, 32], F32, e2)
            pst_ = k.sb("pst_", [128, 32], F32, e2)
            tq = k.sb("tq", [128, 32], F32, e2)
            tq2 = k.sb("tq2", [128, 32], F32, e2)
            bacc = k.sb("bacc", [128, NBk], F32, e2)
            be1 = k.sb("be1", [128, NBk], F32, e2)
            be2 = k.sb("be2", [128, NBk], F32, e2)
            WI = k.sb("WI", [128, NBk], I32, e2)
            zi = k.sb("zi", [128, 512], I32, e2)
            hb2s = [k.sb("hb2", [128, D], BF16, e2) for _ in range(2)]
            eR = ExitStack()
            OH1 = k.sb("OH1", [128, nt, 32], F32, eR)
            OH2 = k.sb("OH2", [128, nt, 32], F32, eR)
            RK = k.sb("RK", [128, nt, 32], F32, eR)
            T32 = k.sb("T32", [128, nt, 32], F32, eR)
            WIf = k.sb("WIf", [128, NBk], F32, eR)
            k.dma("sp", tokid[:], IN("tokid"), writes=[tokid])
            k.dma("sp", iow[:], IN("iota_w"), writes=[iow])
            k.dma("sp", blkS[:], IN("blkS"), writes=[blkS])
            k.op("dve", lambda v: v.memset(run[:], 0.0), writes=[run])
            with ExitStack() as e3:
                A1 = k.sb("A1f", [128, D], F32, e3)
                A0 = k.sb("A0f", [128, D], F32, e3)
                A1c = k.sb("A1cf", [128, D], F32, e3)
                A0c = k.sb("A0cf", [128, D], F32, e3)
                gtl = k.sb("gtf", [128, D], F32, e3)
                k.dma("sp", gtl[:], IN("g_ffn")[l:l + 1, :].to_broadcast([128, D]), writes=[gtl])

                def fill(A1_, A0_, r):
                    mod_bc(A1_, l, r, 4)
                    mod_bc(A0_, l, r, 3)
                    k.op("dve", lambda v: v.scalar_tensor_tensor(out=A1_[:], in0=A1_[:], scalar=1.0, in1=gtl[:], op0=ALU.add, op1=ALU.mult), reads=[A1_, gtl], writes=[A1_])
                if not last:
                    fill(A1c, A0c, 4)
                wr = k.sb("wr", [128, 8, 36], F32, e3)
                k.dma("sp", wr[:], IN("wr")[l].rearrange("(kc p) n -> p kc n", p=128), writes=[wr])
                brb = k.sb("brb", [128, 36], F32, e3)
                k.dma("sp", brb[:], IN("br")[l:l + 1, :].to_broadcast([128, 36]), writes=[brb])
                utri = k.sb("utri", [128, 128], BF16, e3)
                onesb = k.sb("onesb", [128, 128], BF16, e3)
                k.dma("sp", utri[:], IN("utri_b"), writes=[utri])
                k.dma("sp", onesb[:], IN("ones_b"), writes=[onesb])
                xts = [k.sb("xtm", [128, D], F32, e3) for _ in range(2)]
                hfs = [k.sb("hfm", [128, D], F32, e3) for _ in range(1)] * 2
                hbs = [k.sb("hbm", [128, D], BF16, e3) for _ in range(2)]
                hTfs = [k.sb("hTf", [128, 8, 128], F32, e3) for _ in range(2)]
                tmp = k.sb("tmpm", [128, D], F32, e3)
                sts = [[k.sb("stm", [128, 1], F32, e3) for _ in range(3)] for _ in range(2)]
                ptfs = [k.ps("ptf", [128, 8, 128], F32, e3) for _ in range(2)]
                plgs = [k.ps("plg", [128, 512], F32, e3) for _ in range(2)]
                pR1 = k.ps("pR1", [128, 512], F32, e3)
                pR2 = k.ps("pR2", [128, 512], F32, e3)
                LG = k.sb("LG", [128, nt, 36], F32, e3)
                gm = k.sb("gm", [128, 8], F32, e3)
                ohg = k.sb("ohg", [128, 4], F32, e3)
                eg = k.sb("eg", [128, 4], F32, e3)
                les = k.sb("les", [128, 8], F32, e3)
                oh1 = k.sb("oh1", [128, 8], F32, e3)
                msk = k.sb("msk", [128, 8], F32, e3)
                oh2 = k.sb("oh2", [128, 8], F32, e3)
                Mb = k.sb("Mb", [128, 32], BF16, e3)
                curj = None

                def ldA(t_):
                    j_, ti_ = tiles[t_]
                    r0_ = tok_row(j_, ti_)
                    k.dma("sp", xts[t_ % 2][:], xres.t.ap()[r0_:r0_ + 128, :], reads=[xres_b[(j_, ti_)]], writes=[xts[t_ % 2]])
                for t, (j, ti) in enumerate(tiles):
                    col, isctx = TT[ti]
                    if j != curj:
                        fill(A1, A0, j)
                        curj = j
                    xt, hf, hb, hTf, st, ptf, plg = xts[t % 2], hfs[t % 2], hbs[t % 2], hTfs[t % 2], sts[t % 2], ptfs[t % 2], plgs[t % 2]
                    r0 = tok_row(j, ti)
                    if t == 0:
                        ldA(0)
                    if t + 1 < nt:
                        ldA(t + 1)
                    norm_mod(xt, A1c if isctx else A1, A0c if isctx else A0, tmp, hf, st)
                    k.op("dve", lambda v, hf=hf, hb=hb: v.tensor_copy(hb[:], hf[:]), reads=[hf], writes=[hb])
                    k.dma("sp", hrow.t.ap()[r0:r0 + 128, :], hb[:], reads=[hb], writes=[hrow_b[t]])

                    def f(pe, hf=hf, ptf=ptf):
                        for kc in range(8):
                            ins = pe.transpose(ptf[:, kc, :], hf[:, kc * 128:(kc + 1) * 128], ident_f[:, :])
                        return ins
                    k.op("pe", f, reads=[hf, ident_f], writes=[ptf])
                    k.op("dve", lambda v, ptf=ptf, hTf=hTf: v.tensor_copy(hTf[:], ptf[:, :, :]), reads=[ptf], writes=[hTf])

                    def f(pe, hTf=hTf, plg=plg):
                        for kc in range(8):
                            ins = pe.matmul(plg[:, 0:36], hTf[:, kc, :], wr[:, kc, :], start=(kc == 0), stop=(kc == 7))
                        return ins
                    k.op("pe", f, reads=[hTf, wr], writes=[plg])
                    V = lambda fn, rd, wrr: k.op("dve", fn, reads=rd, writes=wrr)
                    V(lambda v, plg=plg, t=t: v.tensor_tensor(out=LG[:, t, :], in0=plg[:, 0:36], in1=brb[:], op=ALU.add), [plg, brb], [LG])
                S1_ = lambda nm: k.sb(nm, [128, nt, 1], F32, e3)
                GMx, SGs, PGs, M1s, M2s, DEs, P1s = [S1_("r1_%d" % i_) for i_ in range(7)]
                OHG = k.sb("OHG", [128, nt, 4], F32, e3)
                EG = k.sb("EG", [128, nt, 4], F32, e3)
                LES = k.sb("LES", [128, nt, 8], F32, e3)
                T8 = k.sb("T8", [128, nt, 8], F32, e3)
                O1s = k.sb("O1s", [128, nt, 8], F32, e3)
                MSK = k.sb("MSK", [128, nt, 8], F32, e3)
                O2s = k.sb("O2s", [128, nt, 8], F32, e3)
                MbA = k.sb("MbA", [128, nt, 32], BF16, e3)
                bc = lambda ap_, n_: ap_.to_broadcast([128, nt, n_])
                V(lambda v: v.reduce_max(out=GMx[:], in_=LG[:, :, 0:4], axis=AX.X), [LG], [GMx])
                V(lambda v: v.tensor_tensor(out=OHG[:], in0=LG[:, :, 0:4], in1=bc(GMx[:, :, 0:1], 4), op=ALU.is_equal), [LG, GMx], [OHG])
                V(lambda v: v.tensor_tensor(out=EG[:], in0=LG[:, :, 0:4], in1=bc(GMx[:, :, 0:1], 4), op=ALU.subtract), [LG, GMx], [EG])
                k.op("act", lambda a: a.activation(out=EG[:], in_=EG[:], func=AF.Exp), reads=[EG], writes=[EG])
                V(lambda v: v.reduce_sum(out=SGs[:], in_=EG[:], axis=AX.X), [EG], [SGs])
                V(lambda v: v.reciprocal(out=PGs[:], in_=SGs[:]), [SGs], [PGs])
                V(lambda v: v.tensor_tensor(out=LES[:], in0=LG[:, :, 4:12], in1=bc(OHG[:, :, 0:1], 8), op=ALU.mult), [LG, OHG], [LES])
                for g_ in range(1, 4):
                    V(lambda v, g_=g_: v.tensor_tensor(out=T8[:], in0=LG[:, :, 4 + 8 * g_:12 + 8 * g_], in1=bc(OHG[:, :, g_:g_ + 1], 8), op=ALU.mult), [LG, OHG], [T8])
                    V(lambda v: v.tensor_tensor(out=LES[:], in0=LES[:], in1=T8[:], op=ALU.add), [LES, T8], [LES])
                V(lambda v: v.reduce_max(out=M1s[:], in_=LES[:], axis=AX.X), [LES], [M1s])
                V(lambda v: v.tensor_tensor(out=O1s[:], in0=LES[:], in1=bc(M1s[:, :, 0:1], 8), op=ALU.is_equal), [LES, M1s], [O1s])
                V(lambda v: v.scalar_tensor_tensor(out=MSK[:], in0=O1s[:], scalar=-1e30, in1=LES[:], op0=ALU.mult, op1=ALU.add), [O1s, LES], [MSK])
                V(lambda v: v.reduce_max(out=M2s[:], in_=MSK[:], axis=AX.X), [MSK], [M2s])
                V(lambda v: v.tensor_tensor(out=O2s[:], in0=MSK[:], in1=bc(M2s[:, :, 0:1], 8), op=ALU.is_equal), [MSK, M2s], [O2s])
                V(lambda v: v.tensor_tensor(out=DEs[:], in0=M2s[:], in1=M1s[:], op=ALU.subtract), [M1s, M2s], [DEs])
                k.op("act", lambda a: a.activation(out=DEs[:], in_=DEs[:], func=AF.Exp), reads=[DEs], writes=[DEs])
                V(lambda v: v.tensor_scalar(out=P1s[:], in0=DEs[:], scalar1=1.0, scalar2=None, op0=ALU.add), [DEs], [P1s])
                V(lambda v: v.reciprocal(out=P1s[:], in_=P1s[:]), [P1s], [P1s])
                V(lambda v: v.tensor_tensor(out=GT[:, :, 0:1], in0=P1s[:], in1=PGs[:], op=ALU.mult), [P1s, PGs], [GT])
                V(lambda v: v.tensor_tensor(out=GT[:, :, 1:2], in0=GT[:, :, 0:1], in1=DEs[:], op=ALU.mult), [GT, DEs], [GT])
                for g_ in range(4):
                    V(lambda v, g_=g_: v.tensor_tensor(out=OH1[:, :, 8 * g_:8 * g_ + 8], in0=O1s[:], in1=bc(OHG[:, :, g_:g_ + 1], 8), op=ALU.mult), [O1s, OHG], [OH1])
                    V(lambda v, g_=g_: v.tensor_tensor(out=OH2[:, :, 8 * g_:8 * g_ + 8], in0=O2s[:], in1=bc(OHG[:, :, g_:g_ + 1], 8), op=ALU.mult), [O2s, OHG], [OH2])
                V(lambda v: v.tensor_tensor(out=MbA[:], in0=OH1[:], in1=OH2[:], op=ALU.add), [OH1, OH2], [MbA])
                for t in range(nt):
                    k.op("pe", lambda pe, t=t: pe.matmul(pR1[:, 0:32], utri[:, :], MbA[:, t, :], start=True, stop=True), reads=[utri, MbA], writes=[pR1])
                    k.op("pe", lambda pe, t=t: pe.matmul(pR2[:, 0:32], onesb[:, :], MbA[:, t, :], start=True, stop=True), reads=[onesb, MbA], writes=[pR2])
                    V(lambda v, t=t: v.tensor_tensor(out=RK[:, t, :], in0=pR1[:, 0:32], in1=run[:], op=ALU.add), [pR1, run], [RK])
                    V(lambda v: v.tensor_tensor(out=run[:], in0=pR2[:, 0:32], in1=run[:], op=ALU.add), [pR2, run], [run])
                k.barrier()
            V = lambda fn, rd, wrr: k.op("dve", fn, reads=rd, writes=wrr)
            V(lambda v: v.tensor_scalar(out=pad_[:], in0=run[:], scalar1=1.0 / MOE_S, scalar2=(MOE_S - 1.0) / MOE_S - 0.499, op0=ALU.mult, op1=ALU.add), [run], [pad_])
            V(lambda v: v.tensor_copy(nbi[:], pad_[:]), [pad_], [nbi])
            V(lambda v: v.tensor_copy(pad_[:], nbi[:]), [nbi], [pad_])
            V(lambda v: v.tensor_scalar(out=pad_[:], in0=pad_[:], scalar1=float(MOE_S), scalar2=None, op0=ALU.mult), [pad_], [pad_])
            V(lambda v: v.tensor_tensor_scan(out=pend[:], data0=ones_f[:, 0:32], data1=pad_[:], initial=0.0, op0=ALU.mult, op1=ALU.add), [ones_f, pad_], [pend])
            V(lambda v: v.tensor_tensor(out=pst_[:], in0=pend[:], in1=pad_[:], op=ALU.subtract), [pend, pad_], [pst_])
            V(lambda v: v.tensor_tensor(out=RK[:], in0=RK[:], in1=pst_[:, :].unsqueeze(1).to_broadcast([128, nt, 32]), op=ALU.add), [RK, pst_], [RK])
            V(lambda v: v.tensor_tensor(out=T32[:], in0=RK[:], in1=OH1[:], op=ALU.mult), [RK, OH1], [T32])
            V(lambda v: v.reduce_sum(out=DST[:, :, 0:1], in_=T32[:], axis=AX.X), [T32], [DST])
            V(lambda v: v.tensor_tensor(out=T32[:], in0=RK[:], in1=OH2[:], op=ALU.mult), [RK, OH2], [T32])
            V(lambda v: v.reduce_sum(out=DST[:, :, 1:2], in_=T32[:], axis=AX.X), [T32], [DST])
            V(lambda v: v.tensor_copy(DSTi[:], DST[:]), [DST], [DSTi])
            V(lambda v: v.memset(bacc[:], 0.0), [], [bacc])
            for e_ in range(NE):
                V(lambda v, e_=e_: v.scalar_tensor_tensor(out=bacc[:], in0=blkS[:, 0:NBk], scalar=pend[:, e_:e_ + 1], in1=bacc[:], op0=ALU.is_ge, op1=ALU.add), [blkS, pend, bacc], [bacc])
            V(lambda v: v.tensor_scalar(out=bacc[:], in0=bacc[:], scalar1=float(NE - 1), scalar2=None, op0=ALU.min), [bacc], [bacc])
            V(lambda v: v.tensor_scalar(out=be1[:], in0=bacc[:], scalar1=128.0, scalar2=float(l * NE * 128), op0=ALU.mult, op1=ALU.add), [bacc], [be1])
            V(lambda v: v.tensor_scalar(out=WIf[:], in0=be1[:], scalar1=iow[:, 0:1], scalar2=None, op0=ALU.add), [be1, iow], [WIf])
            V(lambda v: v.tensor_copy(WI[:], WIf[:]), [WIf], [WI])
            for t, (j, ti) in enumerate(tiles):
                hb2 = hb2s[t % 2]
                r0 = tok_row(j, ti)
                k.dma("sp", hb2[:], hrow.t.ap()[r0:r0 + 128, :], reads=[hrow_b[t]], writes=[hb2])
                for q_ in range(2):
                    k.idma(xs.t.ap(), bass.IndirectOffsetOnAxis(ap=DSTi[:, t, q_:q_ + 1], axis=0), hb2[:, :], None, reads=[DSTi, hb2, xs_z], writes=[xs_w[t]])
            if "moe" in dbg:
                o = dbg_out("dst", [128, nt, 2])
                k.dma("sp", o.ap(), DST[:], reads=[DST])
                o = dbg_out("gt", [128, nt, 2])
                k.dma("sp", o.ap(), GT[:], reads=[GT])
                o = dbg_out("blke", [128, NBk])
                k.dma("sp", o.ap(), bacc[:], reads=[bacc])
                o = dbg_out("oh1", [128, nt, 32])
                k.dma("sp", o.ap(), OH1[:], reads=[OH1])
            k.barrier()
            eR.close()
            with ExitStack() as e3:
                wgs = [k.sb("mwg", [128, 8, 512], BF16, e3) for _ in range(2)]
                wus = [k.sb("mwu", [128, 8, 512], BF16, e3) for _ in range(2)]
                wds = [k.sb("mwd", [128, 4, D], BF16, e3) for _ in range(2)]
                stoks = [k.sb("stok", [128, 4], I32, e3) for _ in range(2)]
                xbs = [k.sb("mxb", [128, 4, D], BF16, e3) for _ in range(2)]
                xbTs = [k.sb("mxbT", [128, 8, 512], BF16, e3) for _ in range(2)]
                hids = [k.sb("mhid", [128, 4, 512], BF16, e3) for _ in range(2)]
                sgs = [k.sb("msg", [128, 512], F32, e3) for _ in range(2)]
                ybs = [k.sb("myb", [128, D], F32, e3) for _ in range(2)]
                ptT = [k.ps("mptT", [128, 512], BF16, e3) for _ in range(2)]
                pgu = [k.ps("mpgu", [128, 512], F32, e3) for _ in range(4)]
                pyy = [k.ps("mpy", [128, 512], F32, e3) for _ in range(2)]
                cnt = 0

                def ldX(b_):
                    k.dma("sp", xbs[b_ % 2][:], xs.t.ap()[b_ * MOE_S:(b_ + 1) * MOE_S, :].rearrange("(p a) d -> p a d", a=4), reads=xs_w + [xs_z], writes=[xbs[b_ % 2]])
                for b in range(NBk):
                    wg_, wu_, wd_, stok, xb, xbT, hid = wgs[b % 2], wus[b % 2], wds[b % 2], stoks[b % 2], xbs[b % 2], xbTs[b % 2], hids[b % 2]
                    if b == 0:
                        ldX(0)
                    if b + 1 < NBk:
                        ldX(b + 1)
                    k.idma(wg_[:].rearrange("p j n -> p (j n)"), None, wgT, bass.IndirectOffsetOnAxis(ap=WI[:, b:b + 1], axis=0), reads=[WI], writes=[wg_])
                    k.idma(wu_[:].rearrange("p j n -> p (j n)"), None, wuT, bass.IndirectOffsetOnAxis(ap=WI[:, b:b + 1], axis=0), reads=[WI], writes=[wu_])
                    k.idma(wd_[:].rearrange("p j n -> p (j n)"), None, wdT, bass.IndirectOffsetOnAxis(ap=WI[:, b:b + 1], axis=0), reads=[WI], writes=[wd_])
                    for kc in range(8):
                        pt = ptT[kc % 2]

                        def f(pe, pt=pt, kc=kc, xb=xb):
                            for a_ in range(4):
                                ins = pe.transpose(pt[:, a_ * 128:(a_ + 1) * 128], xb[:, a_, kc::8], ident_b[:, :])
                            return ins
                        k.op("pe", f, reads=[xb, ident_b], writes=[pt])
                        if kc % 2 == 0:
                            k.op("act", lambda a, pt=pt, kc=kc, xbT=xbT: a.copy(xbT[:, kc, :], pt[:, :]), reads=[pt], writes=[xbT])
                        else:
                            k.op("dve", lambda v, pt=pt, kc=kc, xbT=xbT: v.tensor_copy(xbT[:, kc, :], pt[:, :]), reads=[pt], writes=[xbT])
                    for ec in range(4):
                        pg, pu = pgu[(2 * ec) % 4], pgu[(2 * ec + 1) % 4]
                        sg = sgs[ec % 2]

                        def f(pe, pg=pg, pu=pu, ec=ec, wg_=wg_, wu_=wu_, xbT=xbT):
                            for kc in range(8):
                                pe.matmul(pg[:, :], wg_[:, kc, ec::4], xbT[:, kc, :], start=(kc == 0), stop=(kc == 7))
                            for kc in range(8):
                                ins = pe.matmul(pu[:, :], wu_[:, kc, ec::4], xbT[:, kc, :], start=(kc == 0), stop=(kc == 7))
                            return ins
                        k.op("pe", f, reads=[wg_, wu_, xbT], writes=[pg, pu])
                        k.op("act", lambda a, pg=pg, sg=sg: a.activation(out=sg[:], in_=pg[:, :], func=AF.Silu), reads=[pg], writes=[sg])
                        k.op("dve", lambda v, pu=pu, sg=sg, hid=hid, ec=ec: v.tensor_tensor(out=hid[:, ec, :], in0=pu[:, :], in1=sg[:], op=ALU.mult), reads=[pu, sg], writes=[hid])
                    for a_ in range(4):
                        yb = ybs[a_ % 2]
                        for nh in range(2):
                            py = pyy[nh]

                            def f(pe, py=py, a_=a_, nh=nh, hid=hid, wd_=wd_):
                                for ec in range(4):
                                    ins = pe.matmul(py[:, :], hid[:, ec, a_ * 128:(a_ + 1) * 128], wd_[:, ec, nh * 512:(nh + 1) * 512], start=(ec == 0), stop=(ec == 3))
                                return ins
                            k.op("pe", f, reads=[hid, wd_], writes=[py])
                            if nh == 0:
                                k.op("act", lambda a, py=py, yb=yb: a.copy(yb[:, 0:512], py[:, :]), reads=[py], writes=[yb])
                            else:
                                k.op("dve", lambda v, py=py, yb=yb: v.tensor_copy(yb[:, 512:1024], py[:, :]), reads=[py], writes=[yb])
                        k.dma("sp", yslot.t.ap()[b * MOE_S:(b + 1) * MOE_S, :].rearrange("(p a) d -> p a d", a=4)[:, a_, :], yb[:], reads=[yb], writes=[ysl_b[b]])
                k.barrier()
            with ExitStack() as e3:
                A5 = k.sb("A5", [128, D], F32, e3)
                A5c = k.sb("A5c", [128, D], F32, e3)
                if not last:
                    mod_bc(A5c, l, 4, 5)
                xts = [k.sb("xtc", [128, D], F32, e3) for _ in range(2)]
                o1s = [k.sb("o1c", [128, D], F32, e3) for _ in range(2)]
                o2s = [k.sb("o2c", [128, D], F32, e3) for _ in range(2)]
                curj = None

                def ldC(t_):
                    j_, ti_ = tiles[t_]
                    r0_ = tok_row(j_, ti_)
                    k.dma("sp", xts[t_ % 2][:], xres.t.ap()[r0_:r0_ + 128, :], reads=[xres_b[(j_, ti_)]], writes=[xts[t_ % 2]])
                for t, (j, ti) in enumerate(tiles):
                    col, isctx = TT[ti]
                    if j != curj:
                        mod_bc(A5, l, j, 5)
                        curj = j
                    xt, o1_, o2_ = xts[t % 2], o1s[t % 2], o2s[t % 2]
                    r0 = tok_row(j, ti)
                    xb_ = xres_b[(j, ti)]
                    if t == 0:
                        ldC(0)
                    if t + 1 < nt:
                        ldC(t + 1)
                    k.idma(o1_[:], None, yslot.t.ap(), bass.IndirectOffsetOnAxis(ap=DSTi[:, t, 0:1], axis=0), reads=[DSTi] + ysl_b, writes=[o1_])
                    k.idma(o2_[:], None, yslot.t.ap(), bass.IndirectOffsetOnAxis(ap=DSTi[:, t, 1:2], axis=0), reads=[DSTi] + ysl_b, writes=[o2_])
                    k.op("dve", lambda v, o1_=o1_, t=t: v.tensor_scalar(out=o1_[:], in0=o1_[:], scalar1=GT[:, t, 0:1], scalar2=None, op0=ALU.mult), reads=[o1_, GT], writes=[o1_])
                    k.op("dve", lambda v, o1_=o1_, o2_=o2_, t=t: v.scalar_tensor_tensor(out=o1_[:], in0=o2_[:], scalar=GT[:, t, 1:2], in1=o1_[:], op0=ALU.mult, op1=ALU.add), reads=[o1_, o2_, GT], writes=[o1_])
                    Ax = A5c if isctx else A5
                    k.op("pool", lambda g, o1_=o1_, Ax=Ax: g.tensor_tensor(out=o1_[:], in0=o1_[:], in1=Ax[:], op=ALU.mult), reads=[o1_, Ax], writes=[o1_])
                    k.op("dve", lambda v, o1_=o1_, xt=xt: v.tensor_tensor(out=o1_[:], in0=o1_[:], in1=xt[:], op=ALU.add), reads=[o1_, xt], writes=[o1_])
                    k.dma("sp", xres.t.ap()[r0:r0 + 128, :], o1_[:], reads=[o1_], writes=[xb_])
                k.barrier()

    def final_phase(j):
        with ExitStack() as e2:
            gf = k.sb("gf", [128, D], F32, e2)
            z0 = k.sb("z0", [128, D], F32, e2)
            k.dma("sp", gf[:], IN("g_final")[0:1, :].to_broadcast([128, D]), writes=[gf])
            k.op("dve", lambda v: v.memset(z0[:], 0.0), writes=[z0])
            xts = [k.sb("xtf", [128, D], F32, e2) for _ in range(2)]
            hos = [k.sb("hof", [128, D], F32, e2) for _ in range(2)]
            tmp = k.sb("tmpf", [128, D], F32, e2)
            sts = [[k.sb("stf", [128, 1], F32, e2) for _ in range(3)] for _ in range(2)]
            for ti in range(2, 18):
                xt, ho, st = xts[ti % 2], hos[ti % 2], sts[ti % 2]
                r0 = tok_row(j, ti)
                k.dma("sp", xt[:], xres.t.ap()[r0:r0 + 128, :], reads=[xres_b[(j, ti)]], writes=[xt])
                norm_mod(xt, gf, z0, tmp, ho, st)
                k.dma("sp", out_d.ap()[j, (ti - 2) * 128:(ti - 1) * 128, :], ho[:], reads=[ho])
            k.barrier()

    def finish():
        if "hT" in dbg:
            o = dbg_out("hT", [128, 8, PW], BF16)
            k.dma("sp", o.ap(), hT[:], reads=[hT])
        if "yT" in dbg:
            o = dbg_out("yT", [128, 8, PW], BF16)
            k.dma("sp", o.ap(), yT[:], reads=[yT])
        if "Hd" in dbg:
            for tg_, Lx_ in (("l", L), ("c", LC)):
                o = dbg_out("Hd_" + tg_, [2, Lx_, 512])
                k.dma("sp", o.ap(), Hd[tg_].t.ap(), reads=[Hd[tg_]])
        if "xres" in dbg:
            o = dbg_out("xres", [ntok, D])
            k.dma("sp", o.ap(), xres.t.ap(), reads=list(xres_b.values()))
        k.wait_all("sp")
        k.barrier()
        es.close()
        return nc, dbg_t

    steps = stop_after or "all"
    for l in range(layers):
        last = (l == DEPTH - 1)
        if steps in ("d", "all", "m"):
            hyena_prep(l)
        for j in range(nseq):
            stage1(l, j)
            if steps == "s1":
                return finish()
            if steps in ("a", "all", "w", "m"):
                mixer_a(l, j)
            if steps == "a":
                return finish()
            if steps in ("c", "all", "w", "m"):
                mixer_c(l, j)
            if steps == "c":
                return finish()
            if steps in ("b", "all", "m"):
                mixer_b(l, j)
            if steps == "b":
                return finish()
            if steps in ("d", "all", "m"):
                mixer_d(l, j)
            if steps == "d":
                return finish()
            wout_phase(l, j, last)
            if steps == "w":
                return finish()
        if steps in ("all", "m"):
            ffn_phase(l, last)
        if steps == "m":
            return finish()
    for j in range(nseq):
        final_phase(j)
    return finish()


def kernel(**inputs):
    consts = host_consts()
    n_cores = 8
    maps = []
    for c in range(n_cores):
        m = layout_inputs(inputs, c)
        maps.append(m)
    nc, _ = build(maps[0], consts)
    in_maps = []
    for m in maps:
        im = dict(m)
        im.update(consts)
        in_maps.append(im)
    res = run_bass_kernel_spmd(nc, in_maps, core_ids=list(range(n_cores)))
    out = np.concatenate([np.asarray(r["out"], np.float32) for r in res.results], axis=0)
    return out
```

```python
import math
import os
from contextlib import ExitStack
import numpy as np
import ml_dtypes
import concourse.bass as bass
import concourse.mybir as mybir
from concourse.bass_utils import run_bass_kernel_spmd

F32 = mybir.dt.float32
BF16 = mybir.dt.bfloat16
I32 = mybir.dt.int32
ALU = mybir.AluOpType
AF = mybir.ActivationFunctionType
AX = mybir.AxisListType

D = 1024
L = 2048
LC = 256
NB_ = 4
DEPTH = 4
PAD = 16
C0 = PAD
L0 = PAD + LC + PAD
PW = L0 + L + PAD
TOK = LC + L
NTOK = NB_ * TOK
EPS = 1e-6
FULL_TILES = [(0, 512), (512, 512), (1024, 512), (1536, 512), (2048, PW - 2048)]
VT = [(C0, LC)] + [(L0 + 512 * i, 512) for i in range(4)]
TT = [(C0 + 128 * i, True) for i in range(2)] + [(L0 + 128 * i, False) for i in range(16)]
N_DMA_SEMS = 12
MOE_S = 512
NE = 32


class Buf:
    __slots__ = ("t", "last_w", "readers", "name")

    def __init__(self, t, name=""):
        self.t = t
        self.last_w = None
        self.readers = []
        self.name = name

    def __getitem__(self, idx):
        return self.t[idx]


class K:
    def __init__(self, nc, es):
        self.nc = nc
        self.es = es
        self.eng = {"pe": nc.tensor, "act": nc.scalar, "dve": nc.vector, "pool": nc.gpsimd, "sp": nc.sync}
        self.sem = {}
        self.cnt = {}
        for e in self.eng:
            self.sem[e] = es.enter_context(nc.semaphore("s_" + e))
            self.cnt[e] = 0
        self.dsem = {}
        self.dcnt = {}
        self.dnext = {}
        for q in ("sp", "act", "pool"):
            self.dsem[q] = [es.enter_context(nc.semaphore("d_%s%d" % (q, i))) for i in range(N_DMA_SEMS)]
            self.dcnt[q] = [0] * N_DMA_SEMS
            self.dnext[q] = 0
        self.seen = {e: {} for e in self.eng}
        self.n_wait = 0
        self.n_inst = 0
        self.uid = 0

    def nm(self, s):
        self.uid += 1
        return "%s_%d" % (s, self.uid)

    def sb(self, name, shape, dt, es=None):
        t = (es or self.es).enter_context(self.nc.sbuf_tensor(self.nm(name), shape, dt))
        return Buf(t, name)

    def ps(self, name, shape, dt=F32, es=None):
        t = (es or self.es).enter_context(self.nc.psum_tensor(self.nm(name), shape, dt))
        return Buf(t, name)

    def dram(self, name, shape, dt, kind="Internal"):
        t = self.nc.dram_tensor(name, shape, dt, kind=kind)
        return Buf(t, name)

    def _semh(self, key):
        if key[0] == "e":
            return self.sem[key[1]]
        return self.dsem[key[1]][key[2]]

    def _wait(self, e, tok):
        key, val = tok
        if self.seen[e].get(key, 0) >= val:
            return
        self.eng[e].wait_ge(self._semh(key), val)
        self.seen[e][key] = val
        self.n_wait += 1

    def _deps(self, e, reads, writes):
        for b in reads:
            if b.last_w is not None:
                self._dep1(e, b.last_w, "raw")
        for b in writes:
            if b.last_w is not None:
                self._dep1(e, b.last_w, "waw")
            for r in b.readers:
                self._dep1(e, r, "war")

    def _dep1(self, e, tok, kind):
        key = tok[0]
        if key[0] == "e" and key[1] == e:
            if e == "pe" or kind != "raw":
                return
        self._wait(e, tok)

    def _commit(self, tok, reads, writes):
        for b in reads:
            if len(b.readers) > 24:
                b.readers = b.readers[-24:] if False else b.readers
            b.readers.append(tok)
        for b in writes:
            b.last_w = tok
            b.readers = []

    def op(self, e, fn, reads=(), writes=()):
        self._deps(e, reads, writes)
        inst = fn(self.eng[e])
        self.cnt[e] += 1
        inst.then_inc(self.sem[e], 1)
        self.n_inst += 1
        tok = (("e", e), self.cnt[e])
        self._commit(tok, reads, writes)
        return tok

    def _dq(self, q, reads, writes):
        self._deps(q, reads, writes)
        i = self.dnext[q]
        self.dnext[q] = (i + 1) % N_DMA_SEMS
        key = ("d", q, i)
        if self.dcnt[q][i] > 0:
            self._wait(q, (key, self.dcnt[q][i]))
        return i, key

    def dma(self, q, out, in_, reads=(), writes=(), **kw):
        i, key = self._dq(q, reads, writes)
        inst = self.eng[q].dma_start(out=out, in_=in_, **kw)
        self.dcnt[q][i] += 16
        inst.then_inc(self.dsem[q][i], 16)
        tok = (key, self.dcnt[q][i])
        self._commit(tok, reads, writes)
        self.n_inst += 1
        return tok

    def idma(self, out, out_off, in_, in_off, reads=(), writes=(), **kw):
        q = "pool"
        i, key = self._dq(q, reads, writes)
        inst = self.eng[q].indirect_dma_start(out=out, out_offset=out_off, in_=in_, in_offset=in_off, **kw)
        self.dcnt[q][i] += 16
        inst.then_inc(self.dsem[q][i], 16)
        tok = (key, self.dcnt[q][i])
        self._commit(tok, reads, writes)
        self.n_inst += 1
        return tok

    def wait_all(self, e):
        for e2 in self.eng:
            if self.cnt[e2] > 0 and e2 != e:
                self._wait(e, (("e", e2), self.cnt[e2]))
        for q in self.dsem:
            for i in range(N_DMA_SEMS):
                if self.dcnt[q][i] > 0:
                    self._wait(e, (("d", q, i), self.dcnt[q][i]))

    def barrier(self):
        for e in self.eng:
            self.wait_all(e)


def _bf(a):
    return np.asarray(a, np.float32).astype(ml_dtypes.bfloat16)


def host_consts():
    c = {}
    c["ident_f"] = np.eye(128, dtype=np.float32)
    c["ident_b"] = _bf(np.eye(128))
    c["ones_f"] = np.ones((128, 128), np.float32)
    c["ones_b"] = _bf(np.ones((128, 128)))
    blk = np.zeros((128, 128), np.float32)
    blk[:64, :64] = 1
    blk[64:, 64:] = 1
    c["blk64_f"] = blk
    c["utri_b"] = _bf(np.triu(np.ones((128, 128)), 1))
    inv = 10000.0 ** (-np.arange(8, dtype=np.float32) / 8)
    t = np.arange(L)
    row = (t // 64).astype(np.float32)
    col = (t % 64).astype(np.float32)
    ang = np.concatenate([row[:, None] * inv, col[:, None] * inv], -1)
    cosf = np.ones((128, PW), np.float32)
    sinf = np.zeros((128, PW), np.float32)
    rot = np.zeros((128, 128), np.float32)
    for ch in range(128):
        d = ch % 32
        cosf[ch, L0:L0 + L] = np.cos(ang[:, d % 16])
        s = np.sin(ang[:, d % 16])
        if d < 16:
            sinf[ch, L0:L0 + L] = -s
            rot[ch + 16, ch] = 1.0
        else:
            sinf[ch, L0:L0 + L] = s
            rot[ch - 16, ch] = 1.0
    c["rope_cos"] = cosf
    c["rope_sin"] = sinf
    c["rot_b"] = _bf(rot)
    for tag, Lx in (("l", L), ("c", LC)):
        N = 2 * Lx
        tt = np.arange(Lx, dtype=np.float64)
        ff = np.arange(Lx, dtype=np.float64) + 0.5
        th = 2 * np.pi * np.outer(tt, ff) / N
        nch = Lx // 128
        CF = np.cos(th)
        SF = np.sin(th)
        def fw(M):
            return _bf(M.reshape(nch, 128, nch, 128).transpose(2, 1, 0, 3).copy())
        c["cf_" + tag] = fw(CF)
        c["sf_" + tag] = fw(SF)
        def iv(M):
            return _bf(M.T.reshape(nch, 128, Lx).transpose(1, 0, 2).copy())
        c["ci_" + tag] = iv(CF * (2.0 / N))
        c["si_" + tag] = iv(-SF * (2.0 / N))
        tu = tt / max(Lx - 1, 1)
        bands = np.linspace(1e-4, 15, 16)
        a2 = (2.0 * math.pi / Lx) * tt[:, None] * bands[None, :]
        feats = np.concatenate([tu[:, None], np.cos(a2), -np.sin(a2)], -1)
        c["feats_" + tag] = feats.T.astype(np.float32).copy()
        c["ntu_" + tag] = (-tu).reshape(nch, 128).T.astype(np.float32).copy()
    io = np.zeros((128, 16), np.float32)
    for kc in range(8):
        io[:, kc] = kc * 128 + np.arange(128)
    for ec in range(4):
        io[:, 8 + ec] = ec * 128 + np.arange(128)
    c["iota_w"] = io
    c["iota_p"] = np.arange(128, dtype=np.float32).reshape(128, 1)
    c["iota_e"] = np.tile(np.arange(NE, dtype=np.float32)[None], (128, 1))
    c["zeros_i"] = np.zeros((128, 512), np.int32)
    tk = np.zeros((128, NB_ * 18), np.int32)
    for j in range(NB_):
        for ti in range(18):
            r0 = j * TOK + (ti * 128 if ti < 2 else LC + (ti - 2) * 128)
            tk[:, j * 18 + ti] = r0 + np.arange(128)
    c["tokid"] = tk
    c["blkS"] = np.tile((np.arange(80, dtype=np.float32) * MOE_S)[None], (128, 1))
    return c


def layout_inputs(inp, core, nseq=NB_):
    f = lambda a: np.ascontiguousarray(np.asarray(a, np.float32))
    b0 = core * NB_
    m = {}
    m["x"] = f(inp["x"][b0:b0 + nseq])
    m["ctx"] = f(inp["ctx"][b0:b0 + nseq])
    cv = np.concatenate([np.asarray(inp["c"], np.float32)[b0:b0 + NB_], np.asarray(inp["c_ctx"], np.float32)[None]], 0)
    m["cvT"] = f(cv.reshape(5, 8, 128).transpose(2, 1, 0))
    for n in ("w_ada", "b_ada", "g_mix", "g_ffn", "w_in", "w_out", "c_w_pw", "d_w_f1", "d_w_f2", "d_w_f3", "d_decay",
              "b_lq1", "b_lk1", "b_lq2", "b_lk2", "moe_w_gate", "moe_w_up", "moe_w_down"):
        m[n] = f(inp[n])
    m["g_final"] = f(np.asarray(inp["g_final"]).reshape(1, D))
    vA = np.zeros((DEPTH, 128, 32), np.float32)
    for d in range(2):
        for c in range(2):
            o = (d * 2 + c) * 8
            sl = slice(c * 128, c * 128 + 128)
            vA[:, :, o:o + 4] = np.asarray(inp["a_conv_w"])[:, d, :, sl].transpose(0, 2, 1)
            vA[:, :, o + 4] = np.asarray(inp["a_conv_b"])[:, d, sl]
            vA[:, :, o + 5] = np.asarray(inp["a_b_r"])[:, d, sl]
            vA[:, :, o + 6] = np.asarray(inp["a_b_i"])[:, d, sl]
            vA[:, :, o + 7] = np.asarray(inp["a_lam"])[:, d, sl]
    m["vecA"] = vA
    wb = np.zeros((DEPTH, 128, 8, 128), np.float32)
    for d in range(2):
        for g, nme in enumerate(("a_w_r", "a_w_i")):
            w = np.asarray(inp[nme])
            for c in range(2):
                i = (d * 2 + g) * 2 + c
                wb[:, 0:64, i, 0:64] = w[:, d, 2 * c]
                wb[:, 64:128, i, 64:128] = w[:, d, 2 * c + 1]
    m["wblkA"] = wb
    m["vecB"] = f(np.tile(np.asarray(inp["b_sub_g"]), (1, 2)).reshape(DEPTH, 128, 1))
    vC = np.zeros((DEPTH, 128, 2, 35), np.float32)
    for c in range(2):
        sl = slice(c * 128, c * 128 + 128)
        vC[:, :, c, 0:31] = np.asarray(inp["c_conv_w"])[:, :, sl].transpose(0, 2, 1)
        vC[:, :, c, 31] = np.asarray(inp["c_conv_b"])[:, sl]
        vC[:, :, c, 32] = np.asarray(inp["c_ln_g"])[:, sl]
        vC[:, :, c, 33] = np.asarray(inp["c_ln_b"])[:, sl]
        vC[:, :, c, 34] = np.asarray(inp["c_b_pw"])[:, sl]
    m["vecC"] = vC
    vD = np.zeros((DEPTH, 128, 6, 4), np.float32)
    for c in range(6):
        sl = slice(c * 128, c * 128 + 128)
        vD[:, :, c, 0:3] = np.asarray(inp["d_conv_w"])[:, :, sl].transpose(0, 2, 1)
        vD[:, :, c, 3] = np.asarray(inp["d_conv_b"])[:, sl]
    m["vecD"] = vD
    m["vecD2"] = f(np.asarray(inp["d_bias"]).reshape(DEPTH, 2, 2, 128).transpose(0, 3, 1, 2).reshape(DEPTH, 128, 4))
    m["hyv"] = f(np.stack([np.asarray(inp["d_b_f1"]), np.asarray(inp["d_freq"]), np.asarray(inp["d_b_f2"])], -1))
    m["wr"] = f(np.concatenate([np.asarray(inp["moe_w_rg"]), np.asarray(inp["moe_w_re"])], -1))
    m["br"] = f(np.concatenate([np.asarray(inp["moe_b_rg"]), np.asarray(inp["moe_b_re"])], -1))
    return m


DT_OF = {np.dtype("float32"): F32, np.dtype("int32"): I32, np.dtype(ml_dtypes.bfloat16): BF16}


class Prog:
    pass


def build(in_map, consts, nseq=NB_, layers=DEPTH, dbg=None, stop_after=None):
    nc = bass.Bass("TRN2", target_bir_lowering=False)
    T = Prog()
    T.din = {}
    for n, a in list(in_map.items()) + list(consts.items()):
        T.din[n] = nc.dram_tensor(n, list(a.shape), DT_OF[a.dtype], kind="ExternalInput")
    out_d = nc.dram_tensor("out", [nseq, L, D], F32, kind="ExternalOutput")
    es = ExitStack()
    k = K(nc, es)
    T.k = k
    dbg = dbg or {}
    dbg_t = {}

    def dbg_out(name, shape, dt=F32):
        dbg_t[name] = nc.dram_tensor("dbg_" + name, list(shape), dt, kind="ExternalOutput")
        return dbg_t[name]

    IN = lambda n: T.din[n].ap()
    ntok = nseq * TOK
    xres = k.dram("xres", [ntok, D], F32)
    hrow = k.dram("hrow", [ntok, D], BF16)
    modD = k.dram("modD", [DEPTH, 5, 6 * D], F32)
    NSLOT = ((2 * ntok + MOE_S - 1) // MOE_S + NE) * MOE_S
    yslot = k.dram("yslot", [NSLOT, D], F32)
    slot_tok = k.dram("slot_tok", [NSLOT, 1], I32)
    xs = k.dram("xs", [NSLOT, D], BF16)
    xs_z = Buf(xs.t)
    Hd = {"l": k.dram("Hd_l", [2, L, 512], F32), "c": k.dram("Hd_c", [2, LC, 512], F32)}
    hfil = {"l": k.dram("hfil_l", [L, 1024], F32), "c": k.dram("hfil_c", [LC, 1024], F32)}

    ident_b = k.sb("ident_b", [128, 128], BF16)
    ident_f = k.sb("ident_f", [128, 128], F32)
    ones_f = k.sb("ones_f", [128, 128], F32)
    blk64 = k.sb("blk64", [128, 128], F32)
    eps6 = k.sb("eps6", [128, 1], F32)
    eps5 = k.sb("eps5", [128, 1], F32)
    one1 = k.sb("one1", [128, 1], F32)
    for b_, n_ in ((ident_b, "ident_b"), (ident_f, "ident_f"), (ones_f, "ones_f"), (blk64, "blk64_f")):
        k.dma("sp", b_[:], IN(n_), writes=[b_])
    k.op("dve", lambda v: v.memset(eps6[:], 1e-6), writes=[eps6])
    k.op("dve", lambda v: v.memset(eps5[:], 1e-5), writes=[eps5])
    k.op("dve", lambda v: v.memset(one1[:], 1.0), writes=[one1])
    hT = k.sb("hT", [128, 8, PW], BF16)
    yT = k.sb("yT", [128, 8, PW], BF16)
    k.op("pool", lambda g: g.memset(hT[:], 0.0), writes=[hT])
    k.op("pool", lambda g: g.memset(yT[:], 0.0), writes=[yT])

    def rot(lst, st=[0]):
        st[0] += 1
        return lst[st[0] % len(lst)]

    xres_b = {(j, ti): Buf(xres.t) for j in range(nseq) for ti in range(18)}
    for j in range(nseq):
        k.dma("sp", xres.t.ap()[j * TOK:j * TOK + LC, :], IN("ctx")[j], writes=[xres_b[(j, 0)], xres_b[(j, 1)]])
        k.dma("sp", xres.t.ap()[j * TOK + LC:(j + 1) * TOK, :], IN("x")[j], writes=[xres_b[(j, ti)] for ti in range(2, 18)])

    def prologue():
        with ExitStack() as e2:
            cv = k.sb("cv", [128, 8, 5], F32, e2)
            sT = k.sb("sT", [128, 8, 5], BF16, e2)
            k.dma("sp", cv[:], IN("cvT"), writes=[cv])
            k.op("act", lambda a: a.activation(out=sT[:], in_=cv[:], func=AF.Silu), reads=[cv], writes=[sT])
            was = [k.sb("wa", [128, 8, 512], BF16, e2) for _ in range(2)]
            pms = [k.ps("pm", [128, 512], F32, e2) for _ in range(2)]
            bada = k.sb("bada", [5, 6 * D], F32, e2)
            modsb = k.sb("modsb", [5, 6 * D], F32, e2)
            for l in range(layers):
                k.dma("sp", bada[:], IN("b_ada")[l:l + 1, :].to_broadcast([5, 6 * D]), writes=[bada])
                for ct in range(12):
                    wa = was[ct % 2]
                    pm = pms[ct % 2]
                    k.dma("pool", wa[:], IN("w_ada")[l][:, ct * 512:(ct + 1) * 512].rearrange("(kc p) n -> p kc n", p=128), writes=[wa])

                    def f(pe, wa=wa, pm=pm):
                        for kc in range(8):
                            ins = pe.matmul(pm[0:5, :], sT[:, kc, :], wa[:, kc, :], start=(kc == 0), stop=(kc == 7))
                        return ins
                    k.op("pe", f, reads=[sT, wa], writes=[pm])
                    k.op("dve", lambda v, pm=pm, ct=ct: v.tensor_tensor(out=modsb[0:5, ct * 512:(ct + 1) * 512], in0=pm[0:5, :],
                                                                     in1=bada[0:5, ct * 512:(ct + 1) * 512], op=ALU.add),
                         reads=[pm, bada], writes=[modsb])
                k.dma("sp", modD.t.ap()[l], modsb[:], reads=[modsb], writes=[modD])
            k.barrier()

    prologue()
    if "mod" in dbg:
        o = dbg_out("mod", [DEPTH, 5, 6 * D])
        k.dma("sp", o.ap(), modD.t.ap(), reads=[modD])

    def mod_bc(dst, l, r, m_):
        k.dma("sp", dst[:], modD.t.ap()[l, r:r + 1, m_ * D:(m_ + 1) * D].to_broadcast([128, D]), reads=[modD], writes=[dst])

    def norm_mod(xt, A1, A0, tmp, hout, st, heng="pool"):
        ss, sd, rs = st
        k.op("act", lambda a: a.activation(out=tmp[:], in_=xt[:], func=AF.Square, accum_out=ss[:, 0:1]), reads=[xt], writes=[tmp, ss])
        k.op("act", lambda a: a.activation(out=sd[:], in_=ss[:], func=AF.Sqrt, scale=1.0 / D, bias=eps6[:, 0:1]), reads=[ss, eps6], writes=[sd])
        k.op("dve", lambda v: v.reciprocal(out=rs[:], in_=sd[:]), reads=[sd], writes=[rs])
        k.op("dve", lambda v: v.scalar_tensor_tensor(out=tmp[:], in0=xt[:], scalar=rs[:, 0:1], in1=A1[:], op0=ALU.mult, op1=ALU.mult),
             reads=[xt, rs, A1], writes=[tmp])
        k.op(heng, lambda g: g.tensor_tensor(out=hout[:], in0=tmp[:], in1=A0[:], op=ALU.add), reads=[tmp, A0], writes=[hout])

    def load_mods(e2, l, r, mi_scale, mi_shift, gname):
        A1 = k.sb("A1", [128, D], F32, e2)
        A0 = k.sb("A0", [128, D], F32, e2)
        gt = k.sb("gt", [128, D], F32, e2)
        mod_bc(A1, l, r, mi_scale)
        mod_bc(A0, l, r, mi_shift)
        k.dma("sp", gt[:], IN(gname)[l:l + 1, :].to_broadcast([128, D]), writes=[gt])
        k.op("dve", lambda v: v.scalar_tensor_tensor(out=A1[:], in0=A1[:], scalar=1.0, in1=gt[:], op0=ALU.add, op1=ALU.mult),
             reads=[A1, gt], writes=[A1])
        return A1, A0

    def tok_row(j, ti):
        col, isctx = TT[ti]
        return j * TOK + (ti * 128 if isctx else LC + (ti - 2) * 128)

    def stage1(l, j):
        with ExitStack() as e2:
            A1, A0 = load_mods(e2, l, j, 1, 0, "g_mix")
            A1c, A0c = load_mods(e2, l, 4, 1, 0, "g_mix")
            xts = [k.sb("xt", [128, D], F32, e2) for _ in range(2)]
            hbs = [k.sb("hb", [128, D], BF16, e2) for _ in range(2)]
            tmp = k.sb("tmp", [128, D], F32, e2)
            sts = [[k.sb("st", [128, 1], F32, e2) for _ in range(3)] for _ in range(2)]
            psts = [k.ps("pst", [128, 8, 128], BF16, e2) for _ in range(2)]
            for ti, (col, isctx) in enumerate(TT):
                xt, hb, st, pst = xts[ti % 2], hbs[ti % 2], sts[ti % 2], psts[ti % 2]
                r0 = tok_row(j, ti)
                k.dma("sp", xt[:], xres.t.ap()[r0:r0 + 128, :], reads=[xres_b[(j, ti)]], writes=[xt])
                norm_mod(xt, A1c if isctx else A1, A0c if isctx else A0, tmp, hb, st)

                def f(pe, hb=hb, pst=pst):
                    for kc in range(8):
                        ins = pe.transpose(pst[:, kc, :], hb[:, kc * 128:(kc + 1) * 128], ident_b[:, :])
                    return ins
                k.op("pe", f, reads=[hb, ident_b], writes=[pst])
                k.op("act", lambda a, pst=pst, col=col: a.copy(hT[:, :, col:col + 128], pst[:, :, :]), reads=[pst], writes=[hT])
            k.barrier()

    def proj(wg, wcol0, pp, evac):
        for (c0, n) in FULL_TILES:
            ps = rot(pp)

            def f(pe, ps=ps, c0=c0, n=n):
                for kc in range(8):
                    ins = pe.matmul(ps[:, 0:n], wg[:, kc, wcol0:wcol0 + 128], hT[:, kc, c0:c0 + n], start=(kc == 0), stop=(kc == 7))
                return ins
            k.op("pe", f, reads=[wg, hT], writes=[ps])
            evac(ps, c0, n)

    def load_w_in(e2, l, c_lo, c_n, nm="wg"):
        wg = k.sb(nm, [128, 8, c_n], BF16, e2)
        k.dma("pool", wg[:], IN("w_in")[l][:, c_lo:c_lo + c_n].rearrange("(kc p) n -> p kc n", p=128), writes=[wg])
        return wg

    def mixer_a(l, j):
        with ExitStack() as e2:
            wg = load_w_in(e2, l, 0, 512)
            pp = [k.ps("ppa", [128, 512], F32, e2) for _ in range(4)]
            pq = [k.ps("pqa", [128, 512], F32, e2) for _ in range(4)]
            xr = [k.sb("xr", [128, PW], BF16, e2) for _ in range(2)]
            xg = [k.sb("xg", [128, PW], F32, e2) for _ in range(2)]
            hsum = [k.sb("hsum", [128, PW], F32, e2) for _ in range(2)]
            hrev = k.sb("hrev", [128, PW], F32, e2)
            vA = k.sb("vA", [128, 32], F32, e2)
            k.dma("sp", vA[:], IN("vecA")[l], writes=[vA])
            wblk = k.sb("wblk", [128, 8, 128], BF16, e2)
            k.dma("pool", wblk[:], IN("wblkA")[l], writes=[wblk])
            lam4 = k.sb("lam4", [128, 4], F32, e2)
            c1 = k.sb("c1", [128, 4], F32, e2)
            c2 = k.sb("c2", [128, 4], F32, e2)
            k.op("act", lambda a: a.activation(out=lam4[:], in_=vA[:, 7::8], func=AF.Exp, scale=-1.0), reads=[vA], writes=[lam4])
            k.op("act", lambda a: a.activation(out=lam4[:], in_=lam4[:], func=AF.Ln, bias=one1[:, 0:1]), reads=[lam4, one1], writes=[lam4])
            k.op("dve", lambda v: v.tensor_scalar(out=c1[:], in0=lam4[:], scalar1=-8.0, scalar2=None, op0=ALU.mult), reads=[lam4], writes=[c1])
            k.op("dve", lambda v: v.tensor_scalar(out=c2[:], in0=lam4[:], scalar1=-16.0, scalar2=None, op0=ALU.mult), reads=[lam4], writes=[c2])
            if os.environ.get('KSTOP') == '0a':
                k.barrier(); return
            dg = k.sb("dgA", [128, 16, 128], BF16, e2)
            for dc in range(4):
                for kk in range(4):
                    k.op("dve", lambda v, dc=dc, kk=kk: v.tensor_scalar(out=dg[:, dc * 4 + kk, :], in0=ident_f[:], scalar1=vA[:, dc * 8 + kk:dc * 8 + kk + 1],
                                                                        scalar2=None, op0=ALU.mult), reads=[ident_f, vA], writes=[dg])
            if os.environ.get('KSTOP') == '0b':
                k.barrier(); return
            for oc in range(4):
                if oc < 2:
                    proj(wg, oc * 128, pp, lambda ps, c0, n, oc=oc: k.op("act", lambda a: a.copy(xr[oc][:, c0:c0 + n], ps[:, 0:n]), reads=[ps], writes=[xr[oc]]))
                else:
                    proj(wg, oc * 128, pp, lambda ps, c0, n, oc=oc: k.op("dve", lambda v: v.tensor_copy(xg[oc - 2][:, c0:c0 + n], ps[:, 0:n]), reads=[ps], writes=[xg[oc - 2]]))
            if os.environ.get('KSTOP') == '1':
                k.barrier(); return
            tl = [k.sb("tA%d" % i, [128, 512], F32, e2) for i in range(12)]
            ubs = [k.sb("ubA", [128, 512], BF16, e2) for _ in range(2)]
            for c in range(2):
                for d in range(2):
                    dc = d * 2 + c
                    hb = hsum[c] if d == 0 else hrev
                    order = [0, 1, 2, 3, 4] if d == 0 else [0, 4, 3, 2, 1]
                    for oi, vi in enumerate(order):
                        c0, n = VT[vi]
                        half = (oi % 2) * 6
                        uf, r_, i_, a_, e_, iu = tl[half:half + 6]
                        ub = ubs[oi % 2]
                        pu = rot(pq)

                        def fconv(pe, pu=pu, c0=c0, n=n, dc=dc, d=d, c=c):
                            for kk in range(4):
                                off = kk - 3 if d == 0 else kk
                                ins = pe.matmul(pu[:, 0:n], dg[:, dc * 4 + kk, :], xr[c][:, c0 + off:c0 + off + n], start=(kk == 0), stop=(kk == 3))
                            return ins
                        k.op("pe", fconv, reads=[dg, xr[c]], writes=[pu])
                        if os.environ.get('KSTOP') == '15':
                            k.barrier(); return
                        cb = vA[:, dc * 8 + 4:dc * 8 + 5]
                        k.op("act", lambda a, pu=pu, n=n, uf=uf, cb=cb: a.activation(out=uf[:, 0:n], in_=pu[:, 0:n], func=AF.Identity, bias=cb), reads=[pu, vA], writes=[uf])
                        if os.environ.get('KSTOP') == '16':
                            k.barrier(); return
                        k.op("dve", lambda v, uf=uf, n=n, ub=ub: v.tensor_copy(ub[:, 0:n], uf[:, 0:n]), reads=[uf], writes=[ub])
                        if os.environ.get('KSTOP') == '2':
                            k.barrier(); return
                        pr = rot(pq)
                        pi = rot(pq)
                        k.op("pe", lambda pe, pr=pr, n=n, ub=ub, d=d, c=c: pe.matmul(pr[:, 0:n], wblk[:, (d * 2 + 0) * 2 + c, :], ub[:, 0:n], start=True, stop=True), reads=[wblk, ub], writes=[pr])
                        k.op("pe", lambda pe, pi=pi, n=n, ub=ub, d=d, c=c: pe.matmul(pi[:, 0:n], wblk[:, (d * 2 + 1) * 2 + c, :], ub[:, 0:n], start=True, stop=True), reads=[wblk, ub], writes=[pi])
                        k.op("act", lambda a, pr=pr, n=n, r_=r_, dc=dc: a.activation(out=r_[:, 0:n], in_=pr[:, 0:n], func=AF.Sigmoid, bias=vA[:, dc * 8 + 5:dc * 8 + 6]), reads=[pr, vA], writes=[r_])
                        k.op("act", lambda a, pi=pi, n=n, i_=i_, dc=dc: a.activation(out=i_[:, 0:n], in_=pi[:, 0:n], func=AF.Sigmoid, bias=vA[:, dc * 8 + 6:dc * 8 + 7]), reads=[pi, vA], writes=[i_])
                        k.op("act", lambda a, n=n, r_=r_, a_=a_, dc=dc: a.activation(out=a_[:, 0:n], in_=r_[:, 0:n], func=AF.Exp, scale=c1[:, dc:dc + 1]), reads=[r_, c1], writes=[a_])
                        k.op("act", lambda a, n=n, r_=r_, e_=e_, dc=dc: a.activation(out=e_[:, 0:n], in_=r_[:, 0:n], func=AF.Exp, scale=c2[:, dc:dc + 1]), reads=[r_, c2], writes=[e_])
                        k.op("dve", lambda v, n=n, e_=e_: v.tensor_scalar(out=e_[:, 0:n], in0=e_[:, 0:n], scalar1=1.0, scalar2=-1.0, op0=ALU.min, op1=ALU.mult), reads=[e_], writes=[e_])
                        k.op("act", lambda a, n=n, e_=e_: a.activation(out=e_[:, 0:n], in_=e_[:, 0:n], func=AF.Sqrt, bias=one1[:, 0:1]), reads=[e_, one1], writes=[e_])
                        k.op("pool", lambda g, n=n, i_=i_, uf=uf, iu=iu: g.tensor_tensor(out=iu[:, 0:n], in0=i_[:, 0:n], in1=uf[:, 0:n], op=ALU.mult), reads=[i_, uf], writes=[iu])
                        k.op("dve", lambda v, n=n, e_=e_, iu=iu: v.tensor_tensor(out=iu[:, 0:n], in0=iu[:, 0:n], in1=e_[:, 0:n], op=ALU.mult), reads=[iu, e_], writes=[iu])
                        if os.environ.get('KSTOP') == '3':
                            k.barrier(); return
                        if vi == 0:
                            init = 0.0
                        elif d == 0:
                            init = hb[:, c0 - 1:c0] if vi > 1 else hb[:, C0 + LC - 1:C0 + LC]
                        else:
                            init = hb[:, c0 + n:c0 + n + 1] if vi < 4 else hb[:, C0:C0 + 1]
                        if d == 0:
                            k.op("dve", lambda v, n=n, c0=c0, a_=a_, iu=iu, hb=hb, init=init: v.tensor_tensor_scan(
                                out=hb[:, c0:c0 + n], data0=a_[:, 0:n], data1=iu[:, 0:n], initial=init, op0=ALU.mult, op1=ALU.add), reads=[a_, iu, hb], writes=[hb])
                        else:
                            k.op("dve", lambda v, n=n, c0=c0, a_=a_, iu=iu, hb=hb, init=init: v.tensor_tensor_scan(
                                out=hb[:, c0:c0 + n][:, ::-1], data0=a_[:, 0:n][:, ::-1], data1=iu[:, 0:n][:, ::-1], initial=init, op0=ALU.mult, op1=ALU.add),
                                reads=[a_, iu, hb], writes=[hb])
                if os.environ.get('KSTOP') == '4':
                    k.barrier(); return
                for (c0, n) in VT:
                    k.op("act", lambda a, c=c, c0=c0, n=n: a.activation(out=xg[c][:, c0:c0 + n], in_=xg[c][:, c0:c0 + n], func=AF.Gelu_apprx_tanh), reads=[xg[c]], writes=[xg[c]])
                    k.op("pool", lambda g, c=c, c0=c0, n=n: g.tensor_tensor(out=hsum[c][:, c0:c0 + n], in0=hsum[c][:, c0:c0 + n], in1=hrev[:, c0:c0 + n], op=ALU.add), reads=[hsum[c], hrev], writes=[hsum[c]])
                    k.op("dve", lambda v, c=c, c0=c0, n=n: v.tensor_tensor(out=yT[:, c, c0:c0 + n], in0=hsum[c][:, c0:c0 + n], in1=xg[c][:, c0:c0 + n], op=ALU.mult), reads=[hsum[c], xg[c]], writes=[yT])
            k.barrier()
    T.stage1, T.mixer_a = stage1, mixer_a

    def mixer_c(l, j):
        with ExitStack() as e2:
            wg = load_w_in(e2, l, 1280, 512)
            pp = [k.ps("ppc", [128, 512], F32, e2) for _ in range(8)]
            vC = k.sb("vC", [128, 2, 35], F32, e2)
            k.dma("sp", vC[:], IN("vecC")[l], writes=[vC])
            wpw = k.sb("wpw", [128, 2, 256], BF16, e2)
            k.dma("pool", wpw[:], IN("c_w_pw")[l].rearrange("(ic p) j -> p ic j", p=128), writes=[wpw])
            dg = k.sb("dgC", [128, 62, 128], BF16, e2)
            for c in range(2):
                for kk in range(31):
                    k.op("dve", lambda v, c=c, kk=kk: v.tensor_scalar(out=dg[:, c * 31 + kk, :], in0=ident_f[:], scalar1=vC[:, c, kk:kk + 1], scalar2=None, op0=ALU.mult),
                         reads=[ident_f, vC], writes=[dg])
            ub = [k.sb("ubC", [128, PW], BF16, e2) for _ in range(2)]
            sgt = [k.sb("sgt", [128, 512], F32, e2) for _ in range(2)]
            for c in range(2):
                for ti, (c0, n) in enumerate(FULL_TILES):
                    pv, pg = rot(pp), rot(pp)

                    def f(pe, pv=pv, pg=pg, c0=c0, n=n, c=c):
                        for kc in range(8):
                            pe.matmul(pv[:, 0:n], wg[:, kc, c * 128:c * 128 + 128], hT[:, kc, c0:c0 + n], start=(kc == 0), stop=(kc == 7))
                        for kc in range(8):
                            ins = pe.matmul(pg[:, 0:n], wg[:, kc, 256 + c * 128:256 + c * 128 + 128], hT[:, kc, c0:c0 + n], start=(kc == 0), stop=(kc == 7))
                        return ins
                    k.op("pe", f, reads=[wg, hT], writes=[pv, pg])
                    sg = sgt[ti % 2]
                    k.op("act", lambda a, pg=pg, n=n, sg=sg: a.activation(out=sg[:, 0:n], in_=pg[:, 0:n], func=AF.Sigmoid), reads=[pg], writes=[sg])
                    k.op("dve", lambda v, pv=pv, n=n, sg=sg, c=c, c0=c0: v.tensor_tensor(out=ub[c][:, c0:c0 + n], in0=pv[:, 0:n], in1=sg[:, 0:n], op=ALU.mult), reads=[pv, sg], writes=[ub[c]])
            tl = [k.sb("tC%d" % i, [128, 512], F32, e2) for i in range(10)]
            slb = [k.sb("slC", [128, 512], BF16, e2) for _ in range(2)]
            for (c0, n) in VT:
                cv = tl[0:2]
                sq = tl[2:4]
                mean, msq, var, t1 = tl[4:8]
                for c in range(2):
                    pc = rot(pp)

                    def f(pe, pc=pc, c0=c0, n=n, c=c):
                        for kk in range(31):
                            ins = pe.matmul(pc[:, 0:n], dg[:, c * 31 + kk, :], ub[c][:, c0 + kk - 15:c0 + kk - 15 + n], start=(kk == 0), stop=(kk == 30))
                        return ins
                    k.op("pe", f, reads=[dg, ub[c]], writes=[pc])
                    k.op("act", lambda a, pc=pc, n=n, c=c: a.activation(out=cv[c][:, 0:n], in_=pc[:, 0:n], func=AF.Identity, bias=vC[:, c, 31:32]), reads=[pc, vC], writes=[cv[c]])
                    k.op("pool", lambda g, n=n, c=c: g.tensor_tensor(out=sq[c][:, 0:n], in0=cv[c][:, 0:n], in1=cv[c][:, 0:n], op=ALU.mult), reads=[cv[c]], writes=[sq[c]])
                p1, p2 = rot(pp), rot(pp)

                def f(pe, p1=p1, p2=p2, n=n):
                    pe.matmul(p1[:, 0:n], ones_f[:, :], cv[0][:, 0:n], start=True, stop=False)
                    pe.matmul(p1[:, 0:n], ones_f[:, :], cv[1][:, 0:n], start=False, stop=True)
                    pe.matmul(p2[:, 0:n], ones_f[:, :], sq[0][:, 0:n], start=True, stop=False)
                    return pe.matmul(p2[:, 0:n], ones_f[:, :], sq[1][:, 0:n], start=False, stop=True)
                k.op("pe", f, reads=[ones_f, cv[0], cv[1], sq[0], sq[1]], writes=[p1, p2])
                k.op("act", lambda a, p1=p1, n=n: a.activation(out=mean[:, 0:n], in_=p1[:, 0:n], func=AF.Copy, scale=1.0 / 256), reads=[p1], writes=[mean])
                k.op("pool", lambda g, n=n: g.tensor_tensor(out=msq[:, 0:n], in0=mean[:, 0:n], in1=mean[:, 0:n], op=ALU.mult), reads=[mean], writes=[msq])
                k.op("dve", lambda v, p2=p2, n=n: v.scalar_tensor_tensor(out=var[:, 0:n], in0=p2[:, 0:n], scalar=1.0 / 256, in1=msq[:, 0:n], op0=ALU.mult, op1=ALU.subtract), reads=[p2, msq], writes=[var])
                k.op("act", lambda a, n=n: a.activation(out=var[:, 0:n], in_=var[:, 0:n], func=AF.Sqrt, bias=eps5[:, 0:1]), reads=[var, eps5], writes=[var])
                k.op("dve", lambda v, n=n: v.reciprocal(out=var[:, 0:n], in_=var[:, 0:n]), reads=[var], writes=[var])
                for c in range(2):
                    k.op("dve", lambda v, n=n, c=c: v.tensor_tensor(out=t1[:, 0:n], in0=cv[c][:, 0:n], in1=mean[:, 0:n], op=ALU.subtract), reads=[cv[c], mean], writes=[t1])
                    k.op("pool", lambda g, n=n: g.tensor_tensor(out=t1[:, 0:n], in0=t1[:, 0:n], in1=var[:, 0:n], op=ALU.mult), reads=[t1, var], writes=[t1])
                    k.op("act", lambda a, n=n, c=c: a.activation(out=slb[c][:, 0:n], in_=t1[:, 0:n], func=AF.Silu, scale=vC[:, c, 32:33], bias=vC[:, c, 33:34]), reads=[t1, vC], writes=[slb[c]])
                for jc in range(2):
                    po = rot(pp)

                    def f(pe, po=po, n=n, jc=jc):
                        pe.matmul(po[:, 0:n], wpw[:, 0, jc * 128:jc * 128 + 128], slb[0][:, 0:n], start=True, stop=False)
                        return pe.matmul(po[:, 0:n], wpw[:, 1, jc * 128:jc * 128 + 128], slb[1][:, 0:n], start=False, stop=True)
                    k.op("pe", f, reads=[wpw, slb[0], slb[1]], writes=[po])
                    k.op("act", lambda a, po=po, n=n, jc=jc, c0=c0: a.activation(out=yT[:, 4 + jc, c0:c0 + n], in_=po[:, 0:n], func=AF.Identity, bias=vC[:, jc, 34:35]), reads=[po, vC], writes=[yT])
            k.barrier()

    def wout_phase(l, j, last):
        with ExitStack() as e2:
            wo = k.sb("wo", [128, 8, D], BF16, e2)
            k.dma("pool", wo[:], IN("w_out")[l].rearrange("(kc p) n -> p kc n", p=128), writes=[wo])
            A2 = k.sb("A2", [128, D], F32, e2)
            A2c = k.sb("A2c", [128, D], F32, e2)
            mod_bc(A2, l, j, 2)
            mod_bc(A2c, l, 4, 2)
            xts = [k.sb("xtw", [128, D], F32, e2) for _ in range(2)]
            tws = [k.sb("tw", [128, D], F32, e2) for _ in range(2)]
            pps = [k.ps("ppw", [128, D], F32, e2) for _ in range(2)]
            tl_ = [ti for ti, (col, isctx) in enumerate(TT) if not (last and isctx)]

            def ldw(i_):
                ti_ = tl_[i_]
                r0_ = tok_row(j, ti_)
                k.dma("sp", xts[ti_ % 2][:], xres.t.ap()[r0_:r0_ + 128, :], reads=[xres_b[(j, ti_)]], writes=[xts[ti_ % 2]])
            ldw(0)
            for i_w, ti in enumerate(tl_):
                col, isctx = TT[ti]
                if i_w + 1 < len(tl_):
                    ldw(i_w + 1)
                xt, tw, pw = xts[ti % 2], tws[ti % 2], pps[ti % 2]
                r0 = tok_row(j, ti)

                def f(pe, pw=pw, col=col):
                    for nh in range(2):
                        for kc in range(8):
                            ins = pe.matmul(pw[:, nh * 512:(nh + 1) * 512], yT[:, kc, col:col + 128], wo[:, kc, nh * 512:(nh + 1) * 512], start=(kc == 0), stop=(kc == 7))
                    return ins
                k.op("pe", f, reads=[yT, wo], writes=[pw])
                Ax = A2c if isctx else A2
                k.op("dve", lambda v, pw=pw, tw=tw, Ax=Ax: v.tensor_tensor(out=tw[:], in0=pw[:, :], in1=Ax[:], op=ALU.mult), reads=[pw, Ax], writes=[tw])
                k.op("pool", lambda g, tw=tw, xt=xt: g.tensor_tensor(out=tw[:], in0=tw[:], in1=xt[:], op=ALU.add), reads=[tw, xt], writes=[tw])
                k.dma("sp", xres.t.ap()[r0:r0 + 128, :], tw[:], reads=[tw], writes=[xres_b[(j, ti)]])
            k.barrier()

    def mixer_b(l, j):
        lam_init = 0.8 - 0.6 * math.exp(-0.3 * l)
        scale = 32.0 ** -0.5
        with ExitStack() as e2:
            qk = [k.sb("qk", [128, PW], BF16, e2) for _ in range(4)]
            va = k.sb("va", [128, 18, 4, 128], BF16, e2)
            k.op("pool", lambda g: g.memset(va[:], 1.0), writes=[va])
            neglam = k.sb("neglam", [128, 1], F32, e2)
            sgv = k.sb("sgv", [128, 1], F32, e2)
            with ExitStack() as e3:
                wg = load_w_in(e3, l, 512, 768)
                rotb = k.sb("rotb", [128, 128], BF16, e3)
                k.dma("sp", rotb[:], IN("rot_b"), writes=[rotb])
                cosT = k.sb("cosT", [128, PW], F32, e3)
                sinT = k.sb("sinT", [128, PW], F32, e3)
                k.dma("sp", cosT[:], IN("rope_cos"), writes=[cosT])
                k.dma("sp", sinT[:], IN("rope_sin"), writes=[sinT])
                pp = [k.ps("ppb", [128, 512], F32, e3) for _ in range(8)]
                lq = k.sb("lq", [128, 4, 32], F32, e3)
                for i_, nme in enumerate(("b_lq1", "b_lk1", "b_lq2", "b_lk2")):
                    k.dma("sp", lq[:, i_, :], IN(nme)[l:l + 1, :].to_broadcast([128, 32]), writes=[lq])
                s12 = k.sb("s12", [128, 2], F32, e3)
                pr_ = k.sb("pr_", [128, 32], F32, e3)
                for i_ in range(2):
                    k.op("dve", lambda v, i_=i_: v.tensor_tensor(out=pr_[:], in0=lq[:, 2 * i_, :], in1=lq[:, 2 * i_ + 1, :], op=ALU.mult), reads=[lq], writes=[pr_])
                    k.op("dve", lambda v, i_=i_: v.reduce_sum(out=s12[:, i_:i_ + 1], in_=pr_[:], axis=AX.X), reads=[pr_], writes=[s12])
                k.op("act", lambda a: a.activation(out=s12[:], in_=s12[:], func=AF.Exp), reads=[s12], writes=[s12])
                k.op("dve", lambda v: v.tensor_tensor(out=neglam[:], in0=s12[:, 1:2], in1=s12[:, 0:1], op=ALU.subtract), reads=[s12], writes=[neglam])
                k.op("dve", lambda v: v.tensor_scalar(out=neglam[:], in0=neglam[:], scalar1=-lam_init, scalar2=None, op0=ALU.add), reads=[neglam], writes=[neglam])
                k.dma("sp", sgv[:], IN("vecB")[l], writes=[sgv])
                k.op("dve", lambda v: v.tensor_scalar(out=sgv[:], in0=sgv[:], scalar1=1.0 - lam_init, scalar2=None, op0=ALU.mult), reads=[sgv], writes=[sgv])
                if os.environ.get('KSTOP') == 'ba':
                    k.barrier(); return
                qbs = [k.sb("qb", [128, 512], BF16, e3) for _ in range(2)]
                t1s = [k.sb("t1b", [128, 512], F32, e3) for _ in range(2)]
                t2s = [k.sb("t2b", [128, 512], F32, e3) for _ in range(2)]
                cnt = 0
                for oc in range(4):
                    for (c0, n) in FULL_TILES:
                        ps, p2 = rot(pp), rot(pp)
                        qb, t1, t2 = qbs[cnt % 2], t1s[cnt % 2], t2s[cnt % 2]
                        cnt += 1

                        def f(pe, ps=ps, c0=c0, n=n, oc=oc):
                            for kc in range(8):
                                ins = pe.matmul(ps[:, 0:n], wg[:, kc, oc * 128:oc * 128 + 128], hT[:, kc, c0:c0 + n], start=(kc == 0), stop=(kc == 7))
                            return ins
                        k.op("pe", f, reads=[wg, hT], writes=[ps])
                        k.op("act", lambda a, ps=ps, n=n, qb=qb: a.copy(qb[:, 0:n], ps[:, 0:n]), reads=[ps], writes=[qb])
                        k.op("dve", lambda v, ps=ps, n=n, t1=t1, c0=c0: v.tensor_tensor(out=t1[:, 0:n], in0=ps[:, 0:n], in1=cosT[:, c0:c0 + n], op=ALU.mult), reads=[ps, cosT, qb], writes=[t1])
                        k.op("pe", lambda pe, p2=p2, n=n, qb=qb: pe.matmul(p2[:, 0:n], rotb[:, :], qb[:, 0:n], start=True, stop=True), reads=[rotb, qb], writes=[p2])
                        k.op("dve", lambda v, p2=p2, n=n, t2=t2, c0=c0: v.tensor_tensor(out=t2[:, 0:n], in0=p2[:, 0:n], in1=sinT[:, c0:c0 + n], op=ALU.mult), reads=[p2, sinT], writes=[t2])
                        k.op("dve" if os.environ.get("KPOOL") else "pool", lambda g, n=n, t1=t1, t2=t2, oc=oc, c0=c0: g.tensor_tensor(out=qk[oc][:, c0:c0 + n], in0=t1[:, 0:n], in1=t2[:, 0:n], op=ALU.add), reads=[t1, t2], writes=[qk[oc]])
                        if os.environ.get('KSTOP') == 'bb':
                            k.barrier(); return
                if os.environ.get('KSTOP') == 'b0':
                    k.barrier(); return
                for ti, (col, isctx) in enumerate(TT):
                    ps = rot(pp)

                    def f(pe, ps=ps, col=col):
                        for kc in range(8):
                            ins = pe.matmul(ps[:, 0:256], hT[:, kc, col:col + 128], wg[:, kc, 512:768], start=(kc == 0), stop=(kc == 7))
                        return ins
                    k.op("pe", f, reads=[wg, hT], writes=[ps])
                    for h in range(4):
                        dst = va[:, ti, h, 0:64] if h % 2 == 0 else va[:, ti, h, 64:128]
                        k.op("dve", lambda v, ps=ps, h=h, dst=dst: v.tensor_copy(dst, ps[:, h * 64:(h + 1) * 64]), reads=[ps], writes=[va])
                k.barrier()
            if os.environ.get('KSTOP') == 'b1':
                return
            with ExitStack() as e3:
                pS = [k.ps("pS", [128, 1024], F32, e3) for _ in range(2)]
                pO = k.ps("pO", [128, 1024], F32, e3)
                pX = [k.ps("pX", [128, 512], F32, e3) for _ in range(2)]
                Eb = [k.sb("Eb", [128, 1024], BF16, e3) for _ in range(2)]
                ob = [k.sb("ob", [128, PW], F32, e3) for _ in range(2)]
                o1 = k.sb("o1", [128, PW], F32, e3)
                rdt = [k.sb("rdt", [128, 512], F32, e3) for _ in range(2)]
                for h in range(4):
                    ch, hl = h // 2, h % 2
                    nr = slice(0, 64) if hl == 0 else slice(64, 128)
                    dr = slice(64, 128) if hl == 0 else slice(0, 64)
                    for c in range(2):
                        base = hl * 64 + c * 32
                        dest = ob[ch] if c == 0 else o1
                        qq, kk_ = qk[ch], qk[2 + ch]
                        segs = [(L0, 1024, 18), (L0 + 1024, 1024, 18), (C0, 256, 2)]
                        for (q0, qn, nkt) in segs:
                            nsub = (qn + 511) // 512
                            def emit_S(kt, q0=q0, qn=qn, nsub=nsub, base=base, qq=qq, kk_=kk_):
                                kcol = TT[kt][0]
                                ps_ = pS[kt % 2]

                                def f(pe):
                                    for i_ in range(nsub):
                                        w_ = min(512, qn - i_ * 512)
                                        ins = pe.matmul(ps_[:, i_ * 512:i_ * 512 + w_], kk_[base:base + 32, kcol:kcol + 128], qq[base:base + 32, q0 + i_ * 512:q0 + i_ * 512 + w_],
                                                        start=True, stop=True, tile_position=(base, 0))
                                    return ins
                                k.op("pe", f, reads=[qq, kk_], writes=[ps_])

                            def emit_E(kt, qn=qn):
                                ps_, E_ = pS[kt % 2], Eb[kt % 2]
                                k.op("act", lambda a: a.activation(out=E_[:, 0:qn], in_=ps_[:, 0:qn], func=AF.Exp, scale=scale), reads=[ps_], writes=[E_])

                            def emit_PV(kt, nkt=nkt, qn=qn, nsub=nsub, h=h):
                                E_ = Eb[kt % 2]

                                def f2(pe):
                                    for i_ in range(nsub):
                                        w_ = min(512, qn - i_ * 512)
                                        ins = pe.matmul(pO[:, i_ * 512:i_ * 512 + w_], va[:, kt, h, :], E_[:, i_ * 512:i_ * 512 + w_], start=(kt == 0), stop=(kt == nkt - 1))
                                    return ins
                                k.op("pe", f2, reads=[va, E_], writes=[pO])
                            emit_S(0)
                            for kt in range(nkt):
                                emit_E(kt)
                                if kt + 1 < nkt:
                                    emit_S(kt + 1)
                                emit_PV(kt)
                            for i_ in range(nsub):
                                w_ = min(512, qn - i_ * 512)
                                rd = rdt[i_ % 2]
                                k.op("dve", lambda v, rd=rd, i_=i_, w_=w_, dr=dr: v.reciprocal(out=rd[dr, 0:w_], in_=pO[dr, i_ * 512:i_ * 512 + w_]), reads=[pO], writes=[rd])
                                k.op("dve", lambda v, rd=rd, i_=i_, w_=w_, dr=dr, nr=nr, dest=dest, q0=q0: v.tensor_tensor(
                                    out=dest[nr, q0 + i_ * 512:q0 + i_ * 512 + w_], in0=pO[nr, i_ * 512:i_ * 512 + w_], in1=rd[dr, 0:w_], op=ALU.mult), reads=[pO, rd], writes=[dest])
                    for (c0, n) in VT:
                        k.op("dve", lambda v, c0=c0, n=n, nr=nr, ch=ch: v.scalar_tensor_tensor(out=ob[ch][nr, c0:c0 + n], in0=o1[nr, c0:c0 + n], scalar=neglam[nr, 0:1], in1=ob[ch][nr, c0:c0 + n],
                                                                                            op0=ALU.mult, op1=ALU.add), reads=[o1, neglam, ob[ch]], writes=[ob[ch]])
                sqs = [k.sb("sqb", [128, 512], F32, e3) for _ in range(2)]
                for ch in range(2):
                    for vi, (c0, n) in enumerate(VT):
                        sq, px = sqs[vi % 2], pX[vi % 2]
                        k.op("pool", lambda g, sq=sq, c0=c0, n=n, ch=ch: g.tensor_tensor(out=sq[:, 0:n], in0=ob[ch][:, c0:c0 + n], in1=ob[ch][:, c0:c0 + n], op=ALU.mult), reads=[ob[ch]], writes=[sq])
                        k.op("pe", lambda pe, sq=sq, px=px, n=n: pe.matmul(px[:, 0:n], blk64[:, :], sq[:, 0:n], start=True, stop=True), reads=[blk64, sq], writes=[px])
                        k.op("act", lambda a, sq=sq, px=px, n=n: a.activation(out=sq[:, 0:n], in_=px[:, 0:n], func=AF.Sqrt, scale=1.0 / 64, bias=eps6[:, 0:1]), reads=[px, eps6], writes=[sq])
                        k.op("dve", lambda v, sq=sq, n=n: v.reciprocal(out=sq[:, 0:n], in_=sq[:, 0:n]), reads=[sq], writes=[sq])
                        k.op("dve", lambda v, sq=sq, c0=c0, n=n, ch=ch: v.scalar_tensor_tensor(out=yT[:, 2 + ch, c0:c0 + n], in0=ob[ch][:, c0:c0 + n], scalar=sgv[:, 0:1], in1=sq[:, 0:n],
                                                                                         op0=ALU.mult, op1=ALU.mult), reads=[ob[ch], sgv, sq], writes=[yT])
                k.barrier()

    SEGS = {"l": (L0, L, 16), "c": (C0, LC, 2)}

    def hyena_prep(l):
        TWO_PI = 2.0 * math.pi
        for tag, (col0, Lx, nch) in SEGS.items():
            with ExitStack() as e2:
                pp = [k.ps("pph", [128, 512], F32, e2) for _ in range(4)]
                pe_ = [k.ps("ppe", [128, 512], F32, e2) for _ in range(2)]
                feats = k.sb("feats", [33, Lx], F32, e2)
                k.dma("sp", feats[:], IN("feats_" + tag), writes=[feats])
                wf1 = k.sb("wf1", [33, 64], F32, e2)
                wf2 = k.sb("wf2", [64, 64], F32, e2)
                wf3 = k.sb("wf3", [64, 1024], F32, e2)
                k.dma("sp", wf1[:], IN("d_w_f1")[l], writes=[wf1])
                k.dma("sp", wf2[:], IN("d_w_f2")[l], writes=[wf2])
                k.dma("sp", wf3[:], IN("d_w_f3")[l], writes=[wf3])
                hv = k.sb("hv", [64, 3], F32, e2)
                k.dma("sp", hv[:], IN("hyv")[l], writes=[hv])
                fb = k.sb("fb", [64, 2], F32, e2)
                k.op("dve", lambda v: v.tensor_tensor(out=fb[:, 0:1], in0=hv[:, 0:1], in1=hv[:, 1:2], op=ALU.mult), reads=[hv], writes=[fb])
                k.op("dve", lambda v: v.tensor_tensor(out=fb[:, 1:2], in0=hv[:, 2:3], in1=hv[:, 1:2], op=ALU.mult), reads=[hv], writes=[fb])
                ntu = k.sb("ntu", [128, nch], F32, e2)
                k.dma("sp", ntu[:], IN("ntu_" + tag), writes=[ntu])
                dec = k.sb("dec", [128, 1024], F32, e2)
                dec2 = k.sb("dec2", [128, 1024], F32, e2)
                k.dma("sp", dec[:], IN("d_decay")[l:l + 1, :].to_broadcast([128, 1024]), writes=[dec])
                k.op("dve", lambda v: v.tensor_scalar(out=dec2[:], in0=dec[:], scalar1=-1.0, scalar2=None, op0=ALU.mult), reads=[dec], writes=[dec2])
                k.op("dve", lambda v: v.tensor_tensor(out=dec[:], in0=dec[:], in1=dec2[:], op=ALU.max), reads=[dec, dec2], writes=[dec])
                f1 = k.sb("f1", [64, Lx], F32, e2)
                f2 = k.sb("f2", [64, Lx], F32, e2)
                arg = k.sb("arg", [64, 512], F32, e2)
                ki = k.sb("ki", [64, 512], I32, e2)
                kf = k.sb("kf", [64, 512], F32, e2)
                nt = min(512, Lx)
                for (src, w_, dst, bcol, K_) in ((feats, wf1, f1, 0, 33), (f1, wf2, f2, 1, 64)):
                    for t0 in range(0, Lx, nt):
                        ps = rot(pp)
                        k.op("pe", lambda pe, ps=ps, src=src, w_=w_, t0=t0, K_=K_: pe.matmul(ps[0:64, 0:nt], w_[0:K_, :], src[0:K_, t0:t0 + nt], start=True, stop=True), reads=[w_, src], writes=[ps])
                        k.op("act", lambda a, ps=ps, bcol=bcol: a.activation(out=arg[:, 0:nt], in_=ps[0:64, 0:nt], func=AF.Identity, scale=hv[:, 1:2], bias=fb[:, bcol:bcol + 1]), reads=[ps, hv, fb], writes=[arg])
                        k.op("dve", lambda v: v.tensor_scalar(out=ki[:, 0:nt], in0=arg[:, 0:nt], scalar1=1.0 / TWO_PI, scalar2=None, op0=ALU.mult), reads=[arg], writes=[ki])
                        k.op("dve", lambda v: v.tensor_copy(kf[:, 0:nt], ki[:, 0:nt]), reads=[ki], writes=[kf])
                        k.op("dve", lambda v: v.scalar_tensor_tensor(out=arg[:, 0:nt], in0=kf[:, 0:nt], scalar=-TWO_PI, in1=arg[:, 0:nt], op0=ALU.mult, op1=ALU.add), reads=[kf, arg], writes=[arg])
                        k.op("dve", lambda v: v.tensor_scalar(out=arg[:, 0:nt], in0=arg[:, 0:nt], scalar1=3.14159, scalar2=-3.14159, op0=ALU.min, op1=ALU.max), reads=[arg], writes=[arg])
                        k.op("act", lambda a, dst=dst, t0=t0: a.activation(out=dst[:, t0:t0 + nt], in_=arg[:, 0:nt], func=AF.Sin), reads=[arg], writes=[dst])
                hrs = [k.sb("hr", [128, 1024], F32, e2) for _ in range(2)]
                ed = k.sb("ed", [128, 1024], F32, e2)
                sq = k.sb("sqh", [128, 1024], F32, e2)
                hsd = k.sb("hsd", [128, nch, 2, 512], BF16, e2)
                pen = pe_[0]
                for tc in range(nch):
                    hr = hrs[tc % 2]
                    k.op("act", lambda a, tc=tc: a.activation(out=ed[:], in_=dec[:], func=AF.Exp, scale=ntu[:, tc:tc + 1]), reads=[dec, ntu], writes=[ed])
                    for hh in range(2):
                        ps = rot(pp)
                        k.op("pe", lambda pe, ps=ps, tc=tc, hh=hh: pe.matmul(ps[:, :], f2[0:64, tc * 128:(tc + 1) * 128], wf3[0:64, hh * 512:(hh + 1) * 512], start=True, stop=True), reads=[f2, wf3], writes=[ps])
                        k.op("dve", lambda v, ps=ps, hr=hr, hh=hh: v.tensor_tensor(out=hr[:, hh * 512:(hh + 1) * 512], in0=ps[:, :], in1=ed[:, hh * 512:(hh + 1) * 512], op=ALU.mult), reads=[ps, ed], writes=[hr])
                    if tc == 0:
                        for o_ in range(2):
                            k.op("dve", lambda v, o_=o_, hr=hr: v.memset(hr[0:1, o_ * 512 + 256:o_ * 512 + 512], 0.0), reads=[hr], writes=[hr])
                    k.op("pool", lambda g, hr=hr: g.tensor_tensor(out=sq[:], in0=hr[:], in1=hr[:], op=ALU.mult), reads=[hr], writes=[sq])

                    def f(pe, tc=tc):
                        for o_ in range(2):
                            for dd in range(2):
                                ins = pe.matmul(pe_[o_][:, 0:256], ones_f[:, :], sq[:, o_ * 512 + dd * 256:o_ * 512 + dd * 256 + 256],
                                                start=(tc == 0 and dd == 0), stop=(tc == nch - 1 and dd == 1))
                        return ins
                    k.op("pe", f, reads=[ones_f, sq], writes=[pe_[0], pe_[1]])
                    for o_ in range(2):
                        fw_ = hr[:, o_ * 512:o_ * 512 + 256]
                        bw_ = hr[:, o_ * 512 + 256:o_ * 512 + 512]
                        k.op("pool", lambda g, tc=tc, o_=o_, fw_=fw_, bw_=bw_: g.tensor_tensor(out=hsd[:, tc, 0, o_ * 256:(o_ + 1) * 256], in0=fw_, in1=bw_, op=ALU.add), reads=[hr], writes=[hsd])
                        k.op("dve", lambda v, tc=tc, o_=o_, fw_=fw_, bw_=bw_: v.tensor_tensor(out=hsd[:, tc, 1, o_ * 256:(o_ + 1) * 256], in0=bw_, in1=fw_, op=ALU.subtract), reads=[hr], writes=[hsd])
                rn = k.sb("rn", [128, 512], F32, e2)
                for o_ in range(2):
                    k.op("act", lambda a, o_=o_: a.activation(out=rn[:, o_ * 256:(o_ + 1) * 256], in_=pe_[o_][:, 0:256], func=AF.Sqrt, bias=eps6[:, 0:1]), reads=[pe_[o_], eps6], writes=[rn])
                k.op("dve", lambda v: v.reciprocal(out=rn[:], in_=rn[:]), reads=[rn], writes=[rn])
                cfs = [k.sb("cfp", [128, nch, 128], BF16, e2) for _ in range(2)]
                sfs = [k.sb("sfp", [128, nch, 128], BF16, e2) for _ in range(2)]
                hks = [k.sb("hk", [128, 2, 512], F32, e2) for _ in range(2)]
                for fc in range(nch):
                    cf, sf, hk = cfs[fc % 2], sfs[fc % 2], hks[fc % 2]
                    k.dma("sp", cf[:], IN("cf_" + tag)[fc], writes=[cf])
                    k.dma("sp", sf[:], IN("sf_" + tag)[fc], writes=[sf])
                    pr_, pi_ = rot(pp), rot(pp)

                    def f(pe, cf=cf, sf=sf, pr_=pr_, pi_=pi_):
                        for tc in range(nch):
                            pe.matmul(pr_[:, :], cf[:, tc, :], hsd[:, tc, 0, :], start=(tc == 0), stop=(tc == nch - 1))
                        for tc in range(nch):
                            ins = pe.matmul(pi_[:, :], sf[:, tc, :], hsd[:, tc, 1, :], start=(tc == 0), stop=(tc == nch - 1))
                        return ins
                    k.op("pe", f, reads=[cf, sf, hsd], writes=[pr_, pi_])
                    k.op("dve", lambda v, hk=hk, pr_=pr_: v.tensor_tensor(out=hk[:, 0, :], in0=pr_[:, :], in1=rn[:], op=ALU.mult), reads=[pr_, rn], writes=[hk])
                    k.op("dve", lambda v, hk=hk, pi_=pi_: v.tensor_tensor(out=hk[:, 1, :], in0=pi_[:, :], in1=rn[:], op=ALU.mult), reads=[pi_, rn], writes=[hk])
                    k.dma("sp", Hd[tag].t.ap()[:, fc * 128:(fc + 1) * 128, :].rearrange("r p n -> p r n"), hk[:], reads=[hk], writes=[Hd[tag]])
                k.barrier()

    def mixer_d(l, j):
        with ExitStack() as e2:
            pdb = [k.sb("pdb", [128, PW], BF16, e2) for _ in range(6)]
            z = [k.sb("zD", [128, PW], F32, e2) for _ in range(2)]
            vD = k.sb("vD", [128, 6, 4], F32, e2)
            vD2 = k.sb("vD2", [128, 4], F32, e2)
            k.dma("sp", vD[:], IN("vecD")[l], writes=[vD])
            k.dma("sp", vD2[:], IN("vecD2")[l], writes=[vD2])
            dg = k.sb("dgD", [128, 18, 128], BF16, e2)
            for c in range(6):
                for kk in range(3):
                    k.op("dve", lambda v, c=c, kk=kk: v.tensor_scalar(out=dg[:, c * 3 + kk, :], in0=ident_f[:], scalar1=vD[:, c, kk:kk + 1], scalar2=None, op0=ALU.mult), reads=[ident_f, vD], writes=[dg])
            with ExitStack() as e3:
                wg = load_w_in(e3, l, 1792, 768)
                pp = [k.ps("ppd0", [128, 512], F32, e3) for _ in range(4)]
                for oc in range(6):
                    proj(wg, oc * 128, pp, lambda ps, c0, n, oc=oc: k.op("act" if oc % 2 == 0 else "dve",
                         (lambda a: a.copy(pdb[oc][:, c0:c0 + n], ps[:, 0:n])) if oc % 2 == 0 else (lambda v: v.tensor_copy(pdb[oc][:, c0:c0 + n], ps[:, 0:n])), reads=[ps], writes=[pdb[oc]]))
                k.barrier()
            pp = [k.ps("ppd", [128, 512], F32, e2) for _ in range(6)]
            ptr = [k.ps("ptr", [128, 4, 128], BF16, e2) for _ in range(2)]

            def sconv(c, c0, n, ps):
                def f(pe):
                    for kk in range(3):
                        ins = pe.matmul(ps[:, 0:n], dg[:, c * 3 + kk, :], pdb[c][:, c0 + kk - 1:c0 + kk - 1 + n], start=(kk == 0), stop=(kk == 2))
                    return ins
                k.op("pe", f, reads=[dg, pdb[c]], writes=[ps])
            for c in range(2):
                for (c0, n) in VT:
                    ps = rot(pp)
                    sconv(c, c0, n, ps)
                    k.op("act", lambda a, ps=ps, c=c, c0=c0, n=n: a.activation(out=z[c][:, c0:c0 + n], in_=ps[:, 0:n], func=AF.Identity, bias=vD[:, c, 3:4]), reads=[ps, vD], writes=[z[c]])
            zb = k.sb("zb", [128, 2, L], BF16, e2)
            zT = k.sb("zT", [128, 16, 256], BF16, e2)
            Y = k.sb("Yd", [128, 32, 256], BF16, e2)
            cfs = [k.sb("cfd", [128, 16, 128], BF16, e2) for _ in range(2)]
            sfs = [k.sb("sfd", [128, 16, 128], BF16, e2) for _ in range(2)]
            hks = [k.sb("hkd", [128, 2, 256], F32, e2) for _ in range(2)]
            tms = [k.sb("tmd", [128, 256], F32, e2) for _ in range(4)]
            cis = [k.sb("cid", [128, 4, 512], BF16, e2) for _ in range(2)]
            sis = [k.sb("sid", [128, 4, 512], BF16, e2) for _ in range(2)]
            gts = [k.sb("gtd", [128, 512], F32, e2) for _ in range(2)]
            tts = [k.sb("ttd", [128, 512], F32, e2) for _ in range(2)]
            for o_ in range(2):
                for tag, (col0, Lx, nch) in SEGS.items():
                    for c in range(2):
                        k.op("act", lambda a, c=c, col0=col0, Lx=Lx: a.copy(zb[:, c, 0:Lx], z[c][:, col0:col0 + Lx]), reads=[z[c]], writes=[zb])
                    for tc in range(nch):
                        pt = ptr[tc % 2]

                        def f(pe, pt=pt, tc=tc):
                            for c in range(2):
                                ins = pe.transpose(pt[:, c, :], zb[:, c, tc * 128:(tc + 1) * 128], ident_b[:, :])
                            return ins
                        k.op("pe", f, reads=[zb, ident_b], writes=[pt])
                        k.op("dve", lambda v, pt=pt, tc=tc: v.tensor_copy(zT[:, tc, :], pt[:, 0:2, :]), reads=[pt], writes=[zT])
                    for fc in range(nch):
                        cf, sf, hk = cfs[fc % 2], sfs[fc % 2], hks[fc % 2]
                        k.dma("sp", cf[:, 0:nch, :], IN("cf_" + tag)[fc], writes=[cf])
                        k.dma("sp", sf[:, 0:nch, :], IN("sf_" + tag)[fc], writes=[sf])
                        k.dma("sp", hk[:], Hd[tag].t.ap()[:, fc * 128:(fc + 1) * 128, o_ * 256:(o_ + 1) * 256].rearrange("r p n -> p r n"), reads=[Hd[tag]], writes=[hk])
                        pa, pb = rot(pp), rot(pp)

                        def f(pe, cf=cf, sf=sf, pa=pa, pb=pb, nch=nch):
                            for tc in range(nch):
                                pe.matmul(pa[:, 0:256], cf[:, tc, :], zT[:, tc, :], start=(tc == 0), stop=(tc == nch - 1))
                            for tc in range(nch):
                                ins = pe.matmul(pb[:, 0:256], sf[:, tc, :], zT[:, tc, :], start=(tc == 0), stop=(tc == nch - 1))
                            return ins
                        k.op("pe", f, reads=[cf, sf, zT], writes=[pa, pb])
                        t1, t2, t3, t4 = tms
                        k.op("dve", lambda v, pa=pa, hk=hk: v.tensor_tensor(out=t1[:], in0=pa[:, 0:256], in1=hk[:, 0, :], op=ALU.mult), reads=[pa, hk], writes=[t1])
                        k.op("dve", lambda v, pb=pb, hk=hk: v.tensor_tensor(out=t2[:], in0=pb[:, 0:256], in1=hk[:, 1, :], op=ALU.mult), reads=[pb, hk], writes=[t2])
                        k.op("pool", lambda g, fc=fc: g.tensor_tensor(out=Y[:, fc, :], in0=t1[:], in1=t2[:], op=ALU.add), reads=[t1, t2], writes=[Y])
                        k.op("dve", lambda v, pa=pa, hk=hk: v.tensor_tensor(out=t3[:], in0=pa[:, 0:256], in1=hk[:, 1, :], op=ALU.mult), reads=[pa, hk], writes=[t3])
                        k.op("dve", lambda v, pb=pb, hk=hk: v.tensor_tensor(out=t4[:], in0=pb[:, 0:256], in1=hk[:, 0, :], op=ALU.mult), reads=[pb, hk], writes=[t4])
                        k.op("pool", lambda g, fc=fc, nch=nch: g.tensor_tensor(out=Y[:, nch + fc, :], in0=t3[:], in1=t4[:], op=ALU.subtract), reads=[t3, t4], writes=[Y])
                    nt = min(512, Lx)
                    for ti_, t0 in enumerate(range(0, Lx, nt)):
                        py = [rot(pp), rot(pp)]
                        ngrp = (nch + 3) // 4
                        for g_ in range(ngrp):
                            ci, si = cis[g_ % 2], sis[g_ % 2]
                            nf = min(4, nch - g_ * 4)
                            k.dma("sp", ci[:, 0:nf, 0:nt], IN("ci_" + tag)[:, g_ * 4:g_ * 4 + nf, t0:t0 + nt], writes=[ci])
                            k.dma("sp", si[:, 0:nf, 0:nt], IN("si_" + tag)[:, g_ * 4:g_ * 4 + nf, t0:t0 + nt], writes=[si])

                            def f(pe, ci=ci, si=si, g_=g_, nf=nf, py=py, nch=nch, ngrp=ngrp):
                                for c in range(2):
                                    for ff in range(nf):
                                        fc = g_ * 4 + ff
                                        pe.matmul(py[c][:, 0:nt], Y[:, fc, c * 128:(c + 1) * 128], ci[:, ff, 0:nt], start=(fc == 0), stop=False)
                                        ins = pe.matmul(py[c][:, 0:nt], Y[:, nch + fc, c * 128:(c + 1) * 128], si[:, ff, 0:nt], start=False, stop=(fc == nch - 1))
                                return ins
                            k.op("pe", f, reads=[Y, ci, si], writes=[py[0], py[1]])
                        c0 = col0 + t0
                        for c in range(2):
                            gc = 2 + 2 * o_ + c
                            pg = rot(pp)
                            sconv(gc, c0, nt, pg)
                            gt, tt_ = gts[c], tts[c]
                            k.op("act", lambda a, pg=pg, gt=gt, gc=gc: a.activation(out=gt[:, 0:nt], in_=pg[:, 0:nt], func=AF.Identity, bias=vD[:, gc, 3:4]), reads=[pg, vD], writes=[gt])
                            k.op("dve", lambda v, c=c, tt_=tt_, c0=c0, py=py, o_=o_: v.scalar_tensor_tensor(out=tt_[:, 0:nt], in0=z[c][:, c0:c0 + nt], scalar=vD2[:, o_ * 2 + c:o_ * 2 + c + 1], in1=py[c][:, 0:nt],
                                                                                                op0=ALU.mult, op1=ALU.add), reads=[z[c], vD2, py[c]], writes=[tt_])
                            if o_ == 0:
                                k.op("pool", lambda g, c=c, tt_=tt_, gt=gt, c0=c0: g.tensor_tensor(out=z[c][:, c0:c0 + nt], in0=tt_[:, 0:nt], in1=gt[:, 0:nt], op=ALU.mult), reads=[tt_, gt, zb], writes=[z[c]])
                            else:
                                k.op("pool", lambda g, c=c, tt_=tt_, gt=gt, c0=c0: g.tensor_tensor(out=yT[:, 6 + c, c0:c0 + nt], in0=tt_[:, 0:nt], in1=gt[:, 0:nt], op=ALU.mult), reads=[tt_, gt], writes=[yT])
            k.barrier()

    def ffn_phase(l, last):
        tiles = [(j, ti) for j in range(nseq) for ti in (range(2, 18) if last else range(18))]
        nt = len(tiles)
        NBk = (2 * nt * 128) // MOE_S + NE
        wgT = IN("moe_w_gate").rearrange("l e (p j) n -> (l e p) (j n)", j=8)
        wuT = IN("moe_w_up").rearrange("l e (p j) n -> (l e p) (j n)", j=8)
        wdT = IN("moe_w_down").rearrange("l e (p j) n -> (l e p) (j n)", j=4)
        hrow_b = [Buf(hrow.t) for _ in range(nt)]
        xs_w = [Buf(xs.t) for _ in range(2 * nt)]
        ysl_b = [Buf(yslot.t) for _ in range(NBk)]
        with ExitStack() as e2:
            zt = k.sb("zt", [128, 2 * D], BF16, e2)
            k.op("pool", lambda g: g.memset(zt[:], 0.0), writes=[zt])
            nfill = 2 * NBk
            xs_zb = [Buf(xs.t) for _ in range(nfill)]
            fill_next = [0]

            def fill_xs(n_):
                for _ in range(n_):
                    i_ = fill_next[0]
                    if i_ >= nfill:
                        return
                    fill_next[0] += 1
                    k.dma("sp", xs.t.ap()[0:NBk * MOE_S, :].rearrange("(p r) d -> p r d", p=128)[:, 2 * i_:2 * i_ + 2, :], zt[:].rearrange("p (r d) -> p r d", r=2),
                          reads=[zt], writes=([xs_z, xs_zb[i_]] if i_ == 0 else [xs_zb[i_]]))
            nper_fill = (nfill + nt - 1) // nt
            GT = k.sb("GT", [128, nt, 2], F32, e2)
            DST = k.sb("DST", [128, nt, 2], F32, e2)
            DSTi = k.sb("DSTi", [128, nt, 2], I32, e2)
            run = k.sb("run", [128, 32], F32, e2)
            tokid = k.sb("tokid", [128, NB_ * 18], I32, e2)
            iow = k.sb("iow", [128, 16], F32, e2)
            blkS = k.sb("blkS", [128, 80], F32, e2)
            nbi = k.sb("nbi", [128, 32], I32, e2)
            pad_ = k.sb("pad_", [128, 32], F32, e2)
            pend = k.sb("pend", [128, 32], F32, e2)
            pst_ = k.sb("pst_", [128, 32], F32, e2)
            tq = k.sb("tq", [128, 32], F32, e2)
            tq2 = k.sb("tq2", [128, 32], F32, e2)
            bacc = k.sb("bacc", [128, NBk], F32, e2)
            be1 = k.sb("be1", [128, NBk], F32, e2)
            be2 = k.sb("be2", [128, NBk], F32, e2)
            WI = k.sb("WI", [128, NBk], I32, e2)
            zi = k.sb("zi", [128, 512], I32, e2)
            hb2s = [k.sb("hb2", [128, D], BF16, e2) for _ in range(2)]
            eR = ExitStack()
            OH1 = k.sb("OH1", [128, nt, 32], F32, eR)
            OH2 = k.sb("OH2", [128, nt, 32], F32, eR)
            RK = k.sb("RK", [128, nt, 32], F32, eR)
            T32 = k.sb("T32", [128, nt, 32], F32, eR)
            WIf = k.sb("WIf", [128, NBk], F32, eR)
            k.dma("sp", tokid[:], IN("tokid"), writes=[tokid])
            k.dma("sp", iow[:], IN("iota_w"), writes=[iow])
            k.dma("sp", blkS[:], IN("blkS"), writes=[blkS])
            k.op("dve", lambda v: v.memset(run[:], 0.0), writes=[run])
            with ExitStack() as e3:
                A1 = k.sb("A1f", [128, D], F32, e3)
                A0 = k.sb("A0f", [128, D], F32, e3)
                A1c = k.sb("A1cf", [128, D], F32, e3)
                A0c = k.sb("A0cf", [128, D], F32, e3)
                gtl = k.sb("gtf", [128, D], F32, e3)
                k.dma("sp", gtl[:], IN("g_ffn")[l:l + 1, :].to_broadcast([128, D]), writes=[gtl])

                def fill(A1_, A0_, r):
                    mod_bc(A1_, l, r, 4)
                    mod_bc(A0_, l, r, 3)
                    k.op("dve", lambda v: v.scalar_tensor_tensor(out=A1_[:], in0=A1_[:], scalar=1.0, in1=gtl[:], op0=ALU.add, op1=ALU.mult), reads=[A1_, gtl], writes=[A1_])
                if not last:
                    fill(A1c, A0c, 4)
                wr = k.sb("wr", [128, 8, 36], F32, e3)
                k.dma("sp", wr[:], IN("wr")[l].rearrange("(kc p) n -> p kc n", p=128), writes=[wr])
                brb = k.sb("brb", [128, 36], F32, e3)
                k.dma("sp", brb[:], IN("br")[l:l + 1, :].to_broadcast([128, 36]), writes=[brb])
                utri = k.sb("utri", [128, 128], BF16, e3)
                onesb = k.sb("onesb", [128, 128], BF16, e3)
                k.dma("sp", utri[:], IN("utri_b"), writes=[utri])
                k.dma("sp", onesb[:], IN("ones_b"), writes=[onesb])
                xts = [k.sb("xtm", [128, D], F32, e3) for _ in range(2)]
                hfs = [k.sb("hfm", [128, D], F32, e3) for _ in range(1)] * 2
                hbs = [k.sb("hbm", [128, D], BF16, e3) for _ in range(2)]
                hTfs = [k.sb("hTf", [128, 8, 128], F32, e3) for _ in range(2)]
                tmp = k.sb("tmpm", [128, D], F32, e3)
                sts = [[k.sb("stm", [128, 1], F32, e3) for _ in range(3)] for _ in range(2)]
                ptfs = [k.ps("ptf", [128, 8, 128], F32, e3) for _ in range(2)]
                plgs = [k.ps("plg", [128, 512], F32, e3) for _ in range(2)]
                pR1 = k.ps("pR1", [128, 512], F32, e3)
                pR2 = k.ps("pR2", [128, 512], F32, e3)
                LG = k.sb("LG", [128, nt, 36], F32, e3)
                gm = k.sb("gm", [128, 8], F32, e3)
                ohg = k.sb("ohg", [128, 4], F32, e3)
                eg = k.sb("eg", [128, 4], F32, e3)
                les = k.sb("les", [128, 8], F32, e3)
                oh1 = k.sb("oh1", [128, 8], F32, e3)
                msk = k.sb("msk", [128, 8], F32, e3)
                oh2 = k.sb("oh2", [128, 8], F32, e3)
                Mb = k.sb("Mb", [128, 32], BF16, e3)
                curj = None

                def ldA(t_):
                    j_, ti_ = tiles[t_]
                    r0_ = tok_row(j_, ti_)
                    k.dma("sp", xts[t_ % 2][:], xres.t.ap()[r0_:r0_ + 128, :], reads=[xres_b[(j_, ti_)]], writes=[xts[t_ % 2]])
                for t, (j, ti) in enumerate(tiles):
                    col, isctx = TT[ti]
                    if j != curj:
                        fill(A1, A0, j)
                        curj = j
                    xt, hf, hb, hTf, st, ptf, plg = xts[t % 2], hfs[t % 2], hbs[t % 2], hTfs[t % 2], sts[t % 2], ptfs[t % 2], plgs[t % 2]
                    r0 = tok_row(j, ti)
                    if t == 0:
                        ldA(0)
                    if t + 1 < nt:
                        ldA(t + 1)
                    fill_xs(nper_fill)
                    norm_mod(xt, A1c if isctx else A1, A0c if isctx else A0, tmp, hf, st)
                    k.op("dve", lambda v, hf=hf, hb=hb: v.tensor_copy(hb[:], hf[:]), reads=[hf], writes=[hb])
                    k.dma("sp", hrow.t.ap()[r0:r0 + 128, :], hb[:], reads=[hb], writes=[hrow_b[t]])

                    def f(pe, hf=hf, ptf=ptf):
                        for kc in range(8):
                            ins = pe.transpose(ptf[:, kc, :], hf[:, kc * 128:(kc + 1) * 128], ident_f[:, :])
                        return ins
                    k.op("pe", f, reads=[hf, ident_f], writes=[ptf])
                    k.op("dve", lambda v, ptf=ptf, hTf=hTf: v.tensor_copy(hTf[:], ptf[:, :, :]), reads=[ptf], writes=[hTf])

                    def f(pe, hTf=hTf, plg=plg):
                        for kc in range(8):
                            ins = pe.matmul(plg[:, 0:36], hTf[:, kc, :], wr[:, kc, :], start=(kc == 0), stop=(kc == 7))
                        return ins
                    k.op("pe", f, reads=[hTf, wr], writes=[plg])
                    V = lambda fn, rd, wrr: k.op("dve", fn, reads=rd, writes=wrr)
                    V(lambda v, plg=plg, t=t: v.tensor_tensor(out=LG[:, t, :], in0=plg[:, 0:36], in1=brb[:], op=ALU.add), [plg, brb], [LG])
                S1_ = lambda nm: k.sb(nm, [128, nt, 1], F32, e3)
                GMx, SGs, PGs, M1s, M2s, DEs, P1s = [S1_("r1_%d" % i_) for i_ in range(7)]
                OHG = k.sb("OHG", [128, nt, 4], F32, e3)
                EG = k.sb("EG", [128, nt, 4], F32, e3)
                LES = k.sb("LES", [128, nt, 8], F32, e3)
                T8 = k.sb("T8", [128, nt, 8], F32, e3)
                O1s = k.sb("O1s", [128, nt, 8], F32, e3)
                MSK = k.sb("MSK", [128, nt, 8], F32, e3)
                O2s = k.sb("O2s", [128, nt, 8], F32, e3)
                MbA = k.sb("MbA", [128, nt, 32], BF16, e3)
                bc = lambda ap_, n_: ap_.to_broadcast([128, nt, n_])
                V(lambda v: v.reduce_max(out=GMx[:], in_=LG[:, :, 0:4], axis=AX.X), [LG], [GMx])
                V(lambda v: v.tensor_tensor(out=OHG[:], in0=LG[:, :, 0:4], in1=bc(GMx[:, :, 0:1], 4), op=ALU.is_equal), [LG, GMx], [OHG])
                V(lambda v: v.tensor_tensor(out=EG[:], in0=LG[:, :, 0:4], in1=bc(GMx[:, :, 0:1], 4), op=ALU.subtract), [LG, GMx], [EG])
                k.op("act", lambda a: a.activation(out=EG[:], in_=EG[:], func=AF.Exp), reads=[EG], writes=[EG])
                V(lambda v: v.reduce_sum(out=SGs[:], in_=EG[:], axis=AX.X), [EG], [SGs])
                V(lambda v: v.reciprocal(out=PGs[:], in_=SGs[:]), [SGs], [PGs])
                V(lambda v: v.tensor_tensor(out=LES[:], in0=LG[:, :, 4:12], in1=bc(OHG[:, :, 0:1], 8), op=ALU.mult), [LG, OHG], [LES])
                for g_ in range(1, 4):
                    V(lambda v, g_=g_: v.tensor_tensor(out=T8[:], in0=LG[:, :, 4 + 8 * g_:12 + 8 * g_], in1=bc(OHG[:, :, g_:g_ + 1], 8), op=ALU.mult), [LG, OHG], [T8])
                    V(lambda v: v.tensor_tensor(out=LES[:], in0=LES[:], in1=T8[:], op=ALU.add), [LES, T8], [LES])
                V(lambda v: v.reduce_max(out=M1s[:], in_=LES[:], axis=AX.X), [LES], [M1s])
                V(lambda v: v.tensor_tensor(out=O1s[:], in0=LES[:], in1=bc(M1s[:, :, 0:1], 8), op=ALU.is_equal), [LES, M1s], [O1s])
                V(lambda v: v.scalar_tensor_tensor(out=MSK[:], in0=O1s[:], scalar=-1e30, in1=LES[:], op0=ALU.mult, op1=ALU.add), [O1s, LES], [MSK])
                V(lambda v: v.reduce_max(out=M2s[:], in_=MSK[:], axis=AX.X), [MSK], [M2s])
                V(lambda v: v.tensor_tensor(out=O2s[:], in0=MSK[:], in1=bc(M2s[:, :, 0:1], 8), op=ALU.is_equal), [MSK, M2s], [O2s])
                V(lambda v: v.tensor_tensor(out=DEs[:], in0=M2s[:], in1=M1s[:], op=ALU.subtract), [M1s, M2s], [DEs])
                k.op("act", lambda a: a.activation(out=DEs[:], in_=DEs[:], func=AF.Exp), reads=[DEs], writes=[DEs])
                V(lambda v: v.tensor_scalar(out=P1s[:], in0=DEs[:], scalar1=1.0, scalar2=None, op0=ALU.add), [DEs], [P1s])
                V(lambda v: v.reciprocal(out=P1s[:], in_=P1s[:]), [P1s], [P1s])
                V(lambda v: v.tensor_tensor(out=GT[:, :, 0:1], in0=P1s[:], in1=PGs[:], op=ALU.mult), [P1s, PGs], [GT])
                V(lambda v: v.tensor_tensor(out=GT[:, :, 1:2], in0=GT[:, :, 0:1], in1=DEs[:], op=ALU.mult), [GT, DEs], [GT])
                for g_ in range(4):
                    V(lambda v, g_=g_: v.tensor_tensor(out=OH1[:, :, 8 * g_:8 * g_ + 8], in0=O1s[:], in1=bc(OHG[:, :, g_:g_ + 1], 8), op=ALU.mult), [O1s, OHG], [OH1])
                    V(lambda v, g_=g_: v.tensor_tensor(out=OH2[:, :, 8 * g_:8 * g_ + 8], in0=O2s[:], in1=bc(OHG[:, :, g_:g_ + 1], 8), op=ALU.mult), [O2s, OHG], [OH2])
                V(lambda v: v.tensor_tensor(out=MbA[:], in0=OH1[:], in1=OH2[:], op=ALU.add), [OH1, OH2], [MbA])
                for t in range(nt):
                    k.op("pe", lambda pe, t=t: pe.matmul(pR1[:, 0:32], utri[:, :], MbA[:, t, :], start=True, stop=True), reads=[utri, MbA], writes=[pR1])
                    k.op("pe", lambda pe, t=t: pe.matmul(pR2[:, 0:32], onesb[:, :], MbA[:, t, :], start=True, stop=True), reads=[onesb, MbA], writes=[pR2])
                    V(lambda v, t=t: v.tensor_tensor(out=RK[:, t, :], in0=pR1[:, 0:32], in1=run[:], op=ALU.add), [pR1, run], [RK])
                    V(lambda v: v.tensor_tensor(out=run[:], in0=pR2[:, 0:32], in1=run[:], op=ALU.add), [pR2, run], [run])
                k.barrier()
            V = lambda fn, rd, wrr: k.op("dve", fn, reads=rd, writes=wrr)
            V(lambda v: v.tensor_scalar(out=pad_[:], in0=run[:], scalar1=1.0 / MOE_S, scalar2=(MOE_S - 1.0) / MOE_S - 0.499, op0=ALU.mult, op1=ALU.add), [run], [pad_])
            V(lambda v: v.tensor_copy(nbi[:], pad_[:]), [pad_], [nbi])
            V(lambda v: v.tensor_copy(pad_[:], nbi[:]), [nbi], [pad_])
            V(lambda v: v.tensor_scalar(out=pad_[:], in0=pad_[:], scalar1=float(MOE_S), scalar2=None, op0=ALU.mult), [pad_], [pad_])
            V(lambda v: v.tensor_tensor_scan(out=pend[:], data0=ones_f[:, 0:32], data1=pad_[:], initial=0.0, op0=ALU.mult, op1=ALU.add), [ones_f, pad_], [pend])
            V(lambda v: v.tensor_tensor(out=pst_[:], in0=pend[:], in1=pad_[:], op=ALU.subtract), [pend, pad_], [pst_])
            V(lambda v: v.tensor_tensor(out=RK[:], in0=RK[:], in1=pst_[:, :].unsqueeze(1).to_broadcast([128, nt, 32]), op=ALU.add), [RK, pst_], [RK])
            V(lambda v: v.tensor_tensor(out=T32[:], in0=RK[:], in1=OH1[:], op=ALU.mult), [RK, OH1], [T32])
            V(lambda v: v.reduce_sum(out=DST[:, :, 0:1], in_=T32[:], axis=AX.X), [T32], [DST])
            V(lambda v: v.tensor_tensor(out=T32[:], in0=RK[:], in1=OH2[:], op=ALU.mult), [RK, OH2], [T32])
            V(lambda v: v.reduce_sum(out=DST[:, :, 1:2], in_=T32[:], axis=AX.X), [T32], [DST])
            V(lambda v: v.tensor_copy(DSTi[:], DST[:]), [DST], [DSTi])
            V(lambda v: v.memset(bacc[:], 0.0), [], [bacc])
            for e_ in range(NE):
                V(lambda v, e_=e_: v.scalar_tensor_tensor(out=bacc[:], in0=blkS[:, 0:NBk], scalar=pend[:, e_:e_ + 1], in1=bacc[:], op0=ALU.is_ge, op1=ALU.add), [blkS, pend, bacc], [bacc])
            V(lambda v: v.tensor_scalar(out=bacc[:], in0=bacc[:], scalar1=float(NE - 1), scalar2=None, op0=ALU.min), [bacc], [bacc])
            V(lambda v: v.tensor_scalar(out=be1[:], in0=bacc[:], scalar1=128.0, scalar2=float(l * NE * 128), op0=ALU.mult, op1=ALU.add), [bacc], [be1])
            V(lambda v: v.tensor_scalar(out=WIf[:], in0=be1[:], scalar1=iow[:, 0:1], scalar2=None, op0=ALU.add), [be1, iow], [WIf])
            V(lambda v: v.tensor_copy(WI[:], WIf[:]), [WIf], [WI])
            for t, (j, ti) in enumerate(tiles):
                hb2 = hb2s[t % 2]
                r0 = tok_row(j, ti)
                k.dma("sp", hb2[:], hrow.t.ap()[r0:r0 + 128, :], reads=[hrow_b[t]], writes=[hb2])
                for q_ in range(2):
                    k.idma(xs.t.ap(), bass.IndirectOffsetOnAxis(ap=DSTi[:, t, q_:q_ + 1], axis=0), hb2[:, :], None, reads=[DSTi, hb2, xs_z] + xs_zb, writes=[xs_w[2 * t + q_]])
            if "moe" in dbg:
                o = dbg_out("dst", [128, nt, 2])
                k.dma("sp", o.ap(), DST[:], reads=[DST])
                o = dbg_out("gt", [128, nt, 2])
                k.dma("sp", o.ap(), GT[:], reads=[GT])
                o = dbg_out("blke", [128, NBk])
                k.dma("sp", o.ap(), bacc[:], reads=[bacc])
                o = dbg_out("oh1", [128, nt, 32])
                k.dma("sp", o.ap(), OH1[:], reads=[OH1])
            k.barrier()
            eR.close()
            with ExitStack() as e3:
                wgs = [k.sb("mwg", [128, 8, 512], BF16, e3) for _ in range(2)]
                wus = [k.sb("mwu", [128, 8, 512], BF16, e3) for _ in range(2)]
                wds = [k.sb("mwd", [128, 4, D], BF16, e3) for _ in range(2)]
                stoks = [k.sb("stok", [128, 4], I32, e3) for _ in range(2)]
                xbs = [k.sb("mxb", [128, 4, D], BF16, e3) for _ in range(2)]
                xbTs = [k.sb("mxbT", [128, 8, 512], BF16, e3) for _ in range(2)]
                hids = [k.sb("mhid", [128, 4, 512], BF16, e3) for _ in range(2)]
                sgs = [k.sb("msg", [128, 512], F32, e3) for _ in range(2)]
                ybs = [k.sb("myb", [128, D], F32, e3) for _ in range(2)]
                ptT = [k.ps("mptT", [128, 512], BF16, e3) for _ in range(2)]
                pgu = [k.ps("mpgu", [128, 512], F32, e3) for _ in range(4)]
                pyy = [k.ps("mpy", [128, 512], F32, e3) for _ in range(2)]
                cnt = 0

                def ldX(b_):
                    k.dma("sp", xbs[b_ % 2][:], xs.t.ap()[b_ * MOE_S:(b_ + 1) * MOE_S, :].rearrange("(p a) d -> p a d", a=4), reads=xs_w + xs_zb + [xs_z], writes=[xbs[b_ % 2]])
                for b in range(NBk):
                    wg_, wu_, wd_, stok, xb, xbT, hid = wgs[b % 2], wus[b % 2], wds[b % 2], stoks[b % 2], xbs[b % 2], xbTs[b % 2], hids[b % 2]
                    if b == 0:
                        ldX(0)
                    if b + 1 < NBk:
                        ldX(b + 1)
                    k.idma(wg_[:].rearrange("p j n -> p (j n)"), None, wgT, bass.IndirectOffsetOnAxis(ap=WI[:, b:b + 1], axis=0), reads=[WI], writes=[wg_])
                    k.idma(wu_[:].rearrange("p j n -> p (j n)"), None, wuT, bass.IndirectOffsetOnAxis(ap=WI[:, b:b + 1], axis=0), reads=[WI], writes=[wu_])
                    k.idma(wd_[:].rearrange("p j n -> p (j n)"), None, wdT, bass.IndirectOffsetOnAxis(ap=WI[:, b:b + 1], axis=0), reads=[WI], writes=[wd_])
                    for kc in range(8):
                        pt = ptT[kc % 2]

                        def f(pe, pt=pt, kc=kc, xb=xb):
                            for a_ in range(4):
                                ins = pe.transpose(pt[:, a_ * 128:(a_ + 1) * 128], xb[:, a_, kc::8], ident_b[:, :])
                            return ins
                        k.op("pe", f, reads=[xb, ident_b], writes=[pt])
                        if kc % 2 == 0:
                            k.op("act", lambda a, pt=pt, kc=kc, xbT=xbT: a.copy(xbT[:, kc, :], pt[:, :]), reads=[pt], writes=[xbT])
                        else:
                            k.op("dve", lambda v, pt=pt, kc=kc, xbT=xbT: v.tensor_copy(xbT[:, kc, :], pt[:, :]), reads=[pt], writes=[xbT])
                    for ec in range(4):
                        pg, pu = pgu[(2 * ec) % 4], pgu[(2 * ec + 1) % 4]
                        sg = sgs[ec % 2]

                        def f(pe, pg=pg, pu=pu, ec=ec, wg_=wg_, wu_=wu_, xbT=xbT):
                            for kc in range(8):
                                pe.matmul(pg[:, :], wg_[:, kc, ec::4], xbT[:, kc, :], start=(kc == 0), stop=(kc == 7))
                            for kc in range(8):
                                ins = pe.matmul(pu[:, :], wu_[:, kc, ec::4], xbT[:, kc, :], start=(kc == 0), stop=(kc == 7))
                            return ins
                        k.op("pe", f, reads=[wg_, wu_, xbT], writes=[pg, pu])
                        k.op("act", lambda a, pg=pg, sg=sg: a.activation(out=sg[:], in_=pg[:, :], func=AF.Silu), reads=[pg], writes=[sg])
                        k.op("dve", lambda v, pu=pu, sg=sg, hid=hid, ec=ec: v.tensor_tensor(out=hid[:, ec, :], in0=pu[:, :], in1=sg[:], op=ALU.mult), reads=[pu, sg], writes=[hid])
                    for a_ in range(4):
                        yb = ybs[a_ % 2]
                        for nh in range(2):
                            py = pyy[nh]

                            def f(pe, py=py, a_=a_, nh=nh, hid=hid, wd_=wd_):
                                for ec in range(4):
                                    ins = pe.matmul(py[:, :], hid[:, ec, a_ * 128:(a_ + 1) * 128], wd_[:, ec, nh * 512:(nh + 1) * 512], start=(ec == 0), stop=(ec == 3))
                                return ins
                            k.op("pe", f, reads=[hid, wd_], writes=[py])
                            if nh == 0:
                                k.op("act", lambda a, py=py, yb=yb: a.copy(yb[:, 0:512], py[:, :]), reads=[py], writes=[yb])
                            else:
                                k.op("dve", lambda v, py=py, yb=yb: v.tensor_copy(yb[:, 512:1024], py[:, :]), reads=[py], writes=[yb])
                        k.dma("sp", yslot.t.ap()[b * MOE_S:(b + 1) * MOE_S, :].rearrange("(p a) d -> p a d", a=4)[:, a_, :], yb[:], reads=[yb], writes=[ysl_b[b]])
                k.barrier()
            with ExitStack() as e3:
                A5 = k.sb("A5", [128, D], F32, e3)
                A5c = k.sb("A5c", [128, D], F32, e3)
                if not last:
                    mod_bc(A5c, l, 4, 5)
                xts = [k.sb("xtc", [128, D], F32, e3) for _ in range(2)]
                o1s = [k.sb("o1c", [128, D], F32, e3) for _ in range(2)]
                o2s = [k.sb("o2c", [128, D], F32, e3) for _ in range(2)]
                curj = None

                def ldC(t_):
                    j_, ti_ = tiles[t_]
                    r0_ = tok_row(j_, ti_)
                    k.dma("sp", xts[t_ % 2][:], xres.t.ap()[r0_:r0_ + 128, :], reads=[xres_b[(j_, ti_)]], writes=[xts[t_ % 2]])
                for t, (j, ti) in enumerate(tiles):
                    col, isctx = TT[ti]
                    if j != curj:
                        mod_bc(A5, l, j, 5)
                        curj = j
                    xt, o1_, o2_ = xts[t % 2], o1s[t % 2], o2s[t % 2]
                    r0 = tok_row(j, ti)
                    xb_ = xres_b[(j, ti)]
                    if t == 0:
                        ldC(0)
                    if t + 1 < nt:
                        ldC(t + 1)
                    k.idma(o1_[:], None, yslot.t.ap(), bass.IndirectOffsetOnAxis(ap=DSTi[:, t, 0:1], axis=0), reads=[DSTi] + ysl_b, writes=[o1_])
                    k.idma(o2_[:], None, yslot.t.ap(), bass.IndirectOffsetOnAxis(ap=DSTi[:, t, 1:2], axis=0), reads=[DSTi] + ysl_b, writes=[o2_])
                    k.op("dve", lambda v, o1_=o1_, t=t: v.tensor_scalar(out=o1_[:], in0=o1_[:], scalar1=GT[:, t, 0:1], scalar2=None, op0=ALU.mult), reads=[o1_, GT], writes=[o1_])
                    k.op("dve", lambda v, o1_=o1_, o2_=o2_, t=t: v.scalar_tensor_tensor(out=o1_[:], in0=o2_[:], scalar=GT[:, t, 1:2], in1=o1_[:], op0=ALU.mult, op1=ALU.add), reads=[o1_, o2_, GT], writes=[o1_])
                    Ax = A5c if isctx else A5
                    k.op("pool", lambda g, o1_=o1_, Ax=Ax: g.tensor_tensor(out=o1_[:], in0=o1_[:], in1=Ax[:], op=ALU.mult), reads=[o1_, Ax], writes=[o1_])
                    k.op("dve", lambda v, o1_=o1_, xt=xt: v.tensor_tensor(out=o1_[:], in0=o1_[:], in1=xt[:], op=ALU.add), reads=[o1_, xt], writes=[o1_])
                    k.dma("sp", xres.t.ap()[r0:r0 + 128, :], o1_[:], reads=[o1_], writes=[xb_])
                k.barrier()

    def final_phase(j):
        with ExitStack() as e2:
            gf = k.sb("gf", [128, D], F32, e2)
            z0 = k.sb("z0", [128, D], F32, e2)
            k.dma("sp", gf[:], IN("g_final")[0:1, :].to_broadcast([128, D]), writes=[gf])
            k.op("dve", lambda v: v.memset(z0[:], 0.0), writes=[z0])
            xts = [k.sb("xtf", [128, D], F32, e2) for _ in range(2)]
            hos = [k.sb("hof", [128, D], F32, e2) for _ in range(2)]
            tmp = k.sb("tmpf", [128, D], F32, e2)
            sts = [[k.sb("stf", [128, 1], F32, e2) for _ in range(3)] for _ in range(2)]
            for ti in range(2, 18):
                xt, ho, st = xts[ti % 2], hos[ti % 2], sts[ti % 2]
                r0 = tok_row(j, ti)
                k.dma("sp", xt[:], xres.t.ap()[r0:r0 + 128, :], reads=[xres_b[(j, ti)]], writes=[xt])
                norm_mod(xt, gf, z0, tmp, ho, st)
                k.dma("sp", out_d.ap()[j, (ti - 2) * 128:(ti - 1) * 128, :], ho[:], reads=[ho])
            k.barrier()

    def finish():
        if "hT" in dbg:
            o = dbg_out("hT", [128, 8, PW], BF16)
            k.dma("sp", o.ap(), hT[:], reads=[hT])
        if "yT" in dbg:
            o = dbg_out("yT", [128, 8, PW], BF16)
            k.dma("sp", o.ap(), yT[:], reads=[yT])
        if "Hd" in dbg:
            for tg_, Lx_ in (("l", L), ("c", LC)):
                o = dbg_out("Hd_" + tg_, [2, Lx_, 512])
                k.dma("sp", o.ap(), Hd[tg_].t.ap(), reads=[Hd[tg_]])
        if "xres" in dbg:
            o = dbg_out("xres", [ntok, D])
            k.dma("sp", o.ap(), xres.t.ap(), reads=list(xres_b.values()))
        k.wait_all("sp")
        k.barrier()
        es.close()
        return nc, dbg_t

    steps = stop_after or "all"
    for l in range(layers):
        last = (l == DEPTH - 1)
        if steps in ("d", "all", "m"):
            hyena_prep(l)
        for j in range(nseq):
            stage1(l, j)
            if steps == "s1":
                return finish()
            if steps in ("a", "all", "w", "m"):
                mixer_a(l, j)
            if steps == "a":
                return finish()
            if steps in ("c", "all", "w", "m"):
                mixer_c(l, j)
            if steps == "c":
                return finish()
            if steps in ("b", "all", "m"):
                mixer_b(l, j)
            if steps == "b":
                return finish()
            if steps in ("d", "all", "m"):
                mixer_d(l, j)
            if steps == "d":
                return finish()
            wout_phase(l, j, last)
            if steps == "w":
                return finish()
        if steps in ("all", "m"):
            ffn_phase(l, last)
        if steps == "m":
            return finish()
    for j in range(nseq):
        final_phase(j)
    return finish()


def kernel(**inputs):
    consts = host_consts()
    n_cores = 8
    maps = []
    for c in range(n_cores):
        m = layout_inputs(inputs, c)
        maps.append(m)
    nc, _ = build(maps[0], consts)
    in_maps = []
    for m in maps:
        im = dict(m)
        im.update(consts)
        in_maps.append(im)
    res = run_bass_kernel_spmd(nc, in_maps, core_ids=list(range(n_cores)))
    out = np.concatenate([np.asarray(r["out"], np.float32) for r in res.results], axis=0)
    return out
```
